# Optimizing a Trainium2 kernel written in Bass

```python
import math
import jax, jax.numpy as jnp
from jax import lax
import numpy as np

D_MODEL = 2048
BATCH = 4
SEQ = 2048
DEPTH = 2
DEC_BATCH = 128
DEC_SEQ = 4
PAST_LEN = 16384
PAGE_SIZE = 128

N_EVEN = (DEPTH + 1) // 2
N_ODD = DEPTH // 2
S5_WIDTH = D_MODEL // 2
S5_GROUP = 16
S5_GROUPS = S5_WIDTH // S5_GROUP
S5_STATE = 64
GLA_HEADS = 4
GLA_DV = D_MODEL - S5_WIDTH
GLA_DK = GLA_DV // 2
GLA_HK = GLA_DK // GLA_HEADS
GLA_HV = GLA_DV // GLA_HEADS
GLA_LOWRANK = 16
GLA_TAU = 16.0
GLA_CHUNK = 64
EVEN_IN = S5_WIDTH + 2 * GLA_DK + 2 * GLA_DV + GLA_LOWRANK
ML_HEADS = 4
ML_WIDTH = D_MODEL
ML_DH = ML_WIDTH // ML_HEADS
ML_CHUNK = 64
ODD_IN = 4 * ML_WIDTH + 2 * ML_HEADS
D_FF = 4 * D_MODEL
EPS = 1e-6
F32 = jnp.float32

kernel_name = 'hybrid_s5_gla_mlstm_decode_step'


def rmsnorm(x, g):
    x32 = x.astype(F32)
    y = x32 * lax.rsqrt(jnp.mean(x32 * x32, axis=-1, keepdims=True) + EPS)
    return (y * g.astype(F32)).astype(x.dtype)


def head_rmsnorm(x, g):
    return x * lax.rsqrt(jnp.mean(x * x, axis=-1, keepdims=True) + EPS) * g.astype(F32)


def _complex_affine_combine(e1, e2):
    a1r, a1i, b1r, b1i = e1
    a2r, a2i, b2r, b2i = e2
    return (a2r * a1r - a2i * a1i, a2r * a1i + a2i * a1r,
            a2r * b1r - a2i * b1i + b2r, a2r * b1i + a2i * b1r + b2i)


def s5_mixer(u, h_re, h_im, a_re, a_im, log_step, b_re, b_im, c_re, c_im, d_skip, glu_w, glu_b):
    bsz, seq, _ = u.shape
    u32 = u.astype(F32).reshape(bsz, seq, S5_GROUPS, S5_GROUP)
    ar = a_re.astype(F32)
    ai = a_im.astype(F32)
    dt = jnp.exp(log_step.astype(F32))[:, None]
    mag = jnp.exp(ar * dt)
    abar_re = mag * jnp.cos(ai * dt)
    abar_im = mag * jnp.sin(ai * dt)
    lam2 = ar * ar + ai * ai
    zr = abar_re - 1.0
    cr = (zr * ar + abar_im * ai) / lam2
    ci = (abar_im * ar - zr * ai) / lam2
    br = b_re.astype(F32)
    bi = b_im.astype(F32)
    bbar_re = cr[..., None] * br - ci[..., None] * bi
    bbar_im = cr[..., None] * bi + ci[..., None] * br
    bu_re = jnp.einsum('blgq,gpq->blgp', u32, bbar_re)
    bu_im = jnp.einsum('blgq,gpq->blgp', u32, bbar_im)
    h_re = h_re.astype(F32)
    h_im = h_im.astype(F32)
    bu_re = bu_re.at[:, 0].add(abar_re * h_re - abar_im * h_im)
    bu_im = bu_im.at[:, 0].add(abar_re * h_im + abar_im * h_re)
    a_r = jnp.broadcast_to(abar_re, bu_re.shape)
    a_i = jnp.broadcast_to(abar_im, bu_im.shape)
    _, _, x_re, x_im = lax.associative_scan(_complex_affine_combine, (a_r, a_i, bu_re, bu_im), axis=1)
    y = (jnp.einsum('blgp,gqp->blgq', x_re, c_re.astype(F32))
         - jnp.einsum('blgp,gqp->blgq', x_im, c_im.astype(F32)))
    y = y + d_skip.astype(F32).reshape(S5_GROUPS, S5_GROUP) * u32
    y = jax.nn.gelu(y.reshape(bsz, seq, S5_WIDTH))
    out = y * jax.nn.sigmoid(y @ glu_w.astype(F32) + glu_b.astype(F32))
    return out, x_re[:, -1], x_im[:, -1]


def gla_mixer(q, k, v, g, gk_low, s0, gk_up, gk_b, norm_w):
    bsz, seq, _ = q.shape
    H = GLA_HEADS
    q = q.astype(F32).reshape(bsz, seq, H, GLA_HK) * (GLA_HK ** -0.5)
    k = k.astype(F32).reshape(bsz, seq, H, GLA_HK)
    v = v.astype(F32).reshape(bsz, seq, H, GLA_HV)
    log_a = jax.nn.log_sigmoid(gk_low.astype(F32) @ gk_up.astype(F32) + gk_b.astype(F32)) / GLA_TAU
    log_a = log_a.reshape(bsz, seq, H, GLA_HK)
    csz = math.gcd(seq, GLA_CHUNK)
    nch = seq // csz

    def to_chunks(t):
        return t.reshape(bsz, nch, csz, H, -1).transpose(1, 0, 3, 2, 4)

    mask = jnp.tril(jnp.ones((csz, csz), dtype=bool))

    def step(s, inp):
        qc, kc, vc, lac = inp
        bcum = jnp.cumsum(lac, axis=2)
        blast = bcum[:, :, -1:]
        qd = qc * jnp.exp(bcum)
        kd = kc * jnp.exp(-bcum)
        att = jnp.where(mask, jnp.einsum('bhtd,bhsd->bhts', qd, kd), 0.0)
        o = jnp.einsum('bhts,bhse->bhte', att, vc) + jnp.einsum('bhtd,bhde->bhte', qd, s)
        s_new = (jnp.exp(blast[:, :, 0])[..., None] * s
                 + jnp.einsum('bhsd,bhse->bhde', kc * jnp.exp(blast - bcum), vc))
        return s_new, o

    s_fin, o = lax.scan(step, s0.astype(F32), (to_chunks(q), to_chunks(k), to_chunks(v), to_chunks(log_a)))
    o = o.transpose(1, 0, 3, 2, 4).reshape(bsz, seq, H, GLA_HV)
    o = head_rmsnorm(o, norm_w).reshape(bsz, seq, GLA_DV)
    return o * jax.nn.silu(g.astype(F32)), s_fin


def mlstm_mixer(q, k, v, o_gate, ig, fg, c0, n0, m0, norm_w):
    bsz, seq, _ = q.shape
    H = ML_HEADS
    q = q.astype(F32).reshape(bsz, seq, H, ML_DH)
    k = k.astype(F32).reshape(bsz, seq, H, ML_DH) * (ML_DH ** -0.5)
    v = v.astype(F32).reshape(bsz, seq, H, ML_DH)
    logf = jax.nn.log_sigmoid(fg.astype(F32))
    ig = ig.astype(F32)
    csz = math.gcd(seq, ML_CHUNK)
    nch = seq // csz

    def to_chunks(t):
        return t.reshape(bsz, nch, csz, H, -1).transpose(1, 0, 3, 2, 4)

    def gate_chunks(t):
        return t.reshape(bsz, nch, csz, H).transpose(1, 0, 3, 2)

    mask = jnp.tril(jnp.ones((csz, csz), dtype=bool))

    def step(carry, inp):
        cm, nm, mm = carry
        qc, kc, vc, ic, lfc = inp
        fcum = jnp.cumsum(lfc, axis=-1)
        dmat = jnp.where(mask, fcum[..., :, None] - fcum[..., None, :] + ic[..., None, :], -jnp.inf)
        dprev = fcum + mm[..., None]
        m = jnp.maximum(jnp.max(dmat, axis=-1), dprev)
        w = jnp.exp(dmat - m[..., None])
        wp = jnp.exp(dprev - m)
        sc = jnp.einsum('bhtd,bhsd->bhts', qc, kc) * w
        num = jnp.einsum('bhts,bhse->bhte', sc, vc) + wp[..., None] * jnp.einsum('bhtd,bhde->bhte', qc, cm)
        den = jnp.sum(sc, axis=-1) + wp * jnp.einsum('bhtd,bhd->bht', qc, nm)
        h = num / jnp.maximum(jnp.abs(den), jnp.exp(-m))[..., None]
        m_new = m[..., -1]
        decay = jnp.exp(fcum[..., -1] + mm - m_new)
        wk = jnp.exp(fcum[..., -1:] - fcum + ic - m_new[..., None])
        c_new = decay[..., None, None] * cm + jnp.einsum('bhs,bhsd,bhse->bhde', wk, kc, vc)
        n_new = decay[..., None] * nm + jnp.einsum('bhs,bhsd->bhd', wk, kc)
        return (c_new, n_new, m_new), h

    (c_f, n_f, m_f), h = lax.scan(
        step, (c0.astype(F32), n0.astype(F32), m0.astype(F32)),
        (to_chunks(q), to_chunks(k), to_chunks(v), gate_chunks(ig), gate_chunks(logf)))
    h = h.transpose(1, 0, 3, 2, 4).reshape(bsz, seq, H, ML_DH)
    h = h * jax.nn.sigmoid(o_gate.astype(F32)).reshape(bsz, seq, H, ML_DH)
    h = head_rmsnorm(h, norm_w).reshape(bsz, seq, ML_WIDTH)
    return h, c_f, n_f, m_f


def sq_relu_mlp(x, w_up, w_down):
    hid = jnp.square(jax.nn.relu(x @ w_up))
    return hid @ w_down


def trunk(x, s5_re, s5_im, gla_s, ml_c, ml_n, ml_m, W):
    h = x
    out_s5_re, out_s5_im, out_gla, out_c, out_n, out_m = [], [], [], [], [], []
    for layer in range(DEPTH):
        xn = rmsnorm(h, W['norm_mix'][layer])
        if layer % 2 == 0:
            e = layer // 2
            proj = xn @ W['w_in_even'][e]
            o1 = S5_WIDTH
            o2 = o1 + GLA_DK
            o3 = o2 + GLA_DK
            o4 = o3 + GLA_DV
            o5 = o4 + GLA_DV
            u, q, k, v, g, gk_low = (proj[..., :o1], proj[..., o1:o2], proj[..., o2:o3],
                                     proj[..., o3:o4], proj[..., o4:o5], proj[..., o5:])
            y_s5, hr, hi = s5_mixer(u, s5_re[e], s5_im[e], W['s5_a_re'][e], W['s5_a_im'][e],
                                    W['s5_log_step'][e], W['s5_b_re'][e], W['s5_b_im'][e],
                                    W['s5_c_re'][e], W['s5_c_im'][e], W['s5_d'][e],
                                    W['s5_glu_w'][e], W['s5_glu_b'][e])
            y_gla, sg = gla_mixer(q, k, v, g, gk_low, gla_s[e], W['gla_gk_up'][e],
                                  W['gla_gk_b'][e], W['gla_norm'][e])
            mix = jnp.concatenate([y_s5, y_gla], axis=-1).astype(h.dtype) @ W['w_out_even'][e]
            out_s5_re.append(hr)
            out_s5_im.append(hi)
            out_gla.append(sg)
        else:
            o = layer // 2
            proj = xn @ W['w_in_odd'][o]
            q = proj[..., 0:ML_WIDTH]
            k = proj[..., ML_WIDTH:2 * ML_WIDTH]
            v = proj[..., 2 * ML_WIDTH:3 * ML_WIDTH]
            og = proj[..., 3 * ML_WIDTH:4 * ML_WIDTH]
            ig = proj[..., 4 * ML_WIDTH:4 * ML_WIDTH + ML_HEADS].astype(F32) + W['mlstm_b_i'][o].astype(F32)
            fg = proj[..., 4 * ML_WIDTH + ML_HEADS:].astype(F32) + W['mlstm_b_f'][o].astype(F32)
            y_ml, cf, nf, mf = mlstm_mixer(q, k, v, og, ig, fg, ml_c[o], ml_n[o], ml_m[o], W['mlstm_norm'][o])
            mix = y_ml.astype(h.dtype) @ W['w_out_odd'][o]
            out_c.append(cf)
            out_n.append(nf)
            out_m.append(mf)
        h = h + mix.astype(h.dtype)
        h = h + sq_relu_mlp(rmsnorm(h, W['norm_mlp'][layer]), W['w_mlp_up'][layer], W['w_mlp_down'][layer]).astype(h.dtype)
    y = rmsnorm(h, W['norm_final'])
    return (y, jnp.stack(out_s5_re), jnp.stack(out_s5_im), jnp.stack(out_gla),
            jnp.stack(out_c), jnp.stack(out_n), jnp.stack(out_m))


def setup_inputs(seed: int = 0) -> dict:
    key = jax.random.key(seed)
    keys = list(jax.random.split(key, 64))

    def nk():
        return keys.pop()

    def normal(shape, scale):
        return jax.random.normal(nk(), shape, F32) * scale

    G, P, Q = S5_GROUPS, S5_STATE, S5_GROUP
    D = D_MODEL
    inputs = {}
    inputs['x_prompt'] = normal((BATCH, SEQ, D), 1.0)
    inputs['x_sample'] = normal((DEC_BATCH, DEC_SEQ, D), 1.0)
    inputs['state_s5_re'] = normal((N_EVEN, DEC_BATCH, G, P), 0.5)
    inputs['state_s5_im'] = normal((N_EVEN, DEC_BATCH, G, P), 0.5)
    inputs['state_gla'] = normal((N_EVEN, DEC_BATCH, GLA_HEADS, GLA_HK, GLA_HV), 0.3)
    inputs['state_mlstm_c'] = normal((N_ODD, DEC_BATCH, ML_HEADS, ML_DH, ML_DH), 0.1)
    inputs['state_mlstm_n'] = normal((N_ODD, DEC_BATCH, ML_HEADS, ML_DH), 0.5)
    inputs['state_mlstm_m'] = normal((N_ODD, DEC_BATCH, ML_HEADS), 1.0)
    inputs['norm_mix'] = 1.0 + normal((DEPTH, D), 0.02)
    inputs['norm_mlp'] = 1.0 + normal((DEPTH, D), 0.02)
    inputs['norm_final'] = 1.0 + normal((D,), 0.02)
    inputs['w_in_even'] = normal((N_EVEN, D, EVEN_IN), D ** -0.5)
    inputs['s5_a_re'] = -0.5 + normal((N_EVEN, G, P), 0.01)
    inputs['s5_a_im'] = jnp.pi * jnp.arange(P, dtype=F32) + normal((N_EVEN, G, P), 0.01)
    inputs['s5_log_step'] = jax.random.uniform(nk(), (N_EVEN, G), F32, math.log(1e-3), math.log(1e-1))
    inputs['s5_b_re'] = normal((N_EVEN, G, P, Q), (2 * Q) ** -0.5)
    inputs['s5_b_im'] = normal((N_EVEN, G, P, Q), (2 * Q) ** -0.5)
    inputs['s5_c_re'] = normal((N_EVEN, G, Q, P), P ** -0.5)
    inputs['s5_c_im'] = normal((N_EVEN, G, Q, P), P ** -0.5)
    inputs['s5_d'] = normal((N_EVEN, S5_WIDTH), 1.0)
    inputs['s5_glu_w'] = normal((N_EVEN, S5_WIDTH, S5_WIDTH), S5_WIDTH ** -0.5)
    inputs['s5_glu_b'] = normal((N_EVEN, S5_WIDTH), 0.02)
    inputs['gla_gk_up'] = normal((N_EVEN, GLA_LOWRANK, GLA_DK), GLA_LOWRANK ** -0.5)
    inputs['gla_gk_b'] = normal((N_EVEN, GLA_DK), 0.02)
    inputs['gla_norm'] = 1.0 + normal((N_EVEN, GLA_HV), 0.02)
    inputs['w_out_even'] = normal((N_EVEN, D, D), D ** -0.5)
    inputs['w_in_odd'] = normal((N_ODD, D, ODD_IN), D ** -0.5)
    inputs['mlstm_b_i'] = normal((N_ODD, ML_HEADS), 0.1)
    inputs['mlstm_b_f'] = jax.random.uniform(nk(), (N_ODD, ML_HEADS), F32, 3.0, 6.0)
    inputs['mlstm_norm'] = 1.0 + normal((N_ODD, ML_DH), 0.02)
    inputs['w_out_odd'] = normal((N_ODD, D, D), D ** -0.5)
    inputs['w_mlp_up'] = normal((DEPTH, D, D_FF), D ** -0.5)
    inputs['w_mlp_down'] = normal((DEPTH, D_FF, D), D_FF ** -0.5)
    return inputs


def reference(x_prompt, x_sample, state_s5_re, state_s5_im, state_gla, state_mlstm_c, state_mlstm_n,
              state_mlstm_m, norm_mix, norm_mlp, norm_final, w_in_even, s5_a_re, s5_a_im, s5_log_step,
              s5_b_re, s5_b_im, s5_c_re, s5_c_im, s5_d, s5_glu_w, s5_glu_b, gla_gk_up, gla_gk_b,
              gla_norm, w_out_even, w_in_odd, mlstm_b_i, mlstm_b_f, mlstm_norm, w_out_odd,
              w_mlp_up, w_mlp_down):
    W = dict(norm_mix=norm_mix, norm_mlp=norm_mlp, norm_final=norm_final, w_in_even=w_in_even,
             s5_a_re=s5_a_re, s5_a_im=s5_a_im, s5_log_step=s5_log_step, s5_b_re=s5_b_re,
             s5_b_im=s5_b_im, s5_c_re=s5_c_re, s5_c_im=s5_c_im, s5_d=s5_d, s5_glu_w=s5_glu_w,
             s5_glu_b=s5_glu_b, gla_gk_up=gla_gk_up, gla_gk_b=gla_gk_b, gla_norm=gla_norm,
             w_out_even=w_out_even, w_in_odd=w_in_odd, mlstm_b_i=mlstm_b_i, mlstm_b_f=mlstm_b_f,
             mlstm_norm=mlstm_norm, w_out_odd=w_out_odd, w_mlp_up=w_mlp_up, w_mlp_down=w_mlp_down)
    bp = x_prompt.shape[0]
    z_s5 = jnp.zeros((N_EVEN, bp, S5_GROUPS, S5_STATE), F32)
    z_gla = jnp.zeros((N_EVEN, bp, GLA_HEADS, GLA_HK, GLA_HV), F32)
    z_c = jnp.zeros((N_ODD, bp, ML_HEADS, ML_DH, ML_DH), F32)
    z_n = jnp.zeros((N_ODD, bp, ML_HEADS, ML_DH), F32)
    z_m = jnp.full((N_ODD, bp, ML_HEADS), -jnp.inf, F32)
    y_prompt, p_s5_re, p_s5_im, p_gla, p_c, p_n, p_m = trunk(x_prompt, z_s5, z_s5, z_gla, z_c, z_n, z_m, W)
    y_sample, s_s5_re, s_s5_im, s_gla, s_c, s_n, s_m = trunk(
        x_sample, state_s5_re, state_s5_im, state_gla, state_mlstm_c, state_mlstm_n, state_mlstm_m, W)
    return (y_prompt, y_sample, p_s5_re, p_s5_im, p_gla, p_c, p_n, p_m,
            s_s5_re, s_s5_im, s_gla, s_c, s_n, s_m)
```

```python
import contextlib
import math
import numpy as np
import concourse.bass as bass
import concourse.mybir as mybir
from concourse.bass_utils import run_bass_kernel_spmd

F32 = mybir.dt.float32
BF16 = mybir.dt.bfloat16
AF = mybir.ActivationFunctionType
ALU = mybir.AluOpType
EPS = 1e-6
NEG = -1.0e30

CH = 16000
COMPUTE = ("tensor", "vector", "scalar", "gpsimd")
ENGS = ("tensor", "vector", "scalar", "gpsimd", "sync")


class Prog:
    def __init__(self, nc):
        self.nc = nc
        self.ops = {e: [] for e in ENGS}
        self.last_w = {}
        self.readers = {}
        self.seen = {e: {} for e in ENGS}
        self.dma_cnt = {}
        self.stack = contextlib.ExitStack()

    def sb(self, name, shape, dt):
        return self.stack.enter_context(self.nc.sbuf_tensor(name, list(shape), dt))

    def ps(self, name, shape, dt):
        return self.stack.enter_context(self.nc.psum_tensor(name, list(shape), dt))

    def _deps(self, eng, reads, writes):
        deps = []
        for r in reads:
            ev = self.last_w.get(r)
            if ev is not None:
                deps.append(ev)
            if r.startswith("pb"):
                deps.extend(o for o in self.readers.get(r, ()) if o[1] != eng)
        for w in writes:
            ev = self.last_w.get(w)
            if ev is not None:
                deps.append(ev)
            deps.extend(self.readers.get(w, ()))
        best = {}
        for (kind, who, idx) in deps:
            if kind == "eng" and who == "tensor" and eng == "tensor":
                continue
            key = (kind, who)
            if best.get(key, -1) < idx:
                best[key] = idx
        out = []
        seen = self.seen[eng]
        for key, idx in best.items():
            if seen.get(key, -1) >= idx:
                continue
            seen[key] = idx
            out.append((key[0], key[1], idx))
        return out

    def _commit(self, ev, reads, writes):
        for w in writes:
            self.last_w[w] = ev
            self.readers[w] = []
        for r in reads:
            if r not in writes:
                lst = self.readers.setdefault(r, [])
                for i, o in enumerate(lst):
                    if o[0] == ev[0] and o[1] == ev[1]:
                        lst[i] = ev
                        break
                else:
                    lst.append(ev)

    def op(self, eng, fn, reads=(), writes=()):
        reads = list(reads)
        writes = list(writes)
        deps = self._deps(eng, reads, writes)
        ev = ("eng", eng, len(self.ops[eng]))
        self.ops[eng].append(dict(fn=fn, deps=deps, ev=ev, dma=None))
        self._commit(ev, reads, writes)
        return ev

    def dma(self, fn, slot, reads=(), writes=(), queue="sync"):
        reads = list(reads)
        writes = list(writes)
        deps = self._deps(queue, reads, writes)
        k = self.dma_cnt.get(slot, 0) + 1
        self.dma_cnt[slot] = k
        ev = ("dma", slot, k)
        self.ops[queue].append(dict(fn=fn, deps=deps, ev=None, dma=(slot, k)))
        self._commit(ev, reads, writes)
        return ev

    def seal(self, slot):
        k = self.dma_cnt.get(slot, 0)
        for key, ev in list(self.last_w.items()):
            if ev[0] == "dma" and ev[1] == slot:
                self.last_w[key] = ("dma", slot, k)

    def emit(self, final_waits=()):
        nc = self.nc
        waited = {e: set() for e in COMPUTE}
        for e in ENGS:
            for o in self.ops[e]:
                for (kind, who, idx) in o["deps"]:
                    if kind == "eng":
                        waited[who].add(idx)
        rank = {}
        for e in COMPUTE:
            for r, idx in enumerate(sorted(waited[e])):
                rank[(e, idx)] = r
        sems = {}
        for e in COMPUTE:
            for j in range((len(waited[e]) + CH - 1) // CH):
                sems[(e, j)] = self.stack.enter_context(nc.semaphore(f"s_{e}_{j}"))
        DCH = 1000
        dsems = {}
        for slot, tot in self.dma_cnt.items():
            for j in range((tot + DCH - 1) // DCH):
                dsems[(slot, j)] = self.stack.enter_context(nc.semaphore(f"d_{slot}_{j}"))
        self.stats = dict(nsem=len(sems) + len(dsems), nops={e: len(self.ops[e]) for e in ENGS},
                          waited={e: len(waited[e]) for e in COMPUTE})

        def wait(engobj, ev):
            kind, who, idx = ev
            if kind == "eng":
                r = rank[(who, idx)]
                engobj.wait_ge(sems[(who, r // CH)], r % CH + 1)
            else:
                engobj.wait_ge(dsems[(who, (idx - 1) // DCH)], 16 * ((idx - 1) % DCH + 1))

        block = self.stack.enter_context(nc.Block())

        def make(e):
            def body(engobj):
                for o in self.ops[e]:
                    for ev in o["deps"]:
                        wait(engobj, ev)
                    name, a, kw = o["fn"]
                    inst = getattr(engobj, name)(*a, **kw)
                    if o["dma"] is not None:
                        inst.then_inc(dsems[(o["dma"][0], (o["dma"][1] - 1) // DCH)], 16)
                    elif (e, o["ev"][2]) in rank:
                        r = rank[(e, o["ev"][2])]
                        inst.then_inc(sems[(e, r // CH)], 1)
                if e == "sync":
                    for ev in final_waits:
                        wait(engobj, ev)
            return body

        block.tensor(make("tensor"))
        block.vector(make("vector"))
        block.scalar(make("scalar"))
        block.gpsimd(make("gpsimd"))
        block.sync(make("sync"))
        self.stack.close()


def I(name, *a, **kw):
    return (name, a, kw)


class Cfg:
    def __init__(self, D=2048, GH=4, MH=4, SEQ=2048, NS=16, T=256):
        self.D, self.GH, self.MH, self.SEQ, self.NS, self.T = D, GH, MH, SEQ, NS, T
        self.KD = D // 128
        self.S5W = D // 2
        self.NG = self.S5W // 16
        self.NCT = self.NG // 2
        self.SC = self.S5W // 128
        self.GDV = D - self.S5W
        self.GDK = self.GDV // 2
        self.HK = self.GDK // GH
        self.HV = self.GDV // GH
        assert self.HK == 128 and self.HV == 256
        self.EIN = self.S5W + 2 * self.GDK + 2 * self.GDV + 16
        self.DH = D // MH
        assert self.DH == 512
        self.OIN = 4 * D + 2 * MH
        self.DFF = 4 * D
        self.DS = 4
        self.TS = NS * self.DS


WCOLS = 256


def build(cfg):
    c = cfg
    D, KD, T, NS = c.D, c.KD, c.T, c.NS
    nc = bass.Bass("TRN2", target_bir_lowering=False)
    P = Prog(nc)

    def din(name, shape):
        return nc.dram_tensor(name, list(shape), F32, kind="ExternalInput").ap()

    def dout(name, shape):
        return nc.dram_tensor(name, list(shape), F32, kind="ExternalOutput").ap()

    xp = din("xp", [c.SEQ, D]); xs = din("xs", [c.TS, D])
    st_s5 = din("st_s5", [2, 128, c.NCT, NS])
    st_gla = din("st_gla", [NS, c.GH, 128, 256])
    st_mc = din("st_mc", [NS, c.MH, 512, 512])
    st_mn = din("st_mn", [NS, c.MH, 128, 4])
    st_mm = din("st_mm", [1, NS * c.MH])
    nrm = din("nrm", [128, 5, KD])
    w_in_e = din("w_in_e", [D, c.EIN]); w_out_e = din("w_out_e", [D, D])
    w_in_o = din("w_in_o", [D, c.OIN]); w_out_o = din("w_out_o", [D, D])
    w_gate = din("w_gate", [D, 2 * c.MH * 128])
    w_up = din("w_up", [2, D, c.DFF]); w_dn = din("w_dn", [2, c.DFF, D])
    s5p = din("s5p", [128, 3, c.NCT])
    bemb = din("bemb", [2, 128, c.SC * 2, 128]); cemb = din("cemb", [2, 128, c.NCT, 128])
    s5d = din("s5d", [128, 2, c.SC])
    glu_w = din("glu_w", [c.S5W, c.S5W])
    gk_up = din("gk_up", [16, c.GDK]); gkb = din("gkb", [128, c.GH]); gnorm = din("gnorm", [128, 2])
    mb = din("mb", [1, 2 * c.MH]); mnorm = din("mnorm", [128, 4])
    ident_d = din("ident", [128, 128]); tri_d = din("tri", [128, 128]); blk_d = din("blk", [128, 128])

    yp = dout("yp", [c.SEQ, D]); ys = dout("ys", [c.TS, D])
    o_ps5 = dout("o_ps5", [2, 128, c.NCT]); o_ss5 = dout("o_ss5", [2, 128, c.NCT, NS])
    o_pgla = dout("o_pgla", [c.GH, 128, 256]); o_sgla = dout("o_sgla", [NS, c.GH, 128, 256])
    o_pmc = dout("o_pmc", [c.MH, 512, 512]); o_smc = dout("o_smc", [NS, c.MH, 512, 512])
    o_pmn = dout("o_pmn", [c.MH, 128, 4]); o_smn = dout("o_smn", [NS, c.MH, 128, 4])
    o_pmm = dout("o_pmm", [1, c.MH]); o_smm = dout("o_smm", [1, c.MH * NS])

    hT = P.sb("hT", [128, KD, T], F32)
    xnT = P.sb("xnT", [128, KD, T], BF16)
    yT = P.sb("yT", [128, KD, T], BF16)
    hid = yT
    xtok = P.sb("xtok", [128, D], F32)
    W4all = P.sb("w4all", [128, 4, 16, 256], BF16)
    W4 = [W4all[:, i] for i in range(4)]
    ws = [W4[i].bitcast(F32) for i in range(2)]
    W4K = ["w4_0", "w4_1", "w4_2", "w4_3"]
    rstdB = P.sb("rstdB", [128, T], F32)
    nrm_s = P.sb("nrm_s", [128, 5, KD], F32)
    ident = P.sb("ident_s", [128, 128], F32)
    tri = P.sb("tri_s", [128, 128], F32); blk = P.sb("blk_s", [128, 128], F32)
    ones_bf = P.sb("ones_bf", [128, 128], BF16)
    ones_f = P.sb("ones_f", [128, T], F32)
    s5p_s = P.sb("s5p_s", [128, 3, c.NCT], F32)
    s5d_s = P.sb("s5d_s", [128, 2, c.SC], F32)
    lam = P.sb("lam", [128, 10, 3, c.NCT], F32)
    cco = P.sb("cco", [128, 2, c.NCT], F32)
    cinv = P.sb("cinv", [128, 2, c.NCT], F32)
    ncci = P.sb("ncci", [128, c.NCT], F32)
    tA = P.sb("tA", [128, 6, c.NCT], F32)
    bemb_s = P.sb("bemb_s", [128, 2, c.SC * 2, 128], BF16)
    cemb_s = P.sb("cemb_s", [128, 2, c.NCT, 128], BF16)
    zst = P.sb("zst", [128, 2, c.NCT, NS], F32)
    scA = P.sb("scA", [128, 2, T], F32); scB = P.sb("scB", [128, 2, T], F32)
    zbf = P.sb("zbf", [128, 2, T], BF16)
    scA2 = P.sb("scA2", [128, 2, T], F32); scB2 = P.sb("scB2", [128, 2, T], F32); zbf2 = P.sb("zbf2", [128, 2, T], BF16)
    uT = P.sb("uT", [128, c.SC, T], BF16)
    ygl = P.sb("ygl", [128, c.SC, T], BF16)
    tmpA = P.sb("tmpA", [128, 4, T], F32)
    tmpB = P.sb("tmpB", [128, 2, T], F32)
    gkl = P.sb("gkl", [16, T], F32); gku = P.sb("gku", [16, c.GDK], F32)
    gkb_s = P.sb("gkb_s", [128, c.GH], F32); ngkb = P.sb("ngkb", [128, c.GH], F32)
    gnorm_s = P.sb("gnorm_s", [128, 2], F32)
    S_sb = P.sb("S_sb", [128, c.GH, 256], F32); S_bf = P.sb("S_bf", [128, 256], BF16)
    cmask = P.sb("cmask", [128, T], F32)
    qd = P.sb("qd", [128, T], BF16); kd = P.sb("kd", [128, T], BF16); kdc = P.sb("kdc", [128, T], BF16)
    Ep = P.sb("Ep", [128, T], F32); Em = P.sb("Em", [128, T], F32)
    vtok = P.sb("vtok", [128, 512], BF16); ktok = P.sb("ktok", [128, 512], BF16)
    kwt = P.sb("kwt", [128, 512], BF16)
    attT = P.sb("attT", [128, 128], BF16)
    gsil = P.sb("gsil", [128, 2, T], F32)
    oT = P.sb("oT", [128, 4, T], F32)
    mb_s = P.sb("mb_s", [128, 2 * c.MH], F32); nmb = P.sb("nmb", [128, 2 * c.MH], F32)
    mnorm_s = P.sb("mnorm_s", [128, 4], F32)
    mm0 = P.sb("mm0", [128, NS * c.MH], F32)
    C_sb = P.sb("C_sb", [128, c.MH, 4, 512], F32); C_bf = P.sb("C_bf", [128, 4, 512], BF16)
    n_sb = P.sb("n_sb", [128, c.MH, 4], F32); Nrep = P.sb("Nrep", [128, 4, 128], BF16)
    Fcar = P.sb("Fcar", [128, c.MH], F32); Mcar = P.sb("Mcar", [128, c.MH], F32)
    FgB = P.sb("FgB", [128, T], F32); MB = P.sb("MB", [128, T], F32); gB = P.sb("gB", [128, T], F32)
    wpB = P.sb("wpB", [128, T], F32); emB = P.sb("emB", [128, T], F32)
    gcol = P.sb("gcol", [128, 1], F32)
    wT = P.sb("wT", [128, 128], F32); wTm = P.sb("wTm", [128, 128], F32)
    qTb = P.sb("qTb", [128, 4, T], BF16); kTb = P.sb("kTb", [128, 4, T], BF16); qwp = P.sb("qwp", [128, 4, T], BF16)
    ogs = P.sb("ogs", [128, 4, T], BF16)
    rden = P.sb("rden", [128, 128], F32)
    mout = P.sb("mout", [1, c.MH * max(NS, 1)], F32)
    pb = [P.ps(f"pb{i}", [128, 512], F32) for i in range(8)]

    V, S, G, PE = "vector", "scalar", "gpsimd", "tensor"
    cnt = {"w": 0, "pd": 0}

    def ld(dst, src, key):
        P.dma(I("dma_start", out=dst, in_=src), "setup", writes=[key])

    ld(nrm_s[:], nrm, "nrm_s"); ld(ident[:], ident_d, "ident"); ld(tri[:], tri_d, "tri"); ld(blk[:], blk_d, "blk")
    ld(s5p_s[:], s5p, "s5p_s"); ld(s5d_s[:], s5d, "s5d_s")
    ld(gku[:], gk_up, "gku"); ld(gkb_s[:], gkb, "gkb_s"); ld(gnorm_s[:], gnorm, "gnorm_s")
    ld(mb_s[:], mb.broadcast_to([128, 2 * c.MH]), "mb_s"); ld(mnorm_s[:], mnorm, "mnorm_s")
    ld(mm0[:], st_mm.broadcast_to([128, NS * c.MH]), "mm0")
    P.seal("setup")
    P.op(V, I("memset", ones_bf[:], 1.0), writes=["ones_bf"])
    P.op(V, I("memset", ones_f[:], 1.0), writes=["ones_f"])
    P.op(V, I("tensor_scalar", out=ngkb[:], in0=gkb_s[:], scalar1=-1.0, scalar2=None, op0=ALU.mult),
         reads=["gkb_s"], writes=["ngkb"])
    P.op(V, I("tensor_scalar", out=nmb[:], in0=mb_s[:], scalar1=-1.0, scalar2=None, op0=ALU.mult),
         reads=["mb_s"], writes=["nmb"])

    def tt(out, a, b, op, keys=("tiny",)):
        P.op(V, I("tensor_tensor", out=out, in0=a, in1=b, op=op), reads=keys, writes=keys)

    def tsc(out, a, s1, op0, s2=None, op1=None, keys=("tiny",)):
        if op1 is None:
            P.op(V, I("tensor_scalar", out=out, in0=a, scalar1=s1, scalar2=None, op0=op0), reads=keys, writes=keys)
        else:
            P.op(V, I("tensor_scalar", out=out, in0=a, scalar1=s1, scalar2=s2, op0=op0, op1=op1), reads=keys, writes=keys)

    def act(out, a, func, keys=("tiny",), **kw):
        P.op(S, I("activation", out=out, in_=a, func=func, **kw), reads=keys, writes=keys)

    def wtile(scrt, cb0, ncb, k0, kc):
        i = cnt["w"] % 4
        cnt["w"] += 1
        view = W4[i].rearrange("p a b -> p (a b)")[:, 0:ncb * kc * 128].rearrange("p (c k n) -> p c k n", c=ncb, k=kc)
        P.dma(I("dma_start", out=view, in_=scrt[cb0:cb0 + ncb, :, k0:k0 + kc, :].rearrange("c p k n -> p c k n")),
              f"w4_{i}", reads=["dw0", "dw1"], writes=[f"w4_{i}"])
        return view, f"w4_{i}"

    def pbank(lo=0, hi=4):
        i = lo + cnt["pd"] % (hi - lo)
        cnt["pd"] += 1
        return pb[i], f"pb{i}"

    def dense_fm(w, kin, ncols_total, inT, inkey, Tn, epi, rows0=0, col0=0):
        assert col0 % 128 == 0 and rows0 % 128 == 0 and ncols_total % 128 == 0
        nb_tot = ncols_total // 128
        for b0 in range(0, nb_tot, 2):
            ncb = min(2, nb_tot - b0)
            wt, wkey = wtile(w, col0 // 128 + b0, ncb, rows0 // 128, kin)
            for sub in range(ncb):
                ps, pkey = pbank()
                for k in range(kin):
                    P.op(PE, I("matmul", out=ps[:, 0:Tn], lhsT=wt[:, sub, k, :], rhs=inT[:, k, 0:Tn],
                               start=(k == 0), stop=(k == kin - 1)), reads=[wkey, inkey], writes=[pkey])
                epi(b0 + sub, ps, pkey, 128)

    def rmsnorm(idx, Tn, dst=None):
        for k in range(KD):
            P.op(S, I("activation", out=xnT[:, k, 0:Tn], in_=hT[:, k, 0:Tn], func=AF.Square),
                 reads=["hT"], writes=["xnT"])
        ps, pkey = pbank()
        for k in range(KD):
            P.op(PE, I("matmul", out=ps[:, 0:Tn], lhsT=ones_bf[:, :], rhs=xnT[:, k, 0:Tn],
                                            start=(k == 0), stop=(k == KD - 1)),
                 reads=["xnT", "ones_bf"], writes=[pkey])
        P.op(S, I("activation", out=rstdB[:, 0:Tn], in_=ps[:, 0:Tn], func=AF.Sqrt, scale=1.0 / D, bias=EPS),
             reads=[pkey], writes=["rstdB"])
        P.op(V, I("reciprocal", out=rstdB[:, 0:Tn], in_=rstdB[:, 0:Tn]), reads=["rstdB"], writes=["rstdB"])
        for k in range(KD):
            o = xnT[:, k, 0:Tn] if dst is None else dst[:, k, 0:Tn]
            P.op(V, I("scalar_tensor_tensor",
                out=o, in0=hT[:, k, 0:Tn], scalar=nrm_s[:, idx, k:k + 1], in1=rstdB[:, 0:Tn],
                op0=ALU.mult, op1=ALU.mult), reads=["hT", "rstdB", "nrm_s"], writes=["xnT" if dst is None else "hT"])

    def scr(name, rows, cols):
        return nc.dram_tensor(name, [(cols + 127) // 128, 128, rows // 128, 128], BF16, kind="Internal").ap()

    bw_in_e = scr("bw_in_e", D, c.EIN); bw_out_e = scr("bw_out_e", D, D)
    bw_in_o = scr("bw_in_o", D, 4 * D); bw_out_o = scr("bw_out_o", D, D)
    bw_gate = scr("bw_gate", D, 2 * c.MH * 128)
    bw_up = [scr(f"bw_up{l_}", D, c.DFF) for l_ in range(2)]; bw_dn = [scr(f"bw_dn{l_}", c.DFF, D) for l_ in range(2)]
    bglu = scr("bglu", c.S5W, c.S5W)
    cnt["pc"] = 0
    stg_f = [C_sb[:].rearrange("p a b c -> p (a b c)"), W4all[:].bitcast(F32).rearrange("p a b c -> p (a b c)")]
    stg_k = [[f"C_sb{i}" for i in range(c.MH)], W4K]
    ob_b = [hT[:].bitcast(BF16).rearrange("p a b -> p (a b)"), cemb_s[:].rearrange("p a b c -> p (a b c)")]
    ob_k = ["hT", "cemb_s"]
    PCW = 2048
    kb_max = min(4, c.MH * 4 * 512 // PCW, 8192 // PCW, KD * T * 2 // PCW, 2 * c.NCT * 128 // PCW)
    assert kb_max >= 1

    def precast(src, dst, rows, cols):
        KC = rows // 128
        for c0 in range(0, cols, PCW):
            ncol = min(PCW, cols - c0)
            ncb = (ncol + 127) // 128
            n_ = min(128, ncol)
            assert ncol % 128 == 0 or ncb == 1
            for k0 in range(0, KC, kb_max):
                kb = min(kb_max, KC - k0)
                i = cnt["pc"] % 2
                cnt["pc"] += 1
                stg = stg_f[i][:, 0:kb * PCW].rearrange("p (k n) -> p k n", k=kb)
                ob = ob_b[i][:, 0:ncb * kb * 128].rearrange("p (c k n) -> p c k n", c=ncb, k=kb)
                P.dma(I("dma_start", out=stg[:, :, 0:ncol], in_=src[k0 * 128:(k0 + kb) * 128, c0:c0 + ncol].rearrange("(k p) n -> p k n", p=128)),
                      f"pcl{i}", writes=stg_k[i])
                cin = stg[:, :, 0:ncb * n_].rearrange("p k (c n) -> p c k n", c=ncb)
                if n_ < 128:
                    P.op(V, I("memset", ob[:, :, :, :], 0.0), reads=[ob_k[i]], writes=[ob_k[i]])
                if i == 0:
                    P.op(V, I("tensor_copy", out=ob[:, :, :, 0:n_], in_=cin), reads=stg_k[i], writes=[ob_k[i]])
                else:
                    P.op(S, I("copy", out=ob[:, :, :, 0:n_], in_=cin), reads=stg_k[i], writes=[ob_k[i]])
                P.dma(I("dma_start", out=dst[c0 // 128:c0 // 128 + ncb, :, k0:k0 + kb, :].rearrange("c p k n -> p c k n"), in_=ob[:, :, :, :]),
                      f"pcs{i}", reads=[ob_k[i]], writes=[f"dw{i}"])

    precast(w_in_e, bw_in_e, D, c.EIN); precast(w_out_e, bw_out_e, D, D)
    precast(w_in_o[:, 0:4 * D], bw_in_o, D, 4 * D); precast(w_out_o, bw_out_o, D, D)
    precast(w_gate, bw_gate, D, 2 * c.MH * 128); precast(glu_w, bglu, c.S5W, c.S5W)
    for l_ in range(2):
        precast(w_up[l_], bw_up[l_], D, c.DFF); precast(w_dn[l_], bw_dn[l_], c.DFF, D)

    NCT = c.NCT
    t0, t1, t2, t3, t4, t5 = (tA[:, i, 0:NCT] for i in range(6))
    sk = ("tiny", "s5p_s")
    act(t0, s5p_s[:, 2, :], AF.Exp, keys=sk)
    tt(t1, s5p_s[:, 0, :], t0, ALU.mult, keys=sk)
    tt(t2, s5p_s[:, 1, :], t0, ALU.mult, keys=sk)
    act(t3, t1, AF.Exp, scale=1.0 / 32.0)
    act(t4, t2, AF.Sin, scale=1.0 / 32.0)
    act(t5, t2, AF.Sin, scale=1.0 / 32.0, bias=math.pi / 2)
    lk = ("tiny", "lam")
    tt(lam[:, 0, 0, :], t3, t5, ALU.mult, keys=lk)
    tt(lam[:, 0, 1, :], t3, t4, ALU.mult, keys=lk)

    def csquare(dst_re, dst_im, a, b):
        tt(t0, a, a, ALU.mult, keys=lk); tt(t1, b, b, ALU.mult, keys=lk); tt(t2, a, b, ALU.mult, keys=lk)
        tt(dst_re, t0, t1, ALU.subtract, keys=lk)
        tsc(dst_im, t2, 2.0, ALU.mult, keys=lk)

    for _ in range(5):
        csquare(t3, t4, lam[:, 0, 0, :], lam[:, 0, 1, :])
        tsc(lam[:, 0, 0, :], t3, 1.0, ALU.mult, keys=lk); tsc(lam[:, 0, 1, :], t4, 1.0, ALU.mult, keys=lk)
    for k in range(1, 10):
        csquare(lam[:, k, 0, :], lam[:, k, 1, :], lam[:, k - 1, 0, :], lam[:, k - 1, 1, :])
    for k in range(10):
        tsc(lam[:, k, 2, :], lam[:, k, 1, :], -1.0, ALU.mult, keys=lk)
    ck = ("tiny", "lam", "cco", "s5p_s")
    ar_, ai_ = s5p_s[:, 0, :], s5p_s[:, 1, :]
    tsc(t0, lam[:, 0, 0, :], -1.0, ALU.add, keys=ck)
    tt(t1, ar_, ar_, ALU.mult, keys=ck); tt(t2, ai_, ai_, ALU.mult, keys=ck); tt(t1, t1, t2, ALU.add, keys=ck)
    P.op(V, I("reciprocal", out=t1, in_=t1), reads=ck, writes=ck)
    tt(t2, t0, ar_, ALU.mult, keys=ck); tt(t3, lam[:, 0, 1, :], ai_, ALU.mult, keys=ck); tt(t2, t2, t3, ALU.add, keys=ck)
    tt(cco[:, 0, :], t2, t1, ALU.mult, keys=ck)
    tt(t2, lam[:, 0, 1, :], ar_, ALU.mult, keys=ck); tt(t3, t0, ai_, ALU.mult, keys=ck); tt(t2, t2, t3, ALU.subtract, keys=ck)
    tt(cco[:, 1, :], t2, t1, ALU.mult, keys=ck)
    tsc(ncci[:, :], cco[:, 1, :], -1.0, ALU.mult, keys=("tiny", "cco", "ncci"))
    tt(t0, cco[:, 0, :], cco[:, 0, :], ALU.mult, keys=ck); tt(t1, cco[:, 1, :], cco[:, 1, :], ALU.mult, keys=ck)
    tt(t0, t0, t1, ALU.add, keys=ck)
    P.op(V, I("reciprocal", out=t0, in_=t0), reads=ck, writes=ck)
    ck2 = ("tiny", "cco", "cinv")
    tt(cinv[:, 0, :], cco[:, 0, :], t0, ALU.mult, keys=ck2)
    tt(t1, cco[:, 1, :], t0, ALU.mult, keys=ck2); tsc(cinv[:, 1, :], t1, -1.0, ALU.mult, keys=ck2)
    for ri in range(2):
        P.dma(I("dma_start", out=ws[ri][:, 0:c.SC * 2, 0:128], in_=bemb[ri]),
              f"w4_{ri}", writes=[f"w4_{ri}"])
        P.op(V, I("tensor_copy", out=bemb_s[:, ri, :, :], in_=ws[ri][:, 0:c.SC * 2, 0:128]),
             reads=[f"w4_{ri}"], writes=["bemb_s"])
    for ct0 in range(0, NCT, 16):
        nb = min(16, NCT - ct0)
        for ri in range(2):
            P.dma(I("dma_start", out=ws[ri][:, 0:nb, 0:128], in_=cemb[ri, :, ct0:ct0 + nb, :]),
                  f"w4_{ri}", writes=[f"w4_{ri}"])
        for j in range(nb):
            ct = ct0 + j
            kk = ["w4_0", "w4_1", "cco", "cemb_s", "tmpA"]
            P.op(V, I("tensor_scalar", out=tmpA[:, 0, 0:128], in0=ws[1][:, j, 0:128], scalar1=cco[:, 1, ct:ct + 1],
                                                           scalar2=None, op0=ALU.mult), reads=kk, writes=["tmpA"])
            P.op(V, I("scalar_tensor_tensor", out=cemb_s[:, 0, ct, :], in0=ws[0][:, j, 0:128], scalar=cco[:, 0, ct:ct + 1],
                                                                  in1=tmpA[:, 0, 0:128], op0=ALU.mult, op1=ALU.subtract), reads=kk, writes=["cemb_s"])
            P.op(V, I("tensor_scalar", out=tmpA[:, 1, 0:128], in0=ws[1][:, j, 0:128], scalar1=cco[:, 0, ct:ct + 1],
                                                           scalar2=None, op0=ALU.mult), reads=kk, writes=["tmpA"])
            P.op(V, I("scalar_tensor_tensor", out=cemb_s[:, 1, ct, :], in0=ws[0][:, j, 0:128], scalar=ncci[:, ct:ct + 1],
                                                                  in1=tmpA[:, 1, 0:128], op0=ALU.mult, op1=ALU.subtract), reads=kk + ["ncci"], writes=["cemb_s"])

    final_evs = []

    def out_dma(dst, src, skey, slot):
        if slot.startswith("fin"):
            slot = f"{slot}_{len(final_evs)}"
        ev = P.dma(I("dma_start", out=dst, in_=src), slot, reads=[skey])
        final_evs.append(ev)
        return ev

    def load_x(src, Tn):
        for g0 in range(0, Tn, 128):
            gs = min(128, Tn - g0)
            P.dma(I("dma_start", out=xtok[0:gs, :], in_=src[g0:g0 + gs, :]), "xtok", writes=["xtok"])
            for k4 in range(0, KD, 4):
                ps, pkey = pbank()
                for j in range(4):
                    k = k4 + j
                    P.op(PE, I("transpose",
                        out=ps[:, j * 128:j * 128 + gs], in_=xtok[0:gs, k * 128:(k + 1) * 128], identity=ident[0:gs, 0:gs]),
                        reads=["xtok", "ident"], writes=[pkey])
                for j in range(4):
                    k = k4 + j
                    eng = V if j % 2 == 0 else S
                    if eng == V:
                        P.op(V, I("tensor_copy", out=hT[:, k, g0:g0 + gs], in_=ps[:, j * 128:j * 128 + gs]),
                             reads=[pkey], writes=["hT"])
                    else:
                        P.op(S, I("copy", out=hT[:, k, g0:g0 + gs], in_=ps[:, j * 128:j * 128 + gs]),
                             reads=[pkey], writes=["hT"])

    def store_y(dst, Tn, norm=True):
        if norm:
            rmsnorm(4, Tn, dst=hT)
        for g0 in range(0, Tn, 128):
            gs = min(128, Tn - g0)
            for k4 in range(0, KD, 4):
                ps, pkey = pbank()
                for j in range(4):
                    k = k4 + j
                    P.op(PE, I("transpose",
                        out=ps[0:gs, j * 128:(j + 1) * 128], in_=hT[:, k, g0:g0 + gs], identity=ident[:, :]),
                        reads=["hT", "ident"], writes=[pkey])
                P.op(V, I("tensor_copy", out=xtok[0:gs, k4 * 128:(k4 + 4) * 128], in_=ps[0:gs, :]),
                     reads=[pkey], writes=["xtok"])
            out_dma(dst[g0:g0 + gs, :], xtok[0:gs, :], "xtok", "ytok")

    def resid_epi(Tn):
        def epi(j, ps, pkey, m):
            P.op(V, I("tensor_tensor", out=hT[:, j, 0:Tn], in0=hT[:, j, 0:Tn], in1=ps[:, 0:Tn], op=ALU.add),
                 reads=[pkey, "hT"], writes=["hT"])
        return epi

    def mlp(layer, Tn):
        rmsnorm(2 + layer, Tn)
        for hb in range(0, c.DFF, D):
            def epi_up(j, ps, pkey, m):
                P.op(S, I("activation", out=tmpA[:, 0, 0:Tn], in_=ps[:, 0:Tn], func=AF.Relu), reads=[pkey], writes=["tmpA"])
                P.op(V, I("tensor_tensor", out=hid[:, j, 0:Tn], in0=tmpA[:, 0, 0:Tn], in1=tmpA[:, 0, 0:Tn], op=ALU.mult),
                     reads=["tmpA"], writes=["yT"])
            dense_fm(bw_up[layer], KD, D, xnT, "xnT", Tn, epi_up, col0=hb)
            dense_fm(bw_dn[layer], KD, D, hid, "yT", Tn, resid_epi(Tn), rows0=hb)

    def s5(Tn, runs, is_sample, hook=None):
        nr = len(runs)
        L = runs[0][1]
        nsteps = int(math.log2(L))
        WAYS = 4

        def f32v(t):
            return t.bitcast(F32).rearrange("p a b -> p (a b)")[:, 0:2 * T].rearrange("p (c t) -> p c t", c=2)

        def bf16v(t):
            return t.bitcast(BF16).rearrange("p (c t) -> p c t", c=2)
        sets = [dict(A=scA, B=scB, kA="scA", kB="scB", tr=tmpA[:, 0, 0:Tn], ti=tmpA[:, 1, 0:Tn], kt="tmpA", zb=zbf, kz="zbf",
                     tn=tA[:, 0, 0:nr], ktn="tiny"),
                dict(A=scA2, B=scB2, kA="scA2", kB="scB2", tr=tmpA[:, 2, 0:Tn], ti=tmpA[:, 3, 0:Tn], kt="tmpA2", zb=zbf2, kz="zbf2",
                     tn=tA[:, 1, 0:nr], ktn="tiny"),
                dict(A=f32v(qTb[:]), B=f32v(kTb[:]), kA="qTb", kB="kTb", tr=emB[:, 0:Tn], ti=wpB[:, 0:Tn], kt="emB", zb=bf16v(gB[:]), kz="gB",
                     tn=tA[:, 2, 0:nr], ktn="tiny"),
                dict(A=f32v(qwp[:]), B=f32v(C_bf[:, 0:2, :].rearrange("p a b -> p (a b)").rearrange("p (a b) -> p a b", a=4)), kA="qwp", kB="C_bf",
                     tr=FgB[:, 0:Tn], ti=MB[:, 0:Tn], kt="FgB", zb=ogs[:, 0:2, :], kz="ogs",
                     tn=tA[:, 3, 0:nr], ktn="tiny")]
        def v3(ap):
            return ap.rearrange("p (r l) -> p r l", l=L)

        def body(ct, bs):
            cch, r = ct // 4, ct % 4
            A, B_, kA, kB = bs["A"], bs["B"], bs["kA"], bs["kB"]
            psr, kr = pbank(4, 8)
            psi, ki = pbank(4, 8)
            for ri, (ps_, k_) in enumerate(((psr, kr), (psi, ki))):
                P.op(PE, I("matmul", out=ps_[:, 0:Tn], lhsT=bemb_s[64 * (r // 2):64 * (r // 2) + 64, ri, cch * 2 + r % 2, :],
                           rhs=uT[64 * (r // 2):64 * (r // 2) + 64, cch, 0:Tn], start=True, stop=True),
                     reads=["bemb_s", "uT"], writes=[k_])
            P.op(S, I("copy", out=A[:, 0, 0:Tn], in_=psr[:, 0:Tn]), reads=[kr], writes=[kA])
            P.op(S, I("copy", out=A[:, 1, 0:Tn], in_=psi[:, 0:Tn]), reads=[ki], writes=[kA])
            yield
            lr, li = lam[:, 0, 0, ct:ct + 1], lam[:, 0, 1, ct:ct + 1]
            z0r, z0i = zst[:, 0, ct, 0:nr], zst[:, 1, ct, 0:nr]
            a0r = v3(A[:, 0, 0:Tn])[:, :, 0]
            a0i = v3(A[:, 1, 0:Tn])[:, :, 0]
            kk = [kA, "zst", "lam"]
            tn, ktn = bs["tn"], bs["ktn"]
            P.op(V, I("scalar_tensor_tensor", out=a0r, in0=z0r, scalar=lr, in1=a0r, op0=ALU.mult, op1=ALU.add), reads=kk, writes=[kA])
            P.op(V, I("scalar_tensor_tensor", out=a0i, in0=z0i, scalar=lr, in1=a0i, op0=ALU.mult, op1=ALU.add), reads=kk, writes=[kA])
            P.op(V, I("tensor_scalar", out=tn, in0=z0i, scalar1=li, scalar2=-1.0, op0=ALU.mult, op1=ALU.mult), reads=kk + [ktn], writes=[ktn])
            P.op(V, I("tensor_tensor", out=a0r, in0=a0r, in1=tn, op=ALU.add), reads=kk + [ktn], writes=[kA])
            P.op(V, I("scalar_tensor_tensor", out=a0i, in0=z0r, scalar=li, in1=a0i, op0=ALU.mult, op1=ALU.add), reads=kk, writes=[kA])
            yield
            src, dst, ks, kd_ = A, B_, kA, kB
            tr, ti, kt = v3(bs["tr"]), v3(bs["ti"]), bs["kt"]
            kts = {"emB": ["emB", "wpB"], "FgB": ["FgB", "MB"]}.get(kt, [kt])
            for st in range(nsteps):
                dlt = 1 << st
                pr, pi, npi = lam[:, st, 0, ct:ct + 1], lam[:, st, 1, ct:ct + 1], lam[:, st, 2, ct:ct + 1]
                sr = v3(src[:, 0, 0:Tn]); si = v3(src[:, 1, 0:Tn])
                dr = v3(dst[:, 0, 0:Tn]); di = v3(dst[:, 1, 0:Tn])
                kk2 = [ks, kd_, "lam"] + kts
                P.op(S, I("copy", out=dr[:, :, 0:dlt], in_=sr[:, :, 0:dlt]), reads=[ks], writes=[kd_])
                P.op(S, I("copy", out=di[:, :, 0:dlt], in_=si[:, :, 0:dlt]), reads=[ks], writes=[kd_])
                P.op(V, I("scalar_tensor_tensor", out=tr[:, :, dlt:L], in0=sr[:, :, 0:L - dlt], scalar=pr, in1=sr[:, :, dlt:L],
                           op0=ALU.mult, op1=ALU.add), reads=kk2, writes=kts)
                P.op(V, I("scalar_tensor_tensor", out=ti[:, :, dlt:L], in0=si[:, :, 0:L - dlt], scalar=pr, in1=si[:, :, dlt:L],
                           op0=ALU.mult, op1=ALU.add), reads=kk2, writes=kts)
                yield
                P.op(V, I("scalar_tensor_tensor", out=dr[:, :, dlt:L], in0=si[:, :, 0:L - dlt], scalar=npi, in1=tr[:, :, dlt:L],
                           op0=ALU.mult, op1=ALU.add), reads=kk2, writes=[kd_])
                P.op(V, I("scalar_tensor_tensor", out=di[:, :, dlt:L], in0=sr[:, :, 0:L - dlt], scalar=pi, in1=ti[:, :, dlt:L],
                           op0=ALU.mult, op1=ALU.add), reads=kk2, writes=[kd_])
                yield
                src, dst, ks, kd_ = dst, src, kd_, ks
            zl_r = v3(src[:, 0, 0:Tn])[:, :, L - 1]
            zl_i = v3(src[:, 1, 0:Tn])[:, :, L - 1]
            P.op(V, I("tensor_copy", out=zst[:, 0, ct, 0:nr], in_=zl_r), reads=[ks, "zst"], writes=["zst"])
            P.op(V, I("tensor_copy", out=zst[:, 1, ct, 0:nr], in_=zl_i), reads=[ks, "zst"], writes=["zst"])
            zb, kz = bs["zb"], bs["kz"]
            P.op(S, I("copy", out=zb[:, :, 0:Tn], in_=src[:, :, 0:Tn]), reads=[ks], writes=[kz])
            if r == 0:
                s5.ps, s5.pk = pbank(0, 4)
            psy, ky = s5.ps, s5.pk
            P.op(PE, I("matmul", out=psy[:, 0:Tn], lhsT=cemb_s[:, 0, ct, :], rhs=zb[:, 0, 0:Tn], start=(r == 0), stop=False),
                 reads=["cemb_s", kz], writes=[ky])
            P.op(PE, I("matmul", out=psy[:, 0:Tn], lhsT=cemb_s[:, 1, ct, :], rhs=zb[:, 1, 0:Tn], start=False, stop=(r == 3)),
                 reads=["cemb_s", kz], writes=[ky])
            if r == 3:
                a, b = tmpB[:, 0, 0:Tn], tmpB[:, 1, 0:Tn]
                P.op(V, I("scalar_tensor_tensor", out=a, in0=uT[:, cch, 0:Tn], scalar=s5d_s[:, 0, cch:cch + 1], in1=psy[:, 0:Tn],
                           op0=ALU.mult, op1=ALU.add), reads=[ky, "uT", "s5d_s"], writes=["tmpB"])
                P.op(V, I("tensor_tensor", out=b, in0=a, in1=a, op=ALU.mult), reads=["tmpB"], writes=["tmpB"])
                P.op(V, I("tensor_scalar", out=b, in0=b, scalar1=0.044715, scalar2=1.0, op0=ALU.mult, op1=ALU.add), reads=["tmpB"], writes=["tmpB"])
                P.op(V, I("tensor_tensor", out=b, in0=b, in1=a, op=ALU.mult), reads=["tmpB"], writes=["tmpB"])
                P.op(S, I("activation", out=b, in_=b, func=AF.Sigmoid, scale=1.5957691216057308), reads=["tmpB"], writes=["tmpB"])
                P.op(V, I("tensor_tensor", out=ygl[:, cch, 0:Tn], in0=a, in1=b, op=ALU.mult), reads=["tmpB"], writes=["ygl"])
            yield

        for ct0 in range(0, NCT, WAYS):
            gens = [body(ct0 + w, sets[w]) for w in range(min(WAYS, NCT - ct0))]
            alive = list(gens)
            while alive:
                for g in list(alive):
                    try:
                        next(g)
                    except StopIteration:
                        alive.remove(g)
            if hook is not None and (ct0 + WAYS) % 4 == 0:
                hook((ct0 + WAYS) // 4 - 1)
        def epi_glu(j, ps, pkey, m):
            P.op(S, I("activation", out=tmpB[:, 0, 0:Tn], in_=ps[:, 0:Tn], func=AF.Sigmoid, bias=s5d_s[:, 1, j:j + 1]),
                 reads=[pkey, "s5d_s"], writes=["tmpB"])
            P.op(V, I("tensor_tensor", out=yT[:, j, 0:Tn], in0=tmpB[:, 0, 0:Tn], in1=ygl[:, j, 0:Tn], op=ALU.mult),
                 reads=["tmpB", "ygl"], writes=["yT"])
        dense_fm(bglu, c.SC, c.S5W, ygl, "ygl", Tn, epi_glu)
    s5.ps = None

    def tok_proj(w, col0, ncols, Tn, g0, gs, dst, dkey, dcol0, scale=None):
        assert col0 % 128 == 0 and ncols % 256 == 0
        for cb in range(0, ncols, 256):
            wt, wkey = wtile(w, (col0 + cb) // 128, 2, 0, KD)
            ps, pkey = pbank()
            for k in range(KD):
                P.op(PE, I("matmul", out=ps[0:gs, 0:256].rearrange("p (c n) -> p c n", c=2), lhsT=xnT[:, k, g0:g0 + gs], rhs=wt[:, :, k, :],
                           start=(k == 0), stop=(k == KD - 1)), reads=[wkey, "xnT"], writes=[pkey])
            if scale is None:
                P.op(S, I("copy", out=dst[0:gs, dcol0 + cb:dcol0 + cb + 256], in_=ps[0:gs, 0:256]), reads=[pkey], writes=[dkey])
            else:
                P.op(S, I("activation", out=dst[0:gs, dcol0 + cb:dcol0 + cb + 256], in_=ps[0:gs, 0:256], func=AF.Copy, scale=scale),
                     reads=[pkey], writes=[dkey])

    def gla(Tn, groups, is_sample):
        o1 = c.S5W; o2 = o1 + c.GDK; o3 = o2 + c.GDK; o4 = o3 + c.GDV; o5 = o4 + c.GDV
        wt, wkey = wtile(bw_in_e, o5 // 128, 1, 0, KD)
        ps, pkey = pbank()
        for k in range(KD):
            P.op(PE, I("matmul", out=ps[0:16, 0:Tn], lhsT=wt[:, 0, k, 0:16], rhs=xnT[:, k, 0:Tn], start=(k == 0), stop=(k == KD - 1)),
                 reads=[wkey, "xnT"], writes=[pkey])
        P.op(V, I("tensor_copy", out=gkl[:, 0:Tn], in_=ps[0:16, 0:Tn]), reads=[pkey], writes=["gkl"])
        yield
        for h in range(c.GH):
            ps, pkey = pbank(4, 8)
            P.op(PE, I("matmul", out=ps[:, 0:Tn], lhsT=gku[:, h * 128:(h + 1) * 128], rhs=gkl[:, 0:Tn], start=True, stop=True),
                 reads=["gku", "gkl"], writes=[pkey])
            P.op(S, I("activation", out=Ep[:, 0:Tn], in_=ps[:, 0:Tn], func=AF.Exp, scale=-1.0, bias=ngkb[:, h:h + 1]), reads=[pkey, "ngkb"], writes=["Ep"])
            P.op(S, I("activation", out=Ep[:, 0:Tn], in_=Ep[:, 0:Tn], func=AF.Ln, bias=1.0), reads=["Ep"], writes=["Ep"])
            P.op(V, I("tensor_scalar", out=Ep[:, 0:Tn], in0=Ep[:, 0:Tn], scalar1=-1.0 / 16.0, scalar2=None, op0=ALU.mult), reads=["Ep"], writes=["Ep"])
            for (g0, gs, chunks) in groups:
                for (c0, cl, sq) in chunks:
                    P.op(V, I("tensor_tensor_scan", out=Em[:, c0:c0 + cl], data0=ones_f[:, 0:cl], data1=Ep[:, c0:c0 + cl],
                                                                          initial=0.0, op0=ALU.mult, op1=ALU.add), reads=["Ep", "ones_f"], writes=["Em"])
            P.op(S, I("activation", out=Ep[:, 0:Tn], in_=Em[:, 0:Tn], func=AF.Exp), reads=["Em"], writes=["Ep"])
            P.op(S, I("activation", out=Em[:, 0:Tn], in_=Em[:, 0:Tn], func=AF.Exp, scale=-1.0), reads=["Em", "Ep"], writes=["Em"])
            def epi_q(j, ps, pkey, m):
                P.op(V, I("scalar_tensor_tensor", out=qd[:, 0:Tn], in0=ps[:, 0:Tn], scalar=float(c.HK) ** -0.5, in1=Ep[:, 0:Tn], op0=ALU.mult, op1=ALU.mult),
                     reads=[pkey, "Ep"], writes=["qd"])
            dense_fm(bw_in_e, KD, 128, xnT, "xnT", Tn, epi_q, col0=o1 + h * 128)
            def epi_k(j, ps, pkey, m):
                P.op(V, I("tensor_tensor", out=kd[:, 0:Tn], in0=ps[:, 0:Tn], in1=Em[:, 0:Tn], op=ALU.mult), reads=[pkey, "Em"], writes=["kd"])
            dense_fm(bw_in_e, KD, 128, xnT, "xnT", Tn, epi_k, col0=o2 + h * 128)
            def epi_g(j, ps, pkey, m):
                P.op(S, I("activation", out=gsil[:, j, 0:Tn], in_=ps[:, 0:Tn], func=AF.Silu), reads=[pkey], writes=["gsil"])
            dense_fm(bw_in_e, KD, 256, xnT, "xnT", Tn, epi_g, col0=o4 + h * 256)
            for (g0, gs, chunks) in groups:
                for (c0, cl, sq) in chunks:
                    P.op(V, I("tensor_scalar", out=kdc[:, c0:c0 + cl], in0=kd[:, c0:c0 + cl], scalar1=Ep[:, c0 + cl - 1:c0 + cl],
                                                                    scalar2=None, op0=ALU.mult), reads=["kd", "Ep"], writes=["kdc"])
            for (g0, gs, chunks) in groups:
                msk = tri if not is_sample else blk
                tok_proj(bw_in_e, o3 + h * 256, 256, Tn, g0, gs, vtok, "vtok", 0)
                ps, pkey = pbank(4, 8)
                P.op(PE, I("matmul", out=ps[0:gs, 0:gs], lhsT=kd[:, g0:g0 + gs], rhs=qd[:, g0:g0 + gs], start=True, stop=True),
                     reads=["kd", "qd"], writes=[pkey])
                P.op(V, I("tensor_tensor", out=attT[0:gs, 0:gs], in0=ps[0:gs, 0:gs], in1=msk[0:gs, 0:gs], op=ALU.mult),
                     reads=[pkey, "tri", "blk"], writes=["attT"])
                ps2, pkey2 = pbank(4, 8)
                P.op(PE, I("matmul", out=ps2[0:gs, 0:128], lhsT=kdc[:, g0:g0 + gs], rhs=ones_id[:, :], start=True, stop=True),
                     reads=["kdc", "ones_id"], writes=[pkey2])
                P.op(S, I("copy", out=ktok[0:gs, 0:128], in_=ps2[0:gs, 0:128]), reads=[pkey2], writes=["ktok"])
                pso = [pbank(4, 8) for _ in range(2)]
                for ec in range(2):
                    P.op(PE, I("matmul", out=pso[ec][0][:, 0:gs], lhsT=vtok[0:gs, ec * 128:(ec + 1) * 128], rhs=attT[0:gs, 0:gs], start=True, stop=False),
                         reads=["vtok", "attT"], writes=[pso[ec][1]])
                GSL = c.GH
                GPF = max(1, min(2, GSL - 1))

                def load_S(ci_):
                    sq_ = chunks[ci_][2]
                    sl_ = ci_ % GSL
                    P.dma(I("dma_start", out=S_sb[:, sl_, :], in_=st_gla[sq_, h]), f"S_ld{sl_}", writes=[f"S_sb{sl_}"])

                if is_sample:
                    for ci_ in range(min(GPF, len(chunks))):
                        load_S(ci_)
                for ci, (c0, cl, sq) in enumerate(chunks):
                    sl = ci % GSL if is_sample else h
                    ksl = f"S_sb{sl}"
                    if is_sample and ci + GPF < len(chunks):
                        load_S(ci + GPF)
                    P.op(S, I("copy", out=S_bf[:, :], in_=S_sb[:, sl, :]), reads=[ksl], writes=["S_bf"])
                    last = ci == len(chunks) - 1
                    for ec in range(2):
                        P.op(PE, I("matmul", out=pso[ec][0][:, c0 - g0:c0 - g0 + cl], lhsT=S_bf[:, ec * 128:(ec + 1) * 128],
                                   rhs=qd[:, c0:c0 + cl], start=False, stop=last),
                             reads=["S_bf", "qd"], writes=[pso[ec][1]])
                    if len(chunks) > 1:
                        P.op(V, I("tensor_scalar", out=kwt[0:gs, 0:128], in0=ktok[0:gs, 0:128], scalar1=blk[0:gs, c0 - g0 + cl - 1:c0 - g0 + cl],
                                   scalar2=None, op0=ALU.mult), reads=["ktok", "blk"], writes=["kwt"])
                        lk_, lkey = kwt, "kwt"
                    else:
                        lk_, lkey = ktok, "ktok"
                    psS, kS = pbank(0, 4)
                    P.op(PE, I("matmul", out=psS[:, 0:256], lhsT=lk_[0:gs, 0:128], rhs=vtok[0:gs, 0:256], start=True, stop=True),
                         reads=[lkey, "vtok"], writes=[kS])
                    P.op(V, I("scalar_tensor_tensor", out=S_sb[:, sl, :], in0=S_sb[:, sl, :], scalar=Ep[:, c0 + cl - 1:c0 + cl],
                               in1=psS[:, 0:256], op0=ALU.mult, op1=ALU.add), reads=[kS, ksl, "S_bf", "Ep"], writes=[ksl])
                    if is_sample:
                        out_dma(o_sgla[sq, h], S_sb[:, sl, :], ksl, f"S_st{sl}")
                for ec in range(2):
                    P.op(V, I("tensor_copy", out=oT[:, ec, g0:g0 + gs], in_=pso[ec][0][:, 0:gs]), reads=[pso[ec][1]], writes=["oT"])
            for ec in range(2):
                P.op(S, I("activation", out=qwp[:, ec, 0:Tn], in_=oT[:, ec, 0:Tn], func=AF.Square), reads=["oT"], writes=["qwp"])
            ps, pkey = pbank()
            for ec in range(2):
                P.op(PE, I("matmul", out=ps[:, 0:Tn], lhsT=ones_bf[:, :], rhs=qwp[:, ec, 0:Tn], start=(ec == 0), stop=(ec == 1)),
                     reads=["qwp", "ones_bf"], writes=[pkey])
            P.op(S, I("activation", out=rstdB[:, 0:Tn], in_=ps[:, 0:Tn], func=AF.Sqrt, scale=1.0 / c.HV, bias=EPS), reads=[pkey], writes=["rstdB"])
            P.op(V, I("reciprocal", out=rstdB[:, 0:Tn], in_=rstdB[:, 0:Tn]), reads=["rstdB"], writes=["rstdB"])
            for ec in range(2):
                P.op(V, I("scalar_tensor_tensor", out=oT[:, ec, 0:Tn], in0=oT[:, ec, 0:Tn], scalar=gnorm_s[:, ec:ec + 1], in1=rstdB[:, 0:Tn],
                                                                op0=ALU.mult, op1=ALU.mult), reads=["oT", "rstdB", "gnorm_s"], writes=["oT"])
                P.op(V, I("tensor_tensor", out=yT[:, c.SC + h * 2 + ec, 0:Tn], in0=oT[:, ec, 0:Tn], in1=gsil[:, ec, 0:Tn], op=ALU.mult),
                     reads=["oT", "gsil"], writes=["yT"])
            yield

    ones_id = P.sb("ones_id", [128, 128], BF16)
    P.op(V, I("tensor_copy", out=ones_id[:, :], in_=ident[:, :]), reads=["ident"], writes=["ones_id"])

    def mlstm(Tn, groups, runs, is_sample):
        for h in range(c.MH):
            bi, nbf = mb_s[:, h:h + 1], nmb[:, c.MH + h:c.MH + h + 1]
            def epi_i(j, ps, pkey, m):
                P.op(V, I("tensor_scalar", out=gB[:, 0:Tn], in0=ps[:, 0:Tn], scalar1=bi, scalar2=None, op0=ALU.add), reads=[pkey, "mb_s"], writes=["gB"])
            dense_fm(bw_gate, KD, 128, xnT, "xnT", Tn, epi_i, col0=h * 128)
            def epi_f(j, ps, pkey, m):
                P.op(S, I("activation", out=FgB[:, 0:Tn], in_=ps[:, 0:Tn], func=AF.Exp, scale=-1.0, bias=nbf), reads=[pkey, "nmb"], writes=["FgB"])
                P.op(S, I("activation", out=FgB[:, 0:Tn], in_=FgB[:, 0:Tn], func=AF.Ln, bias=1.0), reads=["FgB"], writes=["FgB"])
                P.op(V, I("tensor_scalar", out=wpB[:, 0:Tn], in0=FgB[:, 0:Tn], scalar1=-1.0, scalar2=None, op0=ALU.mult), reads=["FgB"], writes=["wpB"])
            dense_fm(bw_gate, KD, 128, xnT, "xnT", Tn, epi_f, col0=(c.MH + h) * 128)
            for ri, (r0, rl) in enumerate(runs):
                if is_sample:
                    fin = 0.0
                    min_ = mm0[:, ri * c.MH + h:ri * c.MH + h + 1]
                else:
                    fin = Fcar[:, h:h + 1]
                    min_ = Mcar[:, h:h + 1]
                P.op(V, I("tensor_tensor_scan", out=FgB[:, r0:r0 + rl], data0=ones_f[:, 0:rl], data1=wpB[:, r0:r0 + rl],
                                                                              initial=fin, op0=ALU.mult, op1=ALU.add), reads=["wpB", "ones_f", "Fcar", "FgB"], writes=["FgB"])
                P.op(V, I("tensor_tensor", out=gB[:, r0:r0 + rl], in0=gB[:, r0:r0 + rl], in1=FgB[:, r0:r0 + rl], op=ALU.subtract),
                     reads=["gB", "FgB"], writes=["gB"])
                P.op(V, I("tensor_tensor_scan", out=MB[:, r0:r0 + rl], data0=ones_f[:, 0:rl], data1=gB[:, r0:r0 + rl],
                                                                                initial=min_, op0=ALU.mult, op1=ALU.max), reads=["gB", "ones_f", "Mcar", "mm0", "MB"], writes=["MB"])
            P.op(V, I("tensor_tensor", out=emB[:, 0:Tn], in0=FgB[:, 0:Tn], in1=MB[:, 0:Tn], op=ALU.add), reads=["FgB", "MB"], writes=["emB"])
            for ri, (r0, rl) in enumerate(runs):
                if is_sample:
                    P.op(V, I("tensor_copy", out=mout[0:1, h * NS + ri:h * NS + ri + 1], in_=emB[0:1, r0 + rl - 1:r0 + rl]),
                         reads=["emB"], writes=["mout"])
                else:
                    P.op(V, I("tensor_copy", out=mout[0:1, h:h + 1], in_=emB[0:1, r0 + rl - 1:r0 + rl]), reads=["emB"], writes=["mout"])
            P.op(S, I("activation", out=emB[:, 0:Tn], in_=emB[:, 0:Tn], func=AF.Exp, scale=-1.0), reads=["emB", "mout"], writes=["emB"])
            for (g0, gs, chunks) in groups:
                for (c0, cl, sq) in chunks:
                    first = any(c0 == r0 for (r0, rl) in runs)
                    if first:
                        ri = [i for i, (r0, rl) in enumerate(runs) if r0 == c0][0]
                        mp = mm0[:, ri * c.MH + h:ri * c.MH + h + 1] if is_sample else Mcar[:, h:h + 1]
                    else:
                        mp = MB[:, c0 - 1:c0]
                    P.op(S, I("activation", out=wpB[:, c0:c0 + cl], in_=MB[:, c0:c0 + cl], func=AF.Exp, scale=-1.0, bias=mp),
                         reads=["MB", "Mcar", "mm0", "wpB", "FgB"], writes=["wpB"])
            def epi_q(j, ps, pkey, m):
                P.op(S, I("copy", out=qTb[:, j, 0:Tn], in_=ps[:, 0:Tn]), reads=[pkey], writes=["qTb"])
                P.op(V, I("tensor_tensor", out=qwp[:, j, 0:Tn], in0=ps[:, 0:Tn], in1=wpB[:, 0:Tn], op=ALU.mult), reads=[pkey, "wpB"], writes=["qwp"])
            dense_fm(bw_in_o, KD, 512, xnT, "xnT", Tn, epi_q, col0=h * 512)
            def epi_k(j, ps, pkey, m):
                P.op(S, I("activation", out=kTb[:, j, 0:Tn], in_=ps[:, 0:Tn], func=AF.Copy, scale=float(c.DH) ** -0.5), reads=[pkey], writes=["kTb"])
            dense_fm(bw_in_o, KD, 512, xnT, "xnT", Tn, epi_k, col0=D + h * 512)
            def epi_og(j, ps, pkey, m):
                P.op(S, I("activation", out=ogs[:, j, 0:Tn], in_=ps[:, 0:Tn], func=AF.Sigmoid), reads=[pkey], writes=["ogs"])
            dense_fm(bw_in_o, KD, 512, xnT, "xnT", Tn, epi_og, col0=3 * D + h * 512)
            if not is_sample:
                P.op(S, I("copy", out=C_bf[:, :, :], in_=C_sb[:, h, :, :]), reads=[f"C_sb{h}"], writes=["C_bf"])
            for (g0, gs, chunks) in groups:
                msk = tri if not is_sample else blk
                tok_proj(bw_in_o, D + h * 512, 512, Tn, g0, gs, ktok, "ktok", 0, scale=float(c.DH) ** -0.5)
                tok_proj(bw_in_o, 2 * D + h * 512, 512, Tn, g0, gs, vtok, "vtok", 0)
                ps, pkey = pbank(5, 8)
                P.op(PE, I("transpose", out=ps[0:gs, 0:128], in_=gB[:, g0:g0 + gs], identity=ident[:, :]), reads=["gB", "ident"], writes=[pkey])
                P.op(V, I("tensor_copy", out=gcol[0:gs, 0:1], in_=ps[0:gs, 0:1]), reads=[pkey], writes=["gcol"])
                P.op(S, I("activation", out=wT[0:gs, 0:gs], in_=MB[0:gs, g0:g0 + gs], func=AF.Exp, scale=-1.0, bias=gcol[0:gs, 0:1]),
                     reads=["MB", "gcol"], writes=["wT"])
                P.op(V, I("tensor_tensor", out=wTm[0:gs, 0:gs], in0=wT[0:gs, 0:gs], in1=msk[0:gs, 0:gs], op=ALU.mult), reads=["wT", "tri", "blk"], writes=["wTm"])
                ps, pkey = pbank(5, 8)
                for dc in range(4):
                    P.op(PE, I("matmul", out=ps[0:gs, 0:gs], lhsT=kTb[:, dc, g0:g0 + gs], rhs=qTb[:, dc, g0:g0 + gs], start=(dc == 0), stop=(dc == 3)),
                         reads=["kTb", "qTb"], writes=[pkey])
                P.op(V, I("tensor_tensor", out=attT[0:gs, 0:gs], in0=ps[0:gs, 0:gs], in1=wTm[0:gs, 0:gs], op=ALU.mult), reads=[pkey, "wTm"], writes=["attT"])
                psn = [(pb[i], f"pb{i}") for i in range(4)]
                psd, kdn = pb[4], "pb4"
                for ec in range(4):
                    P.op(PE, I("matmul", out=psn[ec][0][:, 0:gs], lhsT=vtok[0:gs, ec * 128:(ec + 1) * 128], rhs=attT[0:gs, 0:gs], start=True, stop=False),
                         reads=["vtok", "attT"], writes=[psn[ec][1]])
                P.op(PE, I("matmul", out=psd[:, 0:gs], lhsT=ones_bf[0:gs, :], rhs=attT[0:gs, 0:gs], start=True, stop=False), reads=["ones_bf", "attT"], writes=[kdn])
                NSL = c.MH

                def update_state(ci, c0, cl, sq, sl):
                    lc = c0 - g0 + cl - 1
                    P.op(V, I("tensor_scalar", out=kwt[0:gs, 0:512], in0=ktok[0:gs, 0:512], scalar1=wTm[0:gs, lc:lc + 1], scalar2=None, op0=ALU.mult),
                         reads=["ktok", "wTm"], writes=["kwt"])
                    dcol = wpB[:, c0 + cl - 1:c0 + cl]
                    for dc in range(4):
                        psC, kC = pbank(5, 8)
                        P.op(PE, I("matmul", out=psC[:, 0:512], lhsT=kwt[0:gs, dc * 128:(dc + 1) * 128], rhs=vtok[0:gs, 0:512], start=True, stop=True),
                             reads=["kwt", "vtok"], writes=[kC])
                        P.op(V, I("scalar_tensor_tensor", out=C_sb[:, sl, dc, :], in0=C_sb[:, sl, dc, :], scalar=dcol, in1=psC[:, 0:512],
                                   op0=ALU.mult, op1=ALU.add), reads=[kC, f"C_sb{sl}", "C_bf", "wpB"], writes=[f"C_sb{sl}"])
                        psN, kN = pbank(5, 8)
                        P.op(PE, I("matmul", out=psN[:, 0:2], lhsT=kwt[0:gs, dc * 128:(dc + 1) * 128], rhs=ones_bf[0:gs, 0:2], start=True, stop=True),
                             reads=["kwt", "ones_bf"], writes=[kN])
                        P.op(V, I("scalar_tensor_tensor", out=n_sb[:, sl, dc:dc + 1], in0=n_sb[:, sl, dc:dc + 1], scalar=dcol, in1=psN[:, 0:1],
                                   op0=ALU.mult, op1=ALU.add), reads=[kN, f"n_sb{sl}", "Nrep", "wpB"], writes=[f"n_sb{sl}"])
                    if is_sample:
                        out_dma(o_smc[sq, h].rearrange("(dc p) e -> p dc e", p=128), C_sb[:, sl, :, :], f"C_sb{sl}", f"C_st{sl}")
                        out_dma(o_smn[sq, h], n_sb[:, sl, :], f"n_sb{sl}", f"n_st{sl}")
                    else:
                        P.op(S, I("copy", out=C_bf[:, :, :], in_=C_sb[:, sl, :, :]), reads=[f"C_sb{sl}"], writes=["C_bf"])

                def load_state(ci):
                    sq_ = chunks[ci][2]
                    sl_ = ci % NSL
                    P.dma(I("dma_start", out=C_sb[:, sl_, :, :], in_=st_mc[sq_, h].rearrange("(dc p) e -> p dc e", p=128)), f"C_ld{sl_}", writes=[f"C_sb{sl_}"])
                    P.dma(I("dma_start", out=n_sb[:, sl_, :], in_=st_mn[sq_, h]), f"n_ld{sl_}", writes=[f"n_sb{sl_}"])

                PF = max(1, min(2, NSL - 1))
                if is_sample:
                    for ci_ in range(min(PF, len(chunks))):
                        load_state(ci_)
                for ci, (c0, cl, sq) in enumerate(chunks):
                    last = ci == len(chunks) - 1
                    sl = ci % NSL if is_sample else h
                    if is_sample:
                        if ci + PF < len(chunks):
                            load_state(ci + PF)
                        P.op(S, I("copy", out=C_bf[:, :, :], in_=C_sb[:, sl, :, :]), reads=[f"C_sb{sl}"], writes=["C_bf"])
                    for dc in range(4):
                        P.op(V, I("tensor_scalar", out=Nrep[:, dc, :], in0=ones_bf[:, :], scalar1=n_sb[:, sl, dc:dc + 1], scalar2=None, op0=ALU.mult),
                             reads=["ones_bf", f"n_sb{sl}"], writes=["Nrep"])
                    for dc in range(4):
                        lst = last and dc == 3
                        for ec in range(4):
                            P.op(PE, I("matmul", out=psn[ec][0][:, c0 - g0:c0 - g0 + cl], lhsT=C_bf[:, dc, ec * 128:(ec + 1) * 128],
                                                                                         rhs=qwp[:, dc, c0:c0 + cl], start=False, stop=lst), reads=["C_bf", "qwp"], writes=[psn[ec][1]])
                        P.op(PE, I("matmul", out=psd[:, c0 - g0:c0 - g0 + cl], lhsT=Nrep[:, dc, :], rhs=qwp[:, dc, c0:c0 + cl], start=False, stop=lst),
                             reads=["Nrep", "qwp"], writes=[kdn])
                    if is_sample:
                        update_state(ci, c0, cl, sq, sl)
                P.op(S, I("activation", out=rden[:, 0:gs], in_=psd[:, 0:gs], func=AF.Abs), reads=[kdn], writes=["rden"])
                P.op(V, I("tensor_tensor", out=rden[:, 0:gs], in0=rden[:, 0:gs], in1=emB[:, g0:g0 + gs], op=ALU.max), reads=["rden", "emB"], writes=["rden"])
                P.op(V, I("reciprocal", out=rden[:, 0:gs], in_=rden[:, 0:gs]), reads=["rden"], writes=["rden"])
                for ec in range(4):
                    P.op(V, I("tensor_tensor", out=oT[:, ec, g0:g0 + gs], in0=psn[ec][0][:, 0:gs], in1=rden[:, 0:gs], op=ALU.mult), reads=[psn[ec][1], "rden"], writes=["oT"])
                    P.op(V, I("tensor_tensor", out=oT[:, ec, g0:g0 + gs], in0=oT[:, ec, g0:g0 + gs], in1=ogs[:, ec, g0:g0 + gs], op=ALU.mult), reads=["oT", "ogs"], writes=["oT"])
                if not is_sample:
                    for ci, (c0, cl, sq) in enumerate(chunks):
                        update_state(ci, c0, cl, sq, h)
            if not is_sample:
                r0, rl = runs[-1]
                P.op(V, I("tensor_copy", out=Fcar[:, h:h + 1], in_=FgB[:, r0 + rl - 1:r0 + rl]), reads=["FgB"], writes=["Fcar"])
                P.op(V, I("tensor_copy", out=Mcar[:, h:h + 1], in_=MB[:, r0 + rl - 1:r0 + rl]), reads=["MB"], writes=["Mcar"])
            for ec in range(4):
                P.op(S, I("activation", out=uT[:, ec, 0:Tn], in_=oT[:, ec, 0:Tn], func=AF.Square), reads=["oT"], writes=["uT"])
            ps, pkey = pbank()
            for ec in range(4):
                P.op(PE, I("matmul", out=ps[:, 0:Tn], lhsT=ones_bf[:, :], rhs=uT[:, ec, 0:Tn], start=(ec == 0), stop=(ec == 3)), reads=["uT", "ones_bf"], writes=[pkey])
            P.op(S, I("activation", out=rstdB[:, 0:Tn], in_=ps[:, 0:Tn], func=AF.Sqrt, scale=1.0 / c.DH, bias=EPS), reads=[pkey], writes=["rstdB"])
            P.op(V, I("reciprocal", out=rstdB[:, 0:Tn], in_=rstdB[:, 0:Tn]), reads=["rstdB"], writes=["rstdB"])
            for ec in range(4):
                P.op(V, I("scalar_tensor_tensor", out=yT[:, h * 4 + ec, 0:Tn], in0=oT[:, ec, 0:Tn], scalar=mnorm_s[:, ec:ec + 1], in1=rstdB[:, 0:Tn],
                                                                op0=ALU.mult, op1=ALU.mult), reads=["oT", "rstdB", "mnorm_s"], writes=["yT"])

    def segment(src, dst, Tn, groups, runs, is_sample):
        load_x(src, Tn)
        rmsnorm(0, Tn)
        def epi_u(j, ps, pkey, m):
            P.op(S, I("copy", out=uT[:, j, 0:Tn], in_=ps[:, 0:Tn]), reads=[pkey], writes=["uT"])
        dense_fm(bw_in_e, KD, c.S5W, xnT, "xnT", Tn, epi_u)
        gg = gla(Tn, groups, is_sample)
        next(gg)

        def hook(chunk):
            if chunk % 2 == 1:
                next(gg, None)
        s5(Tn, runs, is_sample, hook=hook)
        for _ in gg:
            pass
        dense_fm(bw_out_e, KD, D, yT, "yT", Tn, resid_epi(Tn))
        if getattr(c, "dbg", 0) == 1:
            store_y(dst, Tn, norm=False)
            return
        mlp(0, Tn)
        rmsnorm(1, Tn)
        mlstm(Tn, groups, runs, is_sample)
        dense_fm(bw_out_o, KD, D, yT, "yT", Tn, resid_epi(Tn))
        mlp(1, Tn)
        store_y(dst, Tn)

    P.op(V, I("memset", zst[:], 0.0), writes=["zst"])
    P.op(V, I("memset", S_sb[:], 0.0), writes=[f"S_sb{i}" for i in range(c.GH)])
    P.op(V, I("memset", C_sb[:], 0.0), writes=[f"C_sb{i}" for i in range(c.MH)])
    P.op(V, I("memset", n_sb[:], 0.0), writes=[f"n_sb{i}" for i in range(c.MH)])
    P.op(V, I("memset", Fcar[:], 0.0), writes=["Fcar"])
    P.op(V, I("memset", mout[:], 0.0), writes=["mout"])
    P.op(V, I("memset", Mcar[:], NEG), writes=["Mcar"])
    for s0 in range(0, c.SEQ, T):
        groups = [(g0, 128, [(g0, 128, 0)]) for g0 in range(0, T, 128)]
        segment(xp[s0:s0 + T, :], yp[s0:s0 + T, :], T, groups, [(0, T)], False)
    zk = ["zst", "cco", "tiny"]
    zr, zi = zst[:, 0, :, 0], zst[:, 1, :, 0]
    tt(t0, zr, cco[:, 0, :], ALU.mult, keys=zk); tt(t1, zi, cco[:, 1, :], ALU.mult, keys=zk); tt(t0, t0, t1, ALU.subtract, keys=zk)
    tt(t2, zr, cco[:, 1, :], ALU.mult, keys=zk); tt(t3, zi, cco[:, 0, :], ALU.mult, keys=zk); tt(t2, t2, t3, ALU.add, keys=zk)
    out_dma(o_ps5[0], t0, "tiny", "fin"); out_dma(o_ps5[1], t2, "tiny", "fin")
    for h in range(c.GH):
        out_dma(o_pgla[h], S_sb[:, h, :], f"S_sb{h}", "fin")
    for h in range(c.MH):
        out_dma(o_pmc[h].rearrange("(dc p) e -> p dc e", p=128), C_sb[:, h, :, :], f"C_sb{h}", "fin")
        out_dma(o_pmn[h], n_sb[:, h, :], f"n_sb{h}", "fin")
    out_dma(o_pmm, mout[0:1, 0:c.MH], "mout", "fin")

    if NS > 0:
        TS = c.TS
        P.dma(I("dma_start", out=zst[:, 0, :, :], in_=st_s5[0]), "zld", reads=["tiny"], writes=["zst"])
        P.dma(I("dma_start", out=zst[:, 1, :, :], in_=st_s5[1]), "zld", reads=["tiny"], writes=["zst"])
        P.seal("zld")
        W_ = NCT * NS
        nsl = (W_ + T - 1) // T
        assert 6 * nsl <= KD
        big = [hT[:, nsl * i:nsl * i + nsl, :].rearrange("p a b -> p (a b)")[:, 0:W_].rearrange("p (a b) -> p a b", b=NS) for i in range(6)]
        cr_b = cinv[:, 0, :].unsqueeze(2).broadcast_to([128, NCT, NS]); ci_b = cinv[:, 1, :].unsqueeze(2).broadcast_to([128, NCT, NS])
        zk2 = ["zst", "cinv", "tiny", "hT"]
        tt(big[0], zst[:, 0, :, :], cr_b, ALU.mult, keys=zk2); tt(big[1], zst[:, 1, :, :], ci_b, ALU.mult, keys=zk2)
        tt(big[2], zst[:, 0, :, :], ci_b, ALU.mult, keys=zk2); tt(big[3], zst[:, 1, :, :], cr_b, ALU.mult, keys=zk2)
        tt(zst[:, 0, :, :], big[0], big[1], ALU.subtract, keys=zk2); tt(zst[:, 1, :, :], big[2], big[3], ALU.add, keys=zk2)
        chunks = [(4 * s, 4, s) for s in range(NS)]
        segment(xs, ys, TS, [(0, TS, chunks)], [(4 * s, 4) for s in range(NS)], True)
        cr_b = cco[:, 0, :].unsqueeze(2).broadcast_to([128, NCT, NS]); ci_b = cco[:, 1, :].unsqueeze(2).broadcast_to([128, NCT, NS])
        zk3 = ["zst", "cco", "tiny", "hT"]
        tt(big[0], zst[:, 0, :, :], cr_b, ALU.mult, keys=zk3); tt(big[1], zst[:, 1, :, :], ci_b, ALU.mult, keys=zk3)
        tt(big[2], zst[:, 0, :, :], ci_b, ALU.mult, keys=zk3); tt(big[3], zst[:, 1, :, :], cr_b, ALU.mult, keys=zk3)
        tt(big[4], big[0], big[1], ALU.subtract, keys=zk3); tt(big[5], big[2], big[3], ALU.add, keys=zk3)
        out_dma(o_ss5[0], big[4], "hT", "fin2"); out_dma(o_ss5[1], big[5], "hT", "fin2")
        out_dma(o_smm, mout[0:1, 0:c.MH * NS], "mout", "fin2")

    fw = {}
    for ev in final_evs:
        fw[ev[1]] = max(fw.get(ev[1], 0), ev[2])
    P.emit(final_waits=[("dma", s, k) for s, k in fw.items()])
    nc._stats = P.stats
    return nc


def _lay_vec(v, n):
    return np.ascontiguousarray(v.reshape(n, 128).T)


def make_inputs(cfg, core, inp):
    c = cfg
    f = np.float32
    b = core % inp["x_prompt"].shape[0]
    NS = c.NS
    s0 = core * NS
    m = {}
    m["xp"] = np.ascontiguousarray(inp["x_prompt"][b], dtype=f)
    m["xs"] = np.ascontiguousarray(inp["x_sample"][s0:s0 + NS].reshape(NS * 4, c.D), dtype=f)

    def s5lay(a):
        return a.reshape(NS, c.NCT, 2, 64).transpose(2, 3, 1, 0).reshape(128, c.NCT, NS)
    m["st_s5"] = np.ascontiguousarray(np.stack([s5lay(inp["state_s5_re"][0, s0:s0 + NS]), s5lay(inp["state_s5_im"][0, s0:s0 + NS])]), dtype=f)
    m["st_gla"] = np.ascontiguousarray(inp["state_gla"][0, s0:s0 + NS], dtype=f)
    m["st_mc"] = np.ascontiguousarray(inp["state_mlstm_c"][0, s0:s0 + NS], dtype=f)
    m["st_mn"] = np.ascontiguousarray(inp["state_mlstm_n"][0, s0:s0 + NS].reshape(NS, c.MH, 4, 128).transpose(0, 1, 3, 2), dtype=f)
    m["st_mm"] = np.ascontiguousarray(inp["state_mlstm_m"][0, s0:s0 + NS].reshape(1, NS * c.MH), dtype=f)
    return m


def make_shared(cfg, inp):
    c = cfg
    f = np.float32
    m = {}
    nv = [inp["norm_mix"][0], inp["norm_mix"][1], inp["norm_mlp"][0], inp["norm_mlp"][1], inp["norm_final"]]
    m["nrm"] = np.ascontiguousarray(np.stack([_lay_vec(v, c.KD) for v in nv], axis=1), dtype=f)
    m["w_in_e"] = np.ascontiguousarray(inp["w_in_even"][0], dtype=f)
    m["w_out_e"] = np.ascontiguousarray(inp["w_out_even"][0], dtype=f)
    m["w_in_o"] = np.ascontiguousarray(inp["w_in_odd"][0], dtype=f)
    m["w_out_o"] = np.ascontiguousarray(inp["w_out_odd"][0], dtype=f)
    m["w_gate"] = np.ascontiguousarray(np.repeat(inp["w_in_odd"][0][:, 4 * c.D:], 128, axis=1), dtype=f)
    m["w_up"] = np.ascontiguousarray(inp["w_mlp_up"], dtype=f)
    m["w_dn"] = np.ascontiguousarray(inp["w_mlp_down"], dtype=f)

    def gl(a):
        return a.reshape(c.NCT, 2, 64).transpose(1, 2, 0).reshape(128, c.NCT)
    ls = np.repeat(inp["s5_log_step"][0][:, None], 64, axis=1)
    m["s5p"] = np.ascontiguousarray(np.stack([gl(inp["s5_a_re"][0]), gl(inp["s5_a_im"][0]), gl(ls)], axis=1), dtype=f)
    bem = np.zeros((2, 128, c.SC, 2, 128), f)
    cem = np.zeros((2, 128, c.NCT, 128), f)
    for ri, (bsrc, csrc) in enumerate(((inp["s5_b_re"][0], inp["s5_c_re"][0]), (inp["s5_b_im"][0], inp["s5_c_im"][0]))):
        bg = bsrc.reshape(c.SC, 4, 2, 64, 16)
        cg = csrc.reshape(c.SC, 4, 2, 16, 64)
        for g2 in range(2):
            blk_ = bg[:, :, g2].transpose(1, 3, 0, 2)
            for r in range(4):
                bem[ri].reshape(4, 2, 16, c.SC, 2, 2, 64)[r, g2, :, :, r % 2, g2, :] = blk_[r]
            cb_ = cg[:, :, g2]
            for r in range(4):
                cem[ri].reshape(2, 64, c.SC, 4, 128)[g2, :, :, r, r * 32 + g2 * 16:r * 32 + g2 * 16 + 16] = cb_[:, r].transpose(2, 0, 1)
    m["bemb"] = np.ascontiguousarray(bem.reshape(2, 128, c.SC * 2, 128))
    m["cemb"] = cem
    m["s5d"] = np.ascontiguousarray(np.stack([_lay_vec(inp["s5_d"][0], c.SC), _lay_vec(inp["s5_glu_b"][0], c.SC)], axis=1), dtype=f)
    m["glu_w"] = np.ascontiguousarray(inp["s5_glu_w"][0], dtype=f)
    m["gk_up"] = np.ascontiguousarray(inp["gla_gk_up"][0], dtype=f)
    m["gkb"] = _lay_vec(inp["gla_gk_b"][0], c.GH).astype(f)
    m["gnorm"] = _lay_vec(inp["gla_norm"][0], 2).astype(f)
    m["mb"] = np.ascontiguousarray(np.concatenate([inp["mlstm_b_i"][0], inp["mlstm_b_f"][0]]).reshape(1, 2 * c.MH), dtype=f)
    m["mnorm"] = _lay_vec(inp["mlstm_norm"][0], 4).astype(f)
    m["ident"] = np.eye(128, dtype=f)
    m["tri"] = np.triu(np.ones((128, 128), f))
    i = np.arange(128)
    m["blk"] = ((i[:, None] // 4 == i[None, :] // 4) & (i[:, None] <= i[None, :])).astype(f)
    return m


def assemble(cfg, results, n_prompt, n_cores):
    c = cfg
    NS = c.NS
    yp = np.stack([results[b]["yp"] for b in range(n_prompt)])
    ys = np.concatenate([results[k]["ys"].reshape(NS, 4, c.D) for k in range(n_cores)], axis=0)

    def unl(a):
        return a.reshape(2, 64, c.NCT).transpose(2, 0, 1).reshape(c.NG, 64)
    ps5 = [np.stack([unl(results[b]["o_ps5"][ri]) for b in range(n_prompt)])[None] for ri in range(2)]
    pgla = np.stack([results[b]["o_pgla"] for b in range(n_prompt)])[None]
    pmc = np.stack([results[b]["o_pmc"] for b in range(n_prompt)])[None]
    pmn = np.stack([results[b]["o_pmn"].transpose(0, 2, 1).reshape(c.MH, 512) for b in range(n_prompt)])[None]
    pmm = np.stack([results[b]["o_pmm"].reshape(c.MH) for b in range(n_prompt)])[None]

    def unls(a):
        return a.reshape(2, 64, c.NCT, NS).transpose(3, 2, 0, 1).reshape(NS, c.NG, 64)
    ss5 = [np.concatenate([unls(results[k]["o_ss5"][ri]) for k in range(n_cores)])[None] for ri in range(2)]
    sgla = np.concatenate([results[k]["o_sgla"] for k in range(n_cores)])[None]
    smc = np.concatenate([results[k]["o_smc"] for k in range(n_cores)])[None]
    smn = np.concatenate([results[k]["o_smn"].transpose(0, 1, 3, 2).reshape(NS, c.MH, 512) for k in range(n_cores)])[None]
    smm = np.concatenate([results[k]["o_smm"].reshape(c.MH, NS).T for k in range(n_cores)])[None]
    outs = (yp, ys, ps5[0], ps5[1], pgla, pmc, pmn, pmm, ss5[0], ss5[1], sgla, smc, smn, smm)
    return tuple(np.ascontiguousarray(o, dtype=np.float32) for o in outs)


def kernel(**inputs):
    n_cores = 8
    cfg = Cfg()
    inp = {k: np.asarray(v) for k, v in inputs.items()}
    nc = build(cfg)
    shared = make_shared(cfg, inp)
    in_maps = []
    for k in range(n_cores):
        m = dict(shared)
        m.update(make_inputs(cfg, k, inp))
        in_maps.append(m)
    res = run_bass_kernel_spmd(nc, in_maps, core_ids=list(range(n_cores)))
    return assemble(cfg, res.results, inp["x_prompt"].shape[0], n_cores)
```

```python
import contextlib
import math
import numpy as np
import concourse.bass as bass
import concourse.mybir as mybir
from concourse.bass_utils import run_bass_kernel_spmd

F32 = mybir.dt.float32
BF16 = mybir.dt.bfloat16
AF = mybir.ActivationFunctionType
ALU = mybir.AluOpType
EPS = 1e-6
NEG = -1.0e30

CH = 16000
COMPUTE = ("tensor", "vector", "scalar", "gpsimd")
ENGS = ("tensor", "vector", "scalar", "gpsimd", "sync")


class Prog:
    def __init__(self, nc):
        self.nc = nc
        self.ops = {e: [] for e in ENGS}
        self.last_w = {}
        self.readers = {}
        self.seen = {e: {} for e in ENGS}
        self.dma_cnt = {}
        self.stack = contextlib.ExitStack()

    def sb(self, name, shape, dt):
        return self.stack.enter_context(self.nc.sbuf_tensor(name, list(shape), dt))

    def ps(self, name, shape, dt):
        return self.stack.enter_context(self.nc.psum_tensor(name, list(shape), dt))

    def _deps(self, eng, reads, writes):
        deps = []
        for r in reads:
            ev = self.last_w.get(r)
            if ev is not None:
                deps.append(ev)
            if r.startswith("pb"):
                deps.extend(o for o in self.readers.get(r, ()) if o[1] != eng)
        for w in writes:
            ev = self.last_w.get(w)
            if ev is not None:
                deps.append(ev)
            deps.extend(self.readers.get(w, ()))
        best = {}
        for (kind, who, idx) in deps:
            if kind == "eng" and who == "tensor" and eng == "tensor":
                continue
            key = (kind, who)
            if best.get(key, -1) < idx:
                best[key] = idx
        out = []
        seen = self.seen[eng]
        for key, idx in best.items():
            if seen.get(key, -1) >= idx:
                continue
            seen[key] = idx
            out.append((key[0], key[1], idx))
        return out

    def _commit(self, ev, reads, writes):
        for w in writes:
            self.last_w[w] = ev
            self.readers[w] = []
        for r in reads:
            if r not in writes:
                lst = self.readers.setdefault(r, [])
                for i, o in enumerate(lst):
                    if o[0] == ev[0] and o[1] == ev[1]:
                        lst[i] = ev
                        break
                else:
                    lst.append(ev)

    def op(self, eng, fn, reads=(), writes=()):
        reads = list(reads)
        writes = list(writes)
        deps = self._deps(eng, reads, writes)
        ev = ("eng", eng, len(self.ops[eng]))
        self.ops[eng].append(dict(fn=fn, deps=deps, ev=ev, dma=None))
        self._commit(ev, reads, writes)
        return ev

    def dma(self, fn, slot, reads=(), writes=(), queue="sync"):
        reads = list(reads)
        writes = list(writes)
        deps = self._deps(queue, reads, writes)
        k = self.dma_cnt.get(slot, 0) + 1
        self.dma_cnt[slot] = k
        ev = ("dma", slot, k)
        self.ops[queue].append(dict(fn=fn, deps=deps, ev=None, dma=(slot, k)))
        self._commit(ev, reads, writes)
        return ev

    def seal(self, slot):
        k = self.dma_cnt.get(slot, 0)
        for key, ev in list(self.last_w.items()):
            if ev[0] == "dma" and ev[1] == slot:
                self.last_w[key] = ("dma", slot, k)

    def emit(self, final_waits=()):
        nc = self.nc
        waited = {e: set() for e in COMPUTE}
        for e in ENGS:
            for o in self.ops[e]:
                for (kind, who, idx) in o["deps"]:
                    if kind == "eng":
                        waited[who].add(idx)
        rank = {}
        for e in COMPUTE:
            for r, idx in enumerate(sorted(waited[e])):
                rank[(e, idx)] = r
        sems = {}
        for e in COMPUTE:
            for j in range((len(waited[e]) + CH - 1) // CH):
                sems[(e, j)] = self.stack.enter_context(nc.semaphore(f"s_{e}_{j}"))
        DCH = 1000
        dsems = {}
        for slot, tot in self.dma_cnt.items():
            for j in range((tot + DCH - 1) // DCH):
                dsems[(slot, j)] = self.stack.enter_context(nc.semaphore(f"d_{slot}_{j}"))
        self.stats = dict(nsem=len(sems) + len(dsems), nops={e: len(self.ops[e]) for e in ENGS},
                          waited={e: len(waited[e]) for e in COMPUTE})

        def wait(engobj, ev):
            kind, who, idx = ev
            if kind == "eng":
                r = rank[(who, idx)]
                engobj.wait_ge(sems[(who, r // CH)], r % CH + 1)
            else:
                engobj.wait_ge(dsems[(who, (idx - 1) // DCH)], 16 * ((idx - 1) % DCH + 1))

        block = self.stack.enter_context(nc.Block())

        def make(e):
            def body(engobj):
                for o in self.ops[e]:
                    for ev in o["deps"]:
                        wait(engobj, ev)
                    name, a, kw = o["fn"]
                    inst = getattr(engobj, name)(*a, **kw)
                    if o["dma"] is not None:
                        inst.then_inc(dsems[(o["dma"][0], (o["dma"][1] - 1) // DCH)], 16)
                    elif (e, o["ev"][2]) in rank:
                        r = rank[(e, o["ev"][2])]
                        inst.then_inc(sems[(e, r // CH)], 1)
                if e == "sync":
                    for ev in final_waits:
                        wait(engobj, ev)
            return body

        block.tensor(make("tensor"))
        block.vector(make("vector"))
        block.scalar(make("scalar"))
        block.gpsimd(make("gpsimd"))
        block.sync(make("sync"))
        self.stack.close()


def I(name, *a, **kw):
    return (name, a, kw)


class Cfg:
    def __init__(self, D=2048, GH=4, MH=4, SEQ=2048, NS=16, T=256):
        self.D, self.GH, self.MH, self.SEQ, self.NS, self.T = D, GH, MH, SEQ, NS, T
        self.KD = D // 128
        self.S5W = D // 2
        self.NG = self.S5W // 16
        self.NCT = self.NG // 2
        self.SC = self.S5W // 128
        self.GDV = D - self.S5W
        self.GDK = self.GDV // 2
        self.HK = self.GDK // GH
        self.HV = self.GDV // GH
        assert self.HK == 128 and self.HV == 256
        self.EIN = self.S5W + 2 * self.GDK + 2 * self.GDV + 16
        self.DH = D // MH
        assert self.DH == 512
        self.OIN = 4 * D + 2 * MH
        self.DFF = 4 * D
        self.DS = 4
        self.TS = NS * self.DS


WCOLS = 256


def build(cfg):
    c = cfg
    D, KD, T, NS = c.D, c.KD, c.T, c.NS
    nc = bass.Bass("TRN2", target_bir_lowering=False)
    P = Prog(nc)

    def din(name, shape):
        return nc.dram_tensor(name, list(shape), F32, kind="ExternalInput").ap()

    def dout(name, shape):
        return nc.dram_tensor(name, list(shape), F32, kind="ExternalOutput").ap()

    xp = din("xp", [c.SEQ, D]); xs = din("xs", [c.TS, D])
    st_s5 = din("st_s5", [2, 128, c.NCT, NS])
    st_gla = din("st_gla", [NS, c.GH, 128, 256])
    st_mc = din("st_mc", [NS, c.MH, 512, 512])
    st_mn = din("st_mn", [NS, c.MH, 128, 4])
    st_mm = din("st_mm", [1, NS * c.MH])
    nrm = din("nrm", [128, 5, KD])
    w_in_e = din("w_in_e", [D, c.EIN]); w_out_e = din("w_out_e", [D, D])
    w_in_o = din("w_in_o", [D, c.OIN]); w_out_o = din("w_out_o", [D, D])
    w_gate = din("w_gate", [D, 2 * c.MH * 128])
    w_up = din("w_up", [2, D, c.DFF]); w_dn = din("w_dn", [2, c.DFF, D])
    s5p = din("s5p", [128, 3, c.NCT])
    bemb = din("bemb", [2, 128, c.SC * 2, 128]); cemb = din("cemb", [2, 128, c.NCT, 128])
    s5d = din("s5d", [128, 2, c.SC])
    glu_w = din("glu_w", [c.S5W, c.S5W])
    gk_up = din("gk_up", [16, c.GDK]); gkb = din("gkb", [128, c.GH]); gnorm = din("gnorm", [128, 2])
    mb = din("mb", [1, 2 * c.MH]); mnorm = din("mnorm", [128, 4])
    ident_d = din("ident", [128, 128]); tri_d = din("tri", [128, 128]); blk_d = din("blk", [128, 128])

    yp = dout("yp", [c.SEQ, D]); ys = dout("ys", [c.TS, D])
    o_ps5 = dout("o_ps5", [2, 128, c.NCT]); o_ss5 = dout("o_ss5", [2, 128, c.NCT, NS])
    o_pgla = dout("o_pgla", [c.GH, 128, 256]); o_sgla = dout("o_sgla", [NS, c.GH, 128, 256])
    o_pmc = dout("o_pmc", [c.MH, 512, 512]); o_smc = dout("o_smc", [NS, c.MH, 512, 512])
    o_pmn = dout("o_pmn", [c.MH, 128, 4]); o_smn = dout("o_smn", [NS, c.MH, 128, 4])
    o_pmm = dout("o_pmm", [1, c.MH]); o_smm = dout("o_smm", [1, c.MH * NS])

    hT = P.sb("hT", [128, KD, T], F32)
    xnT = P.sb("xnT", [128, KD, T], BF16)
    yT = P.sb("yT", [128, KD, T], BF16)
    hid = yT
    xtok = P.sb("xtok", [128, D], F32)
    W4all = P.sb("w4all", [128, 4, 16, 256], BF16)
    W4 = [W4all[:, i] for i in range(4)]
    ws = [W4[i].bitcast(F32) for i in range(2)]
    W4K = ["w4_0", "w4_1", "w4_2", "w4_3"]
    rstdB = P.sb("rstdB", [128, T], F32)
    nrm_s = P.sb("nrm_s", [128, 5, KD], F32)
    ident = P.sb("ident_s", [128, 128], F32)
    tri = P.sb("tri_s", [128, 128], F32); blk = P.sb("blk_s", [128, 128], F32)
    ones_bf = P.sb("ones_bf", [128, 128], BF16)
    ones_f = P.sb("ones_f", [128, T], F32)
    s5p_s = P.sb("s5p_s", [128, 3, c.NCT], F32)
    s5d_s = P.sb("s5d_s", [128, 2, c.SC], F32)
    lam = P.sb("lam", [128, 10, 3, c.NCT], F32)
    cco = P.sb("cco", [128, 2, c.NCT], F32)
    cinv = P.sb("cinv", [128, 2, c.NCT], F32)
    ncci = P.sb("ncci", [128, c.NCT], F32)
    tA = P.sb("tA", [128, 6, c.NCT], F32)
    bemb_s = P.sb("bemb_s", [128, 2, c.SC * 2, 128], BF16)
    cemb_s = P.sb("cemb_s", [128, 2, c.NCT, 128], BF16)
    zst = P.sb("zst", [128, 2, c.NCT, NS], F32)
    scA = P.sb("scA", [128, 2, T], F32); scB = P.sb("scB", [128, 2, T], F32)
    zbf = P.sb("zbf", [128, 2, T], BF16)
    scA2 = P.sb("scA2", [128, 2, T], F32); scB2 = P.sb("scB2", [128, 2, T], F32); zbf2 = P.sb("zbf2", [128, 2, T], BF16)
    uT = P.sb("uT", [128, c.SC, T], BF16)
    ygl = P.sb("ygl", [128, c.SC, T], BF16)
    tmpA = P.sb("tmpA", [128, 4, T], F32)
    tmpB = P.sb("tmpB", [128, 2, T], F32)
    gkl = P.sb("gkl", [16, T], F32); gku = P.sb("gku", [16, c.GDK], F32)
    gkb_s = P.sb("gkb_s", [128, c.GH], F32); ngkb = P.sb("ngkb", [128, c.GH], F32)
    gnorm_s = P.sb("gnorm_s", [128, 2], F32)
    S_sb = P.sb("S_sb", [128, c.GH, 256], F32); S_bf = P.sb("S_bf", [128, 256], BF16)
    cmask = P.sb("cmask", [128, T], F32)
    qd = P.sb("qd", [128, T], BF16); kd = P.sb("kd", [128, T], BF16); kdc = P.sb("kdc", [128, T], BF16)
    Ep = P.sb("Ep", [128, T], F32); Em = P.sb("Em", [128, T], F32)
    vtok = P.sb("vtok", [128, 512], BF16); ktok = P.sb("ktok", [128, 512], BF16)
    kwt = P.sb("kwt", [128, 512], BF16)
    attT = P.sb("attT", [128, 128], BF16)
    gsil = P.sb("gsil", [128, 2, T], F32)
    oT = P.sb("oT", [128, 4, T], F32)
    mb_s = P.sb("mb_s", [128, 2 * c.MH], F32); nmb = P.sb("nmb", [128, 2 * c.MH], F32)
    mnorm_s = P.sb("mnorm_s", [128, 4], F32)
    mm0 = P.sb("mm0", [128, NS * c.MH], F32)
    C_sb = P.sb("C_sb", [128, c.MH, 4, 512], F32); C_bf = P.sb("C_bf", [128, 4, 512], BF16)
    n_sb = P.sb("n_sb", [128, c.MH, 4], F32); Nrep = P.sb("Nrep", [128, 4, 128], BF16)
    Fcar = P.sb("Fcar", [128, c.MH], F32); Mcar = P.sb("Mcar", [128, c.MH], F32)
    FgB = P.sb("FgB", [128, T], F32); MB = P.sb("MB", [128, T], F32); gB = P.sb("gB", [128, T], F32)
    wpB = P.sb("wpB", [128, T], F32); emB = P.sb("emB", [128, T], F32)
    gcol = P.sb("gcol", [128, 1], F32)
    wT = P.sb("wT", [128, 128], F32); wTm = P.sb("wTm", [128, 128], F32)
    qTb = P.sb("qTb", [128, 4, T], BF16); kTb = P.sb("kTb", [128, 4, T], BF16); qwp = P.sb("qwp", [128, 4, T], BF16)
    ogs = P.sb("ogs", [128, 4, T], BF16)
    rden = P.sb("rden", [128, 128], F32)
    mout = P.sb("mout", [1, c.MH * max(NS, 1)], F32)
    pb = [P.ps(f"pb{i}", [128, 512], F32) for i in range(8)]

    V, S, G, PE = "vector", "scalar", "gpsimd", "tensor"
    cnt = {"w": 0, "pd": 0}

    def ld(dst, src, key):
        P.dma(I("dma_start", out=dst, in_=src), "setup", writes=[key])

    ld(nrm_s[:], nrm, "nrm_s"); ld(ident[:], ident_d, "ident"); ld(tri[:], tri_d, "tri"); ld(blk[:], blk_d, "blk")
    ld(s5p_s[:], s5p, "s5p_s"); ld(s5d_s[:], s5d, "s5d_s")
    ld(gku[:], gk_up, "gku"); ld(gkb_s[:], gkb, "gkb_s"); ld(gnorm_s[:], gnorm, "gnorm_s")
    ld(mb_s[:], mb.broadcast_to([128, 2 * c.MH]), "mb_s"); ld(mnorm_s[:], mnorm, "mnorm_s")
    ld(mm0[:], st_mm.broadcast_to([128, NS * c.MH]), "mm0")
    P.seal("setup")
    P.op(V, I("memset", ones_bf[:], 1.0), writes=["ones_bf"])
    P.op(V, I("memset", ones_f[:], 1.0), writes=["ones_f"])
    P.op(V, I("tensor_scalar", out=ngkb[:], in0=gkb_s[:], scalar1=-1.0, scalar2=None, op0=ALU.mult),
         reads=["gkb_s"], writes=["ngkb"])
    P.op(V, I("tensor_scalar", out=nmb[:], in0=mb_s[:], scalar1=-1.0, scalar2=None, op0=ALU.mult),
         reads=["mb_s"], writes=["nmb"])

    def tt(out, a, b, op, keys=("tiny",)):
        P.op(V, I("tensor_tensor", out=out, in0=a, in1=b, op=op), reads=keys, writes=keys)

    def tsc(out, a, s1, op0, s2=None, op1=None, keys=("tiny",)):
        if op1 is None:
            P.op(V, I("tensor_scalar", out=out, in0=a, scalar1=s1, scalar2=None, op0=op0), reads=keys, writes=keys)
        else:
            P.op(V, I("tensor_scalar", out=out, in0=a, scalar1=s1, scalar2=s2, op0=op0, op1=op1), reads=keys, writes=keys)

    def act(out, a, func, keys=("tiny",), **kw):
        P.op(S, I("activation", out=out, in_=a, func=func, **kw), reads=keys, writes=keys)

    def wtile(scrt, cb0, ncb, k0, kc):
        i = cnt["w"] % 4
        cnt["w"] += 1
        view = W4[i].rearrange("p a b -> p (a b)")[:, 0:ncb * kc * 128].rearrange("p (c k n) -> p c k n", c=ncb, k=kc)
        P.dma(I("dma_start", out=view, in_=scrt[cb0:cb0 + ncb, :, k0:k0 + kc, :].rearrange("c p k n -> p c k n")),
              f"w4_{i}", reads=["dw0", "dw1"], writes=[f"w4_{i}"])
        return view, f"w4_{i}"

    def pbank(lo=0, hi=4):
        i = lo + cnt["pd"] % (hi - lo)
        cnt["pd"] += 1
        return pb[i], f"pb{i}"

    def dense_fm(w, kin, ncols_total, inT, inkey, Tn, epi, rows0=0, col0=0):
        assert col0 % 128 == 0 and rows0 % 128 == 0 and ncols_total % 128 == 0
        nb_tot = ncols_total // 128
        for b0 in range(0, nb_tot, 2):
            ncb = min(2, nb_tot - b0)
            wt, wkey = wtile(w, col0 // 128 + b0, ncb, rows0 // 128, kin)
            for sub in range(ncb):
                ps, pkey = pbank()
                for k in range(kin):
                    P.op(PE, I("matmul", out=ps[:, 0:Tn], lhsT=wt[:, sub, k, :], rhs=inT[:, k, 0:Tn],
                               start=(k == 0), stop=(k == kin - 1)), reads=[wkey, inkey], writes=[pkey])
                epi(b0 + sub, ps, pkey, 128)

    def rmsnorm(idx, Tn, dst=None):
        for k in range(KD):
            P.op(S, I("activation", out=xnT[:, k, 0:Tn], in_=hT[:, k, 0:Tn], func=AF.Square),
                 reads=["hT"], writes=["xnT"])
        ps, pkey = pbank()
        for k in range(KD):
            P.op(PE, I("matmul", out=ps[:, 0:Tn], lhsT=ones_bf[:, :], rhs=xnT[:, k, 0:Tn],
                                            start=(k == 0), stop=(k == KD - 1)),
                 reads=["xnT", "ones_bf"], writes=[pkey])
        P.op(S, I("activation", out=rstdB[:, 0:Tn], in_=ps[:, 0:Tn], func=AF.Sqrt, scale=1.0 / D, bias=EPS),
             reads=[pkey], writes=["rstdB"])
        P.op(V, I("reciprocal", out=rstdB[:, 0:Tn], in_=rstdB[:, 0:Tn]), reads=["rstdB"], writes=["rstdB"])
        for k in range(KD):
            o = xnT[:, k, 0:Tn] if dst is None else dst[:, k, 0:Tn]
            P.op(V, I("scalar_tensor_tensor",
                out=o, in0=hT[:, k, 0:Tn], scalar=nrm_s[:, idx, k:k + 1], in1=rstdB[:, 0:Tn],
                op0=ALU.mult, op1=ALU.mult), reads=["hT", "rstdB", "nrm_s"], writes=["xnT" if dst is None else "hT"])

    def scr(name, rows, cols):
        return nc.dram_tensor(name, [(cols + 127) // 128, 128, rows // 128, 128], BF16, kind="Internal").ap()

    bw_in_e = scr("bw_in_e", D, c.EIN); bw_out_e = scr("bw_out_e", D, D)
    bw_in_o = scr("bw_in_o", D, 4 * D); bw_out_o = scr("bw_out_o", D, D)
    bw_gate = scr("bw_gate", D, 2 * c.MH * 128)
    bw_up = [scr(f"bw_up{l_}", D, c.DFF) for l_ in range(2)]; bw_dn = [scr(f"bw_dn{l_}", c.DFF, D) for l_ in range(2)]
    bglu = scr("bglu", c.S5W, c.S5W)
    cnt["pc"] = 0
    stg_f = [C_sb[:].rearrange("p a b c -> p (a b c)"), W4all[:].bitcast(F32).rearrange("p a b c -> p (a b c)")]
    stg_k = [[f"C_sb{i}" for i in range(c.MH)], W4K]
    ob_b = [hT[:].bitcast(BF16).rearrange("p a b -> p (a b)"), cemb_s[:].rearrange("p a b c -> p (a b c)")]
    ob_k = ["hT", "cemb_s"]
    PCW = 2048
    kb_max = min(4, c.MH * 4 * 512 // PCW, 8192 // PCW, KD * T * 2 // PCW, 2 * c.NCT * 128 // PCW)
    assert kb_max >= 1

    def precast(src, dst, rows, cols):
        KC = rows // 128
        for c0 in range(0, cols, PCW):
            ncol = min(PCW, cols - c0)
            ncb = (ncol + 127) // 128
            n_ = min(128, ncol)
            assert ncol % 128 == 0 or ncb == 1
            for k0 in range(0, KC, kb_max):
                kb = min(kb_max, KC - k0)
                i = cnt["pc"] % 2
                cnt["pc"] += 1
                stg = stg_f[i][:, 0:kb * PCW].rearrange("p (k n) -> p k n", k=kb)
                ob = ob_b[i][:, 0:ncb * kb * 128].rearrange("p (c k n) -> p c k n", c=ncb, k=kb)
                P.dma(I("dma_start", out=stg[:, :, 0:ncol], in_=src[k0 * 128:(k0 + kb) * 128, c0:c0 + ncol].rearrange("(k p) n -> p k n", p=128)),
                      f"pcl{i}", writes=stg_k[i])
                cin = stg[:, :, 0:ncb * n_].rearrange("p k (c n) -> p c k n", c=ncb)
                if n_ < 128:
                    P.op(V, I("memset", ob[:, :, :, :], 0.0), reads=[ob_k[i]], writes=[ob_k[i]])
                if i == 0:
                    P.op(V, I("tensor_copy", out=ob[:, :, :, 0:n_], in_=cin), reads=stg_k[i], writes=[ob_k[i]])
                else:
                    P.op(S, I("copy", out=ob[:, :, :, 0:n_], in_=cin), reads=stg_k[i], writes=[ob_k[i]])
                P.dma(I("dma_start", out=dst[c0 // 128:c0 // 128 + ncb, :, k0:k0 + kb, :].rearrange("c p k n -> p c k n"), in_=ob[:, :, :, :]),
                      f"pcs{i}", reads=[ob_k[i]], writes=[f"dw{i}"])

    precast(w_in_e, bw_in_e, D, c.EIN); precast(w_out_e, bw_out_e, D, D)
    precast(w_in_o[:, 0:4 * D], bw_in_o, D, 4 * D); precast(w_out_o, bw_out_o, D, D)
    precast(w_gate, bw_gate, D, 2 * c.MH * 128); precast(glu_w, bglu, c.S5W, c.S5W)
    for l_ in range(2):
        precast(w_up[l_], bw_up[l_], D, c.DFF); precast(w_dn[l_], bw_dn[l_], c.DFF, D)

    NCT = c.NCT
    t0, t1, t2, t3, t4, t5 = (tA[:, i, 0:NCT] for i in range(6))
    sk = ("tiny", "s5p_s")
    act(t0, s5p_s[:, 2, :], AF.Exp, keys=sk)
    tt(t1, s5p_s[:, 0, :], t0, ALU.mult, keys=sk)
    tt(t2, s5p_s[:, 1, :], t0, ALU.mult, keys=sk)
    act(t3, t1, AF.Exp, scale=1.0 / 32.0)
    act(t4, t2, AF.Sin, scale=1.0 / 32.0)
    act(t5, t2, AF.Sin, scale=1.0 / 32.0, bias=math.pi / 2)
    lk = ("tiny", "lam")
    tt(lam[:, 0, 0, :], t3, t5, ALU.mult, keys=lk)
    tt(lam[:, 0, 1, :], t3, t4, ALU.mult, keys=lk)

    def csquare(dst_re, dst_im, a, b):
        tt(t0, a, a, ALU.mult, keys=lk); tt(t1, b, b, ALU.mult, keys=lk); tt(t2, a, b, ALU.mult, keys=lk)
        tt(dst_re, t0, t1, ALU.subtract, keys=lk)
        tsc(dst_im, t2, 2.0, ALU.mult, keys=lk)

    for _ in range(5):
        csquare(t3, t4, lam[:, 0, 0, :], lam[:, 0, 1, :])
        tsc(lam[:, 0, 0, :], t3, 1.0, ALU.mult, keys=lk); tsc(lam[:, 0, 1, :], t4, 1.0, ALU.mult, keys=lk)
    for k in range(1, 10):
        csquare(lam[:, k, 0, :], lam[:, k, 1, :], lam[:, k - 1, 0, :], lam[:, k - 1, 1, :])
    for k in range(10):
        tsc(lam[:, k, 2, :], lam[:, k, 1, :], -1.0, ALU.mult, keys=lk)
    ck = ("tiny", "lam", "cco", "s5p_s")
    ar_, ai_ = s5p_s[:, 0, :], s5p_s[:, 1, :]
    tsc(t0, lam[:, 0, 0, :], -1.0, ALU.add, keys=ck)
    tt(t1, ar_, ar_, ALU.mult, keys=ck); tt(t2, ai_, ai_, ALU.mult, keys=ck); tt(t1, t1, t2, ALU.add, keys=ck)
    P.op(V, I("reciprocal", out=t1, in_=t1), reads=ck, writes=ck)
    tt(t2, t0, ar_, ALU.mult, keys=ck); tt(t3, lam[:, 0, 1, :], ai_, ALU.mult, keys=ck); tt(t2, t2, t3, ALU.add, keys=ck)
    tt(cco[:, 0, :], t2, t1, ALU.mult, keys=ck)
    tt(t2, lam[:, 0, 1, :], ar_, ALU.mult, keys=ck); tt(t3, t0, ai_, ALU.mult, keys=ck); tt(t2, t2, t3, ALU.subtract, keys=ck)
    tt(cco[:, 1, :], t2, t1, ALU.mult, keys=ck)
    tsc(ncci[:, :], cco[:, 1, :], -1.0, ALU.mult, keys=("tiny", "cco", "ncci"))
    tt(t0, cco[:, 0, :], cco[:, 0, :], ALU.mult, keys=ck); tt(t1, cco[:, 1, :], cco[:, 1, :], ALU.mult, keys=ck)
    tt(t0, t0, t1, ALU.add, keys=ck)
    P.op(V, I("reciprocal", out=t0, in_=t0), reads=ck, writes=ck)
    ck2 = ("tiny", "cco", "cinv")
    tt(cinv[:, 0, :], cco[:, 0, :], t0, ALU.mult, keys=ck2)
    tt(t1, cco[:, 1, :], t0, ALU.mult, keys=ck2); tsc(cinv[:, 1, :], t1, -1.0, ALU.mult, keys=ck2)
    for ri in range(2):
        P.dma(I("dma_start", out=ws[ri][:, 0:c.SC * 2, 0:128], in_=bemb[ri]),
              f"w4_{ri}", writes=[f"w4_{ri}"])
        P.op(V, I("tensor_copy", out=bemb_s[:, ri, :, :], in_=ws[ri][:, 0:c.SC * 2, 0:128]),
             reads=[f"w4_{ri}"], writes=["bemb_s"])
    for ct0 in range(0, NCT, 16):
        nb = min(16, NCT - ct0)
        for ri in range(2):
            P.dma(I("dma_start", out=ws[ri][:, 0:nb, 0:128], in_=cemb[ri, :, ct0:ct0 + nb, :]),
                  f"w4_{ri}", writes=[f"w4_{ri}"])
        for j in range(nb):
            ct = ct0 + j
            kk = ["w4_0", "w4_1", "cco", "cemb_s", "tmpA"]
            P.op(V, I("tensor_scalar", out=tmpA[:, 0, 0:128], in0=ws[1][:, j, 0:128], scalar1=cco[:, 1, ct:ct + 1],
                                                           scalar2=None, op0=ALU.mult), reads=kk, writes=["tmpA"])
            P.op(V, I("scalar_tensor_tensor", out=cemb_s[:, 0, ct, :], in0=ws[0][:, j, 0:128], scalar=cco[:, 0, ct:ct + 1],
                                                                  in1=tmpA[:, 0, 0:128], op0=ALU.mult, op1=ALU.subtract), reads=kk, writes=["cemb_s"])
            P.op(V, I("tensor_scalar", out=tmpA[:, 1, 0:128], in0=ws[1][:, j, 0:128], scalar1=cco[:, 0, ct:ct + 1],
                                                           scalar2=None, op0=ALU.mult), reads=kk, writes=["tmpA"])
            P.op(V, I("scalar_tensor_tensor", out=cemb_s[:, 1, ct, :], in0=ws[0][:, j, 0:128], scalar=ncci[:, ct:ct + 1],
                                                                  in1=tmpA[:, 1, 0:128], op0=ALU.mult, op1=ALU.subtract), reads=kk + ["ncci"], writes=["cemb_s"])

    final_evs = []

    def out_dma(dst, src, skey, slot):
        if slot.startswith("fin"):
            slot = f"{slot}_{len(final_evs)}"
        ev = P.dma(I("dma_start", out=dst, in_=src), slot, reads=[skey])
        final_evs.append(ev)
        return ev

    def load_x(src, Tn):
        for g0 in range(0, Tn, 128):
            gs = min(128, Tn - g0)
            P.dma(I("dma_start", out=xtok[0:gs, :], in_=src[g0:g0 + gs, :]), "xtok", writes=["xtok"])
            for k4 in range(0, KD, 4):
                ps, pkey = pbank()
                for j in range(4):
                    k = k4 + j
                    P.op(PE, I("transpose",
                        out=ps[:, j * 128:j * 128 + gs], in_=xtok[0:gs, k * 128:(k + 1) * 128], identity=ident[0:gs, 0:gs]),
                        reads=["xtok", "ident"], writes=[pkey])
                for j in range(4):
                    k = k4 + j
                    eng = V if j % 2 == 0 else S
                    if eng == V:
                        P.op(V, I("tensor_copy", out=hT[:, k, g0:g0 + gs], in_=ps[:, j * 128:j * 128 + gs]),
                             reads=[pkey], writes=["hT"])
                    else:
                        P.op(S, I("copy", out=hT[:, k, g0:g0 + gs], in_=ps[:, j * 128:j * 128 + gs]),
                             reads=[pkey], writes=["hT"])

    def store_y(dst, Tn, norm=True):
        if norm:
            rmsnorm(4, Tn, dst=hT)
        for g0 in range(0, Tn, 128):
            gs = min(128, Tn - g0)
            for k4 in range(0, KD, 4):
                ps, pkey = pbank()
                for j in range(4):
                    k = k4 + j
                    P.op(PE, I("transpose",
                        out=ps[0:gs, j * 128:(j + 1) * 128], in_=hT[:, k, g0:g0 + gs], identity=ident[:, :]),
                        reads=["hT", "ident"], writes=[pkey])
                P.op(V, I("tensor_copy", out=xtok[0:gs, k4 * 128:(k4 + 4) * 128], in_=ps[0:gs, :]),
                     reads=[pkey], writes=["xtok"])
            out_dma(dst[g0:g0 + gs, :], xtok[0:gs, :], "xtok", "ytok")

    def resid_epi(Tn):
        def epi(j, ps, pkey, m):
            P.op(V, I("tensor_tensor", out=hT[:, j, 0:Tn], in0=hT[:, j, 0:Tn], in1=ps[:, 0:Tn], op=ALU.add),
                 reads=[pkey, "hT"], writes=["hT"])
        return epi

    def mlp(layer, Tn):
        rmsnorm(2 + layer, Tn)
        for hb in range(0, c.DFF, D):
            def epi_up(j, ps, pkey, m):
                P.op(S, I("activation", out=tmpA[:, 0, 0:Tn], in_=ps[:, 0:Tn], func=AF.Relu), reads=[pkey], writes=["tmpA"])
                P.op(V, I("tensor_tensor", out=hid[:, j, 0:Tn], in0=tmpA[:, 0, 0:Tn], in1=tmpA[:, 0, 0:Tn], op=ALU.mult),
                     reads=["tmpA"], writes=["yT"])
            dense_fm(bw_up[layer], KD, D, xnT, "xnT", Tn, epi_up, col0=hb)
            dense_fm(bw_dn[layer], KD, D, hid, "yT", Tn, resid_epi(Tn), rows0=hb)

    def s5(Tn, runs, is_sample, hook=None):
        nr = len(runs)
        L = runs[0][1]
        nsteps = int(math.log2(L))
        WAYS = 4

        def f32v(t):
            return t.bitcast(F32).rearrange("p a b -> p (a b)")[:, 0:2 * T].rearrange("p (c t) -> p c t", c=2)

        def bf16v(t):
            return t.bitcast(BF16).rearrange("p (c t) -> p c t", c=2)
        sets = [dict(A=scA, B=scB, kA="scA", kB="scB", tr=tmpA[:, 0, 0:Tn], ti=tmpA[:, 1, 0:Tn], kt="tmpA", zb=zbf, kz="zbf",
                     tn=tA[:, 0, 0:nr], ktn="tiny"),
                dict(A=scA2, B=scB2, kA="scA2", kB="scB2", tr=tmpA[:, 2, 0:Tn], ti=tmpA[:, 3, 0:Tn], kt="tmpA2", zb=zbf2, kz="zbf2",
                     tn=tA[:, 1, 0:nr], ktn="tiny"),
                dict(A=f32v(qTb[:]), B=f32v(kTb[:]), kA="qTb", kB="kTb", tr=emB[:, 0:Tn], ti=wpB[:, 0:Tn], kt="emB", zb=bf16v(gB[:]), kz="gB",
                     tn=tA[:, 2, 0:nr], ktn="tiny"),
                dict(A=f32v(qwp[:]), B=f32v(C_bf[:, 0:2, :].rearrange("p a b -> p (a b)").rearrange("p (a b) -> p a b", a=4)), kA="qwp", kB="C_bf",
                     tr=FgB[:, 0:Tn], ti=MB[:, 0:Tn], kt="FgB", zb=ogs[:, 0:2, :], kz="ogs",
                     tn=tA[:, 3, 0:nr], ktn="tiny")]
        def v3(ap):
            return ap.rearrange("p (r l) -> p r l", l=L)

        def body(ct, bs):
            cch, r = ct // 4, ct % 4
            A, B_, kA, kB = bs["A"], bs["B"], bs["kA"], bs["kB"]
            psr, kr = pbank(4, 8)
            psi, ki = pbank(4, 8)
            for ri, (ps_, k_) in enumerate(((psr, kr), (psi, ki))):
                P.op(PE, I("matmul", out=ps_[:, 0:Tn], lhsT=bemb_s[64 * (r // 2):64 * (r // 2) + 64, ri, cch * 2 + r % 2, :],
                           rhs=uT[64 * (r // 2):64 * (r // 2) + 64, cch, 0:Tn], start=True, stop=True),
                     reads=["bemb_s", "uT"], writes=[k_])
            P.op(S, I("copy", out=A[:, 0, 0:Tn], in_=psr[:, 0:Tn]), reads=[kr], writes=[kA])
            P.op(S, I("copy", out=A[:, 1, 0:Tn], in_=psi[:, 0:Tn]), reads=[ki], writes=[kA])
            yield
            lr, li = lam[:, 0, 0, ct:ct + 1], lam[:, 0, 1, ct:ct + 1]
            z0r, z0i = zst[:, 0, ct, 0:nr], zst[:, 1, ct, 0:nr]
            a0r = v3(A[:, 0, 0:Tn])[:, :, 0]
            a0i = v3(A[:, 1, 0:Tn])[:, :, 0]
            kk = [kA, "zst", "lam"]
            tn, ktn = bs["tn"], bs["ktn"]
            P.op(V, I("scalar_tensor_tensor", out=a0r, in0=z0r, scalar=lr, in1=a0r, op0=ALU.mult, op1=ALU.add), reads=kk, writes=[kA])
            P.op(V, I("scalar_tensor_tensor", out=a0i, in0=z0i, scalar=lr, in1=a0i, op0=ALU.mult, op1=ALU.add), reads=kk, writes=[kA])
            P.op(V, I("tensor_scalar", out=tn, in0=z0i, scalar1=li, scalar2=-1.0, op0=ALU.mult, op1=ALU.mult), reads=kk + [ktn], writes=[ktn])
            P.op(V, I("tensor_tensor", out=a0r, in0=a0r, in1=tn, op=ALU.add), reads=kk + [ktn], writes=[kA])
            P.op(V, I("scalar_tensor_tensor", out=a0i, in0=z0r, scalar=li, in1=a0i, op0=ALU.mult, op1=ALU.add), reads=kk, writes=[kA])
            yield
            src, dst, ks, kd_ = A, B_, kA, kB
            tr, ti, kt = v3(bs["tr"]), v3(bs["ti"]), bs["kt"]
            kts = {"emB": ["emB", "wpB"], "FgB": ["FgB", "MB"]}.get(kt, [kt])
            for st in range(nsteps):
                dlt = 1 << st
                pr, pi, npi = lam[:, st, 0, ct:ct + 1], lam[:, st, 1, ct:ct + 1], lam[:, st, 2, ct:ct + 1]
                sr = v3(src[:, 0, 0:Tn]); si = v3(src[:, 1, 0:Tn])
                dr = v3(dst[:, 0, 0:Tn]); di = v3(dst[:, 1, 0:Tn])
                kk2 = [ks, kd_, "lam"] + kts
                P.op(S, I("copy", out=dr[:, :, 0:dlt], in_=sr[:, :, 0:dlt]), reads=[ks], writes=[kd_])
                P.op(S, I("copy", out=di[:, :, 0:dlt], in_=si[:, :, 0:dlt]), reads=[ks], writes=[kd_])
                P.op(V, I("scalar_tensor_tensor", out=tr[:, :, dlt:L], in0=sr[:, :, 0:L - dlt], scalar=pr, in1=sr[:, :, dlt:L],
                           op0=ALU.mult, op1=ALU.add), reads=kk2, writes=kts)
                P.op(V, I("scalar_tensor_tensor", out=ti[:, :, dlt:L], in0=si[:, :, 0:L - dlt], scalar=pr, in1=si[:, :, dlt:L],
                           op0=ALU.mult, op1=ALU.add), reads=kk2, writes=kts)
                yield
                P.op(V, I("scalar_tensor_tensor", out=dr[:, :, dlt:L], in0=si[:, :, 0:L - dlt], scalar=npi, in1=tr[:, :, dlt:L],
                           op0=ALU.mult, op1=ALU.add), reads=kk2, writes=[kd_])
                P.op(V, I("scalar_tensor_tensor", out=di[:, :, dlt:L], in0=sr[:, :, 0:L - dlt], scalar=pi, in1=ti[:, :, dlt:L],
                           op0=ALU.mult, op1=ALU.add), reads=kk2, writes=[kd_])
                yield
                src, dst, ks, kd_ = dst, src, kd_, ks
            zl_r = v3(src[:, 0, 0:Tn])[:, :, L - 1]
            zl_i = v3(src[:, 1, 0:Tn])[:, :, L - 1]
            P.op(V, I("tensor_copy", out=zst[:, 0, ct, 0:nr], in_=zl_r), reads=[ks, "zst"], writes=["zst"])
            P.op(V, I("tensor_copy", out=zst[:, 1, ct, 0:nr], in_=zl_i), reads=[ks, "zst"], writes=["zst"])
            zb, kz = bs["zb"], bs["kz"]
            P.op(S, I("copy", out=zb[:, :, 0:Tn], in_=src[:, :, 0:Tn]), reads=[ks], writes=[kz])
            if r == 0:
                s5.ps, s5.pk = pbank(0, 4)
            psy, ky = s5.ps, s5.pk
            P.op(PE, I("matmul", out=psy[:, 0:Tn], lhsT=cemb_s[:, 0, ct, :], rhs=zb[:, 0, 0:Tn], start=(r == 0), stop=False),
                 reads=["cemb_s", kz], writes=[ky])
            P.op(PE, I("matmul", out=psy[:, 0:Tn], lhsT=cemb_s[:, 1, ct, :], rhs=zb[:, 1, 0:Tn], start=False, stop=(r == 3)),
                 reads=["cemb_s", kz], writes=[ky])
            if r == 3:
                a, b = tmpB[:, 0, 0:Tn], tmpB[:, 1, 0:Tn]
                P.op(V, I("scalar_tensor_tensor", out=a, in0=uT[:, cch, 0:Tn], scalar=s5d_s[:, 0, cch:cch + 1], in1=psy[:, 0:Tn],
                           op0=ALU.mult, op1=ALU.add), reads=[ky, "uT", "s5d_s"], writes=["tmpB"])
                P.op(V, I("tensor_tensor", out=b, in0=a, in1=a, op=ALU.mult), reads=["tmpB"], writes=["tmpB"])
                P.op(V, I("tensor_scalar", out=b, in0=b, scalar1=0.044715, scalar2=1.0, op0=ALU.mult, op1=ALU.add), reads=["tmpB"], writes=["tmpB"])
                P.op(V, I("tensor_tensor", out=b, in0=b, in1=a, op=ALU.mult), reads=["tmpB"], writes=["tmpB"])
                P.op(S, I("activation", out=b, in_=b, func=AF.Sigmoid, scale=1.5957691216057308), reads=["tmpB"], writes=["tmpB"])
                P.op(V, I("tensor_tensor", out=ygl[:, cch, 0:Tn], in0=a, in1=b, op=ALU.mult), reads=["tmpB"], writes=["ygl"])
            yield

        for ct0 in range(0, NCT, WAYS):
            gens = [body(ct0 + w, sets[w]) for w in range(min(WAYS, NCT - ct0))]
            alive = list(gens)
            while alive:
                for g in list(alive):
                    try:
                        next(g)
                    except StopIteration:
                        alive.remove(g)
            if hook is not None and (ct0 + WAYS) % 4 == 0:
                hook((ct0 + WAYS) // 4 - 1)
        def epi_glu(j, ps, pkey, m):
            P.op(S, I("activation", out=tmpB[:, 0, 0:Tn], in_=ps[:, 0:Tn], func=AF.Sigmoid, bias=s5d_s[:, 1, j:j + 1]),
                 reads=[pkey, "s5d_s"], writes=["tmpB"])
            P.op(V, I("tensor_tensor", out=yT[:, j, 0:Tn], in0=tmpB[:, 0, 0:Tn], in1=ygl[:, j, 0:Tn], op=ALU.mult),
                 reads=["tmpB", "ygl"], writes=["yT"])
        dense_fm(bglu, c.SC, c.S5W, ygl, "ygl", Tn, epi_glu)
    s5.ps = None

    def tok_proj(w, col0, ncols, Tn, g0, gs, dst, dkey, dcol0, scale=None):
        assert col0 % 128 == 0 and ncols % 256 == 0
        for cb in range(0, ncols, 256):
            wt, wkey = wtile(w, (col0 + cb) // 128, 2, 0, KD)
            ps, pkey = pbank()
            for k in range(KD):
                P.op(PE, I("matmul", out=ps[0:gs, 0:256].rearrange("p (c n) -> p c n", c=2), lhsT=xnT[:, k, g0:g0 + gs], rhs=wt[:, :, k, :],
                           start=(k == 0), stop=(k == KD - 1)), reads=[wkey, "xnT"], writes=[pkey])
            if scale is None:
                P.op(S, I("copy", out=dst[0:gs, dcol0 + cb:dcol0 + cb + 256], in_=ps[0:gs, 0:256]), reads=[pkey], writes=[dkey])
            else:
                P.op(S, I("activation", out=dst[0:gs, dcol0 + cb:dcol0 + cb + 256], in_=ps[0:gs, 0:256], func=AF.Copy, scale=scale),
                     reads=[pkey], writes=[dkey])

    def gla(Tn, groups, is_sample):
        o1 = c.S5W; o2 = o1 + c.GDK; o3 = o2 + c.GDK; o4 = o3 + c.GDV; o5 = o4 + c.GDV
        wt, wkey = wtile(bw_in_e, o5 // 128, 1, 0, KD)
        ps, pkey = pbank()
        for k in range(KD):
            P.op(PE, I("matmul", out=ps[0:16, 0:Tn], lhsT=wt[:, 0, k, 0:16], rhs=xnT[:, k, 0:Tn], start=(k == 0), stop=(k == KD - 1)),
                 reads=[wkey, "xnT"], writes=[pkey])
        P.op(V, I("tensor_copy", out=gkl[:, 0:Tn], in_=ps[0:16, 0:Tn]), reads=[pkey], writes=["gkl"])
        yield
        for h in range(c.GH):
            ps, pkey = pbank(4, 8)
            P.op(PE, I("matmul", out=ps[:, 0:Tn], lhsT=gku[:, h * 128:(h + 1) * 128], rhs=gkl[:, 0:Tn], start=True, stop=True),
                 reads=["gku", "gkl"], writes=[pkey])
            P.op(S, I("activation", out=Ep[:, 0:Tn], in_=ps[:, 0:Tn], func=AF.Exp, scale=-1.0, bias=ngkb[:, h:h + 1]), reads=[pkey, "ngkb"], writes=["Ep"])
            P.op(S, I("activation", out=Ep[:, 0:Tn], in_=Ep[:, 0:Tn], func=AF.Ln, bias=1.0), reads=["Ep"], writes=["Ep"])
            P.op(V, I("tensor_scalar", out=Ep[:, 0:Tn], in0=Ep[:, 0:Tn], scalar1=-1.0 / 16.0, scalar2=None, op0=ALU.mult), reads=["Ep"], writes=["Ep"])
            for (g0, gs, chunks) in groups:
                for (c0, cl, sq) in chunks:
                    P.op(V, I("tensor_tensor_scan", out=Em[:, c0:c0 + cl], data0=ones_f[:, 0:cl], data1=Ep[:, c0:c0 + cl],
                                                                          initial=0.0, op0=ALU.mult, op1=ALU.add), reads=["Ep", "ones_f"], writes=["Em"])
            P.op(S, I("activation", out=Ep[:, 0:Tn], in_=Em[:, 0:Tn], func=AF.Exp), reads=["Em"], writes=["Ep"])
            P.op(S, I("activation", out=Em[:, 0:Tn], in_=Em[:, 0:Tn], func=AF.Exp, scale=-1.0), reads=["Em", "Ep"], writes=["Em"])
            def epi_q(j, ps, pkey, m):
                P.op(V, I("scalar_tensor_tensor", out=qd[:, 0:Tn], in0=ps[:, 0:Tn], scalar=float(c.HK) ** -0.5, in1=Ep[:, 0:Tn], op0=ALU.mult, op1=ALU.mult),
                     reads=[pkey, "Ep"], writes=["qd"])
            dense_fm(bw_in_e, KD, 128, xnT, "xnT", Tn, epi_q, col0=o1 + h * 128)
            def epi_k(j, ps, pkey, m):
                P.op(V, I("tensor_tensor", out=kd[:, 0:Tn], in0=ps[:, 0:Tn], in1=Em[:, 0:Tn], op=ALU.mult), reads=[pkey, "Em"], writes=["kd"])
            dense_fm(bw_in_e, KD, 128, xnT, "xnT", Tn, epi_k, col0=o2 + h * 128)
            def epi_g(j, ps, pkey, m):
                P.op(S, I("activation", out=gsil[:, j, 0:Tn], in_=ps[:, 0:Tn], func=AF.Silu), reads=[pkey], writes=["gsil"])
            dense_fm(bw_in_e, KD, 256, xnT, "xnT", Tn, epi_g, col0=o4 + h * 256)
            for (g0, gs, chunks) in groups:
                for (c0, cl, sq) in chunks:
                    P.op(V, I("tensor_scalar", out=kdc[:, c0:c0 + cl], in0=kd[:, c0:c0 + cl], scalar1=Ep[:, c0 + cl - 1:c0 + cl],
                                                                    scalar2=None, op0=ALU.mult), reads=["kd", "Ep"], writes=["kdc"])
            for (g0, gs, chunks) in groups:
                msk = tri if not is_sample else blk
                tok_proj(bw_in_e, o3 + h * 256, 256, Tn, g0, gs, vtok, "vtok", 0)
                ps, pkey = pbank(4, 8)
                P.op(PE, I("matmul", out=ps[0:gs, 0:gs], lhsT=kd[:, g0:g0 + gs], rhs=qd[:, g0:g0 + gs], start=True, stop=True),
                     reads=["kd", "qd"], writes=[pkey])
                P.op(V, I("tensor_tensor", out=attT[0:gs, 0:gs], in0=ps[0:gs, 0:gs], in1=msk[0:gs, 0:gs], op=ALU.mult),
                     reads=[pkey, "tri", "blk"], writes=["attT"])
                ps2, pkey2 = pbank(4, 8)
                P.op(PE, I("matmul", out=ps2[0:gs, 0:128], lhsT=kdc[:, g0:g0 + gs], rhs=ones_id[:, :], start=True, stop=True),
                     reads=["kdc", "ones_id"], writes=[pkey2])
                P.op(S, I("copy", out=ktok[0:gs, 0:128], in_=ps2[0:gs, 0:128]), reads=[pkey2], writes=["ktok"])
                pso = [pbank(4, 8) for _ in range(2)]
                for ec in range(2):
                    P.op(PE, I("matmul", out=pso[ec][0][:, 0:gs], lhsT=vtok[0:gs, ec * 128:(ec + 1) * 128], rhs=attT[0:gs, 0:gs], start=True, stop=False),
                         reads=["vtok", "attT"], writes=[pso[ec][1]])
                for ci, (c0, cl, sq) in enumerate(chunks):
                    if is_sample:
                        P.dma(I("dma_start", out=S_sb[:, h, :], in_=st_gla[sq, h]), "S_ld", writes=["S_sb"])
                    P.op(S, I("copy", out=S_bf[:, :], in_=S_sb[:, h, :]), reads=["S_sb"], writes=["S_bf"])
                    last = ci == len(chunks) - 1
                    for ec in range(2):
                        P.op(PE, I("matmul", out=pso[ec][0][:, c0 - g0:c0 - g0 + cl], lhsT=S_bf[:, ec * 128:(ec + 1) * 128],
                                                                        rhs=qd[:, c0:c0 + cl], start=False, stop=last),
                             reads=["S_bf", "qd"], writes=[pso[ec][1]])
                    if len(chunks) > 1:
                        P.op(V, I("tensor_scalar", out=kwt[0:gs, 0:128], in0=ktok[0:gs, 0:128], scalar1=blk[0:gs, c0 - g0 + cl - 1:c0 - g0 + cl],
                                                                        scalar2=None, op0=ALU.mult), reads=["ktok", "blk"], writes=["kwt"])
                        lk_, lkey = kwt, "kwt"
                    else:
                        lk_, lkey = ktok, "ktok"
                    psS, kS = pbank(0, 4)
                    P.op(PE, I("matmul", out=psS[:, 0:256], lhsT=lk_[0:gs, 0:128], rhs=vtok[0:gs, 0:256], start=True, stop=True),
                         reads=[lkey, "vtok"], writes=[kS])
                    P.op(V, I("scalar_tensor_tensor", out=S_sb[:, h, :], in0=S_sb[:, h, :], scalar=Ep[:, c0 + cl - 1:c0 + cl],
                                                                                    in1=psS[:, 0:256], op0=ALU.mult, op1=ALU.add), reads=[kS, "S_sb", "S_bf", "Ep"], writes=["S_sb"])
                    if is_sample:
                        out_dma(o_sgla[sq, h], S_sb[:, h, :], "S_sb", "S_st")
                for ec in range(2):
                    P.op(V, I("tensor_copy", out=oT[:, ec, g0:g0 + gs], in_=pso[ec][0][:, 0:gs]), reads=[pso[ec][1]], writes=["oT"])
            for ec in range(2):
                P.op(S, I("activation", out=qwp[:, ec, 0:Tn], in_=oT[:, ec, 0:Tn], func=AF.Square), reads=["oT"], writes=["qwp"])
            ps, pkey = pbank()
            for ec in range(2):
                P.op(PE, I("matmul", out=ps[:, 0:Tn], lhsT=ones_bf[:, :], rhs=qwp[:, ec, 0:Tn], start=(ec == 0), stop=(ec == 1)),
                     reads=["qwp", "ones_bf"], writes=[pkey])
            P.op(S, I("activation", out=rstdB[:, 0:Tn], in_=ps[:, 0:Tn], func=AF.Sqrt, scale=1.0 / c.HV, bias=EPS), reads=[pkey], writes=["rstdB"])
            P.op(V, I("reciprocal", out=rstdB[:, 0:Tn], in_=rstdB[:, 0:Tn]), reads=["rstdB"], writes=["rstdB"])
            for ec in range(2):
                P.op(V, I("scalar_tensor_tensor", out=oT[:, ec, 0:Tn], in0=oT[:, ec, 0:Tn], scalar=gnorm_s[:, ec:ec + 1], in1=rstdB[:, 0:Tn],
                                                                op0=ALU.mult, op1=ALU.mult), reads=["oT", "rstdB", "gnorm_s"], writes=["oT"])
                P.op(V, I("tensor_tensor", out=yT[:, c.SC + h * 2 + ec, 0:Tn], in0=oT[:, ec, 0:Tn], in1=gsil[:, ec, 0:Tn], op=ALU.mult),
                     reads=["oT", "gsil"], writes=["yT"])
            yield

    ones_id = P.sb("ones_id", [128, 128], BF16)
    P.op(V, I("tensor_copy", out=ones_id[:, :], in_=ident[:, :]), reads=["ident"], writes=["ones_id"])

    def mlstm(Tn, groups, runs, is_sample):
        for h in range(c.MH):
            bi, nbf = mb_s[:, h:h + 1], nmb[:, c.MH + h:c.MH + h + 1]
            def epi_i(j, ps, pkey, m):
                P.op(V, I("tensor_scalar", out=gB[:, 0:Tn], in0=ps[:, 0:Tn], scalar1=bi, scalar2=None, op0=ALU.add), reads=[pkey, "mb_s"], writes=["gB"])
            dense_fm(bw_gate, KD, 128, xnT, "xnT", Tn, epi_i, col0=h * 128)
            def epi_f(j, ps, pkey, m):
                P.op(S, I("activation", out=FgB[:, 0:Tn], in_=ps[:, 0:Tn], func=AF.Exp, scale=-1.0, bias=nbf), reads=[pkey, "nmb"], writes=["FgB"])
                P.op(S, I("activation", out=FgB[:, 0:Tn], in_=FgB[:, 0:Tn], func=AF.Ln, bias=1.0), reads=["FgB"], writes=["FgB"])
                P.op(V, I("tensor_scalar", out=wpB[:, 0:Tn], in0=FgB[:, 0:Tn], scalar1=-1.0, scalar2=None, op0=ALU.mult), reads=["FgB"], writes=["wpB"])
            dense_fm(bw_gate, KD, 128, xnT, "xnT", Tn, epi_f, col0=(c.MH + h) * 128)
            for ri, (r0, rl) in enumerate(runs):
                if is_sample:
                    fin = 0.0
                    min_ = mm0[:, ri * c.MH + h:ri * c.MH + h + 1]
                else:
                    fin = Fcar[:, h:h + 1]
                    min_ = Mcar[:, h:h + 1]
                P.op(V, I("tensor_tensor_scan", out=FgB[:, r0:r0 + rl], data0=ones_f[:, 0:rl], data1=wpB[:, r0:r0 + rl],
                                                                              initial=fin, op0=ALU.mult, op1=ALU.add), reads=["wpB", "ones_f", "Fcar", "FgB"], writes=["FgB"])
                P.op(V, I("tensor_tensor", out=gB[:, r0:r0 + rl], in0=gB[:, r0:r0 + rl], in1=FgB[:, r0:r0 + rl], op=ALU.subtract),
                     reads=["gB", "FgB"], writes=["gB"])
                P.op(V, I("tensor_tensor_scan", out=MB[:, r0:r0 + rl], data0=ones_f[:, 0:rl], data1=gB[:, r0:r0 + rl],
                                                                                initial=min_, op0=ALU.mult, op1=ALU.max), reads=["gB", "ones_f", "Mcar", "mm0", "MB"], writes=["MB"])
            P.op(V, I("tensor_tensor", out=emB[:, 0:Tn], in0=FgB[:, 0:Tn], in1=MB[:, 0:Tn], op=ALU.add), reads=["FgB", "MB"], writes=["emB"])
            for ri, (r0, rl) in enumerate(runs):
                if is_sample:
                    P.op(V, I("tensor_copy", out=mout[0:1, h * NS + ri:h * NS + ri + 1], in_=emB[0:1, r0 + rl - 1:r0 + rl]),
                         reads=["emB"], writes=["mout"])
                else:
                    P.op(V, I("tensor_copy", out=mout[0:1, h:h + 1], in_=emB[0:1, r0 + rl - 1:r0 + rl]), reads=["emB"], writes=["mout"])
            P.op(S, I("activation", out=emB[:, 0:Tn], in_=emB[:, 0:Tn], func=AF.Exp, scale=-1.0), reads=["emB", "mout"], writes=["emB"])
            for (g0, gs, chunks) in groups:
                for (c0, cl, sq) in chunks:
                    first = any(c0 == r0 for (r0, rl) in runs)
                    if first:
                        ri = [i for i, (r0, rl) in enumerate(runs) if r0 == c0][0]
                        mp = mm0[:, ri * c.MH + h:ri * c.MH + h + 1] if is_sample else Mcar[:, h:h + 1]
                    else:
                        mp = MB[:, c0 - 1:c0]
                    P.op(S, I("activation", out=wpB[:, c0:c0 + cl], in_=MB[:, c0:c0 + cl], func=AF.Exp, scale=-1.0, bias=mp),
                         reads=["MB", "Mcar", "mm0", "wpB", "FgB"], writes=["wpB"])
            def epi_q(j, ps, pkey, m):
                P.op(S, I("copy", out=qTb[:, j, 0:Tn], in_=ps[:, 0:Tn]), reads=[pkey], writes=["qTb"])
                P.op(V, I("tensor_tensor", out=qwp[:, j, 0:Tn], in0=ps[:, 0:Tn], in1=wpB[:, 0:Tn], op=ALU.mult), reads=[pkey, "wpB"], writes=["qwp"])
            dense_fm(bw_in_o, KD, 512, xnT, "xnT", Tn, epi_q, col0=h * 512)
            def epi_k(j, ps, pkey, m):
                P.op(S, I("activation", out=kTb[:, j, 0:Tn], in_=ps[:, 0:Tn], func=AF.Copy, scale=float(c.DH) ** -0.5), reads=[pkey], writes=["kTb"])
            dense_fm(bw_in_o, KD, 512, xnT, "xnT", Tn, epi_k, col0=D + h * 512)
            def epi_og(j, ps, pkey, m):
                P.op(S, I("activation", out=ogs[:, j, 0:Tn], in_=ps[:, 0:Tn], func=AF.Sigmoid), reads=[pkey], writes=["ogs"])
            dense_fm(bw_in_o, KD, 512, xnT, "xnT", Tn, epi_og, col0=3 * D + h * 512)
            if not is_sample:
                P.op(S, I("copy", out=C_bf[:, :, :], in_=C_sb[:, h, :, :]), reads=[f"C_sb{h}"], writes=["C_bf"])
            for (g0, gs, chunks) in groups:
                msk = tri if not is_sample else blk
                tok_proj(bw_in_o, D + h * 512, 512, Tn, g0, gs, ktok, "ktok", 0, scale=float(c.DH) ** -0.5)
                tok_proj(bw_in_o, 2 * D + h * 512, 512, Tn, g0, gs, vtok, "vtok", 0)
                ps, pkey = pbank(5, 8)
                P.op(PE, I("transpose", out=ps[0:gs, 0:128], in_=gB[:, g0:g0 + gs], identity=ident[:, :]), reads=["gB", "ident"], writes=[pkey])
                P.op(V, I("tensor_copy", out=gcol[0:gs, 0:1], in_=ps[0:gs, 0:1]), reads=[pkey], writes=["gcol"])
                P.op(S, I("activation", out=wT[0:gs, 0:gs], in_=MB[0:gs, g0:g0 + gs], func=AF.Exp, scale=-1.0, bias=gcol[0:gs, 0:1]),
                     reads=["MB", "gcol"], writes=["wT"])
                P.op(V, I("tensor_tensor", out=wTm[0:gs, 0:gs], in0=wT[0:gs, 0:gs], in1=msk[0:gs, 0:gs], op=ALU.mult), reads=["wT", "tri", "blk"], writes=["wTm"])
                ps, pkey = pbank(5, 8)
                for dc in range(4):
                    P.op(PE, I("matmul", out=ps[0:gs, 0:gs], lhsT=kTb[:, dc, g0:g0 + gs], rhs=qTb[:, dc, g0:g0 + gs], start=(dc == 0), stop=(dc == 3)),
                         reads=["kTb", "qTb"], writes=[pkey])
                P.op(V, I("tensor_tensor", out=attT[0:gs, 0:gs], in0=ps[0:gs, 0:gs], in1=wTm[0:gs, 0:gs], op=ALU.mult), reads=[pkey, "wTm"], writes=["attT"])
                psn = [(pb[i], f"pb{i}") for i in range(4)]
                psd, kdn = pb[4], "pb4"
                for ec in range(4):
                    P.op(PE, I("matmul", out=psn[ec][0][:, 0:gs], lhsT=vtok[0:gs, ec * 128:(ec + 1) * 128], rhs=attT[0:gs, 0:gs], start=True, stop=False),
                         reads=["vtok", "attT"], writes=[psn[ec][1]])
                P.op(PE, I("matmul", out=psd[:, 0:gs], lhsT=ones_bf[0:gs, :], rhs=attT[0:gs, 0:gs], start=True, stop=False), reads=["ones_bf", "attT"], writes=[kdn])
                NSL = c.MH

                def update_state(ci, c0, cl, sq, sl):
                    lc = c0 - g0 + cl - 1
                    P.op(V, I("tensor_scalar", out=kwt[0:gs, 0:512], in0=ktok[0:gs, 0:512], scalar1=wTm[0:gs, lc:lc + 1], scalar2=None, op0=ALU.mult),
                         reads=["ktok", "wTm"], writes=["kwt"])
                    dcol = wpB[:, c0 + cl - 1:c0 + cl]
                    for dc in range(4):
                        psC, kC = pbank(5, 8)
                        P.op(PE, I("matmul", out=psC[:, 0:512], lhsT=kwt[0:gs, dc * 128:(dc + 1) * 128], rhs=vtok[0:gs, 0:512], start=True, stop=True),
                             reads=["kwt", "vtok"], writes=[kC])
                        P.op(V, I("scalar_tensor_tensor", out=C_sb[:, sl, dc, :], in0=C_sb[:, sl, dc, :], scalar=dcol, in1=psC[:, 0:512],
                                   op0=ALU.mult, op1=ALU.add), reads=[kC, f"C_sb{sl}", "C_bf", "wpB"], writes=[f"C_sb{sl}"])
                        psN, kN = pbank(5, 8)
                        P.op(PE, I("matmul", out=psN[:, 0:2], lhsT=kwt[0:gs, dc * 128:(dc + 1) * 128], rhs=ones_bf[0:gs, 0:2], start=True, stop=True),
                             reads=["kwt", "ones_bf"], writes=[kN])
                        P.op(V, I("scalar_tensor_tensor", out=n_sb[:, sl, dc:dc + 1], in0=n_sb[:, sl, dc:dc + 1], scalar=dcol, in1=psN[:, 0:1],
                                   op0=ALU.mult, op1=ALU.add), reads=[kN, f"n_sb{sl}", "Nrep", "wpB"], writes=[f"n_sb{sl}"])
                    if is_sample:
                        out_dma(o_smc[sq, h].rearrange("(dc p) e -> p dc e", p=128), C_sb[:, sl, :, :], f"C_sb{sl}", f"C_st{sl}")
                        out_dma(o_smn[sq, h], n_sb[:, sl, :], f"n_sb{sl}", f"n_st{sl}")
                    else:
                        P.op(S, I("copy", out=C_bf[:, :, :], in_=C_sb[:, sl, :, :]), reads=[f"C_sb{sl}"], writes=["C_bf"])

                def load_state(ci):
                    sq_ = chunks[ci][2]
                    sl_ = ci % NSL
                    P.dma(I("dma_start", out=C_sb[:, sl_, :, :], in_=st_mc[sq_, h].rearrange("(dc p) e -> p dc e", p=128)), f"C_ld{sl_}", writes=[f"C_sb{sl_}"])
                    P.dma(I("dma_start", out=n_sb[:, sl_, :], in_=st_mn[sq_, h]), f"n_ld{sl_}", writes=[f"n_sb{sl_}"])

                PF = max(1, min(2, NSL - 1))
                if is_sample:
                    for ci_ in range(min(PF, len(chunks))):
                        load_state(ci_)
                for ci, (c0, cl, sq) in enumerate(chunks):
                    last = ci == len(chunks) - 1
                    sl = ci % NSL if is_sample else h
                    if is_sample:
                        if ci + PF < len(chunks):
                            load_state(ci + PF)
                        P.op(S, I("copy", out=C_bf[:, :, :], in_=C_sb[:, sl, :, :]), reads=[f"C_sb{sl}"], writes=["C_bf"])
                    for dc in range(4):
                        P.op(V, I("tensor_scalar", out=Nrep[:, dc, :], in0=ones_bf[:, :], scalar1=n_sb[:, sl, dc:dc + 1], scalar2=None, op0=ALU.mult),
                             reads=["ones_bf", f"n_sb{sl}"], writes=["Nrep"])
                    for dc in range(4):
                        lst = last and dc == 3
                        for ec in range(4):
                            P.op(PE, I("matmul", out=psn[ec][0][:, c0 - g0:c0 - g0 + cl], lhsT=C_bf[:, dc, ec * 128:(ec + 1) * 128],
                                                                                         rhs=qwp[:, dc, c0:c0 + cl], start=False, stop=lst), reads=["C_bf", "qwp"], writes=[psn[ec][1]])
                        P.op(PE, I("matmul", out=psd[:, c0 - g0:c0 - g0 + cl], lhsT=Nrep[:, dc, :], rhs=qwp[:, dc, c0:c0 + cl], start=False, stop=lst),
                             reads=["Nrep", "qwp"], writes=[kdn])
                    if is_sample:
                        update_state(ci, c0, cl, sq, sl)
                P.op(S, I("activation", out=rden[:, 0:gs], in_=psd[:, 0:gs], func=AF.Abs), reads=[kdn], writes=["rden"])
                P.op(V, I("tensor_tensor", out=rden[:, 0:gs], in0=rden[:, 0:gs], in1=emB[:, g0:g0 + gs], op=ALU.max), reads=["rden", "emB"], writes=["rden"])
                P.op(V, I("reciprocal", out=rden[:, 0:gs], in_=rden[:, 0:gs]), reads=["rden"], writes=["rden"])
                for ec in range(4):
                    P.op(V, I("tensor_tensor", out=oT[:, ec, g0:g0 + gs], in0=psn[ec][0][:, 0:gs], in1=rden[:, 0:gs], op=ALU.mult), reads=[psn[ec][1], "rden"], writes=["oT"])
                    P.op(V, I("tensor_tensor", out=oT[:, ec, g0:g0 + gs], in0=oT[:, ec, g0:g0 + gs], in1=ogs[:, ec, g0:g0 + gs], op=ALU.mult), reads=["oT", "ogs"], writes=["oT"])
                if not is_sample:
                    for ci, (c0, cl, sq) in enumerate(chunks):
                        update_state(ci, c0, cl, sq, h)
            if not is_sample:
                r0, rl = runs[-1]
                P.op(V, I("tensor_copy", out=Fcar[:, h:h + 1], in_=FgB[:, r0 + rl - 1:r0 + rl]), reads=["FgB"], writes=["Fcar"])
                P.op(V, I("tensor_copy", out=Mcar[:, h:h + 1], in_=MB[:, r0 + rl - 1:r0 + rl]), reads=["MB"], writes=["Mcar"])
            for ec in range(4):
                P.op(S, I("activation", out=uT[:, ec, 0:Tn], in_=oT[:, ec, 0:Tn], func=AF.Square), reads=["oT"], writes=["uT"])
            ps, pkey = pbank()
            for ec in range(4):
                P.op(PE, I("matmul", out=ps[:, 0:Tn], lhsT=ones_bf[:, :], rhs=uT[:, ec, 0:Tn], start=(ec == 0), stop=(ec == 3)), reads=["uT", "ones_bf"], writes=[pkey])
            P.op(S, I("activation", out=rstdB[:, 0:Tn], in_=ps[:, 0:Tn], func=AF.Sqrt, scale=1.0 / c.DH, bias=EPS), reads=[pkey], writes=["rstdB"])
            P.op(V, I("reciprocal", out=rstdB[:, 0:Tn], in_=rstdB[:, 0:Tn]), reads=["rstdB"], writes=["rstdB"])
            for ec in range(4):
                P.op(V, I("scalar_tensor_tensor", out=yT[:, h * 4 + ec, 0:Tn], in0=oT[:, ec, 0:Tn], scalar=mnorm_s[:, ec:ec + 1], in1=rstdB[:, 0:Tn],
                                                                op0=ALU.mult, op1=ALU.mult), reads=["oT", "rstdB", "mnorm_s"], writes=["yT"])

    def segment(src, dst, Tn, groups, runs, is_sample):
        load_x(src, Tn)
        rmsnorm(0, Tn)
        def epi_u(j, ps, pkey, m):
            P.op(S, I("copy", out=uT[:, j, 0:Tn], in_=ps[:, 0:Tn]), reads=[pkey], writes=["uT"])
        dense_fm(bw_in_e, KD, c.S5W, xnT, "xnT", Tn, epi_u)
        gg = gla(Tn, groups, is_sample)
        next(gg)

        def hook(chunk):
            if chunk % 2 == 0:
                next(gg, None)
        s5(Tn, runs, is_sample, hook=hook)
        for _ in gg:
            pass
        dense_fm(bw_out_e, KD, D, yT, "yT", Tn, resid_epi(Tn))
        if getattr(c, "dbg", 0) == 1:
            store_y(dst, Tn, norm=False)
            return
        mlp(0, Tn)
        rmsnorm(1, Tn)
        mlstm(Tn, groups, runs, is_sample)
        dense_fm(bw_out_o, KD, D, yT, "yT", Tn, resid_epi(Tn))
        mlp(1, Tn)
        store_y(dst, Tn)

    P.op(V, I("memset", zst[:], 0.0), writes=["zst"])
    P.op(V, I("memset", S_sb[:], 0.0), writes=["S_sb"])
    P.op(V, I("memset", C_sb[:], 0.0), writes=[f"C_sb{i}" for i in range(c.MH)])
    P.op(V, I("memset", n_sb[:], 0.0), writes=[f"n_sb{i}" for i in range(c.MH)])
    P.op(V, I("memset", Fcar[:], 0.0), writes=["Fcar"])
    P.op(V, I("memset", mout[:], 0.0), writes=["mout"])
    P.op(V, I("memset", Mcar[:], NEG), writes=["Mcar"])
    for s0 in range(0, c.SEQ, T):
        groups = [(g0, 128, [(g0, 128, 0)]) for g0 in range(0, T, 128)]
        segment(xp[s0:s0 + T, :], yp[s0:s0 + T, :], T, groups, [(0, T)], False)
    zk = ["zst", "cco", "tiny"]
    zr, zi = zst[:, 0, :, 0], zst[:, 1, :, 0]
    tt(t0, zr, cco[:, 0, :], ALU.mult, keys=zk); tt(t1, zi, cco[:, 1, :], ALU.mult, keys=zk); tt(t0, t0, t1, ALU.subtract, keys=zk)
    tt(t2, zr, cco[:, 1, :], ALU.mult, keys=zk); tt(t3, zi, cco[:, 0, :], ALU.mult, keys=zk); tt(t2, t2, t3, ALU.add, keys=zk)
    out_dma(o_ps5[0], t0, "tiny", "fin"); out_dma(o_ps5[1], t2, "tiny", "fin")
    for h in range(c.GH):
        out_dma(o_pgla[h], S_sb[:, h, :], "S_sb", "fin")
    for h in range(c.MH):
        out_dma(o_pmc[h].rearrange("(dc p) e -> p dc e", p=128), C_sb[:, h, :, :], f"C_sb{h}", "fin")
        out_dma(o_pmn[h], n_sb[:, h, :], f"n_sb{h}", "fin")
    out_dma(o_pmm, mout[0:1, 0:c.MH], "mout", "fin")

    if NS > 0:
        TS = c.TS
        P.dma(I("dma_start", out=zst[:, 0, :, :], in_=st_s5[0]), "zld", reads=["tiny"], writes=["zst"])
        P.dma(I("dma_start", out=zst[:, 1, :, :], in_=st_s5[1]), "zld", reads=["tiny"], writes=["zst"])
        P.seal("zld")
        W_ = NCT * NS
        nsl = (W_ + T - 1) // T
        assert 6 * nsl <= KD
        big = [hT[:, nsl * i:nsl * i + nsl, :].rearrange("p a b -> p (a b)")[:, 0:W_].rearrange("p (a b) -> p a b", b=NS) for i in range(6)]
        cr_b = cinv[:, 0, :].unsqueeze(2).broadcast_to([128, NCT, NS]); ci_b = cinv[:, 1, :].unsqueeze(2).broadcast_to([128, NCT, NS])
        zk2 = ["zst", "cinv", "tiny", "hT"]
        tt(big[0], zst[:, 0, :, :], cr_b, ALU.mult, keys=zk2); tt(big[1], zst[:, 1, :, :], ci_b, ALU.mult, keys=zk2)
        tt(big[2], zst[:, 0, :, :], ci_b, ALU.mult, keys=zk2); tt(big[3], zst[:, 1, :, :], cr_b, ALU.mult, keys=zk2)
        tt(zst[:, 0, :, :], big[0], big[1], ALU.subtract, keys=zk2); tt(zst[:, 1, :, :], big[2], big[3], ALU.add, keys=zk2)
        chunks = [(4 * s, 4, s) for s in range(NS)]
        segment(xs, ys, TS, [(0, TS, chunks)], [(4 * s, 4) for s in range(NS)], True)
        cr_b = cco[:, 0, :].unsqueeze(2).broadcast_to([128, NCT, NS]); ci_b = cco[:, 1, :].unsqueeze(2).broadcast_to([128, NCT, NS])
        zk3 = ["zst", "cco", "tiny", "hT"]
        tt(big[0], zst[:, 0, :, :], cr_b, ALU.mult, keys=zk3); tt(big[1], zst[:, 1, :, :], ci_b, ALU.mult, keys=zk3)
        tt(big[2], zst[:, 0, :, :], ci_b, ALU.mult, keys=zk3); tt(big[3], zst[:, 1, :, :], cr_b, ALU.mult, keys=zk3)
        tt(big[4], big[0], big[1], ALU.subtract, keys=zk3); tt(big[5], big[2], big[3], ALU.add, keys=zk3)
        out_dma(o_ss5[0], big[4], "hT", "fin2"); out_dma(o_ss5[1], big[5], "hT", "fin2")
        out_dma(o_smm, mout[0:1, 0:c.MH * NS], "mout", "fin2")

    fw = {}
    for ev in final_evs:
        fw[ev[1]] = max(fw.get(ev[1], 0), ev[2])
    P.emit(final_waits=[("dma", s, k) for s, k in fw.items()])
    nc._stats = P.stats
    return nc


def _lay_vec(v, n):
    return np.ascontiguousarray(v.reshape(n, 128).T)


def make_inputs(cfg, core, inp):
    c = cfg
    f = np.float32
    b = core % inp["x_prompt"].shape[0]
    NS = c.NS
    s0 = core * NS
    m = {}
    m["xp"] = np.ascontiguousarray(inp["x_prompt"][b], dtype=f)
    m["xs"] = np.ascontiguousarray(inp["x_sample"][s0:s0 + NS].reshape(NS * 4, c.D), dtype=f)

    def s5lay(a):
        return a.reshape(NS, c.NCT, 2, 64).transpose(2, 3, 1, 0).reshape(128, c.NCT, NS)
    m["st_s5"] = np.ascontiguousarray(np.stack([s5lay(inp["state_s5_re"][0, s0:s0 + NS]), s5lay(inp["state_s5_im"][0, s0:s0 + NS])]), dtype=f)
    m["st_gla"] = np.ascontiguousarray(inp["state_gla"][0, s0:s0 + NS], dtype=f)
    m["st_mc"] = np.ascontiguousarray(inp["state_mlstm_c"][0, s0:s0 + NS], dtype=f)
    m["st_mn"] = np.ascontiguousarray(inp["state_mlstm_n"][0, s0:s0 + NS].reshape(NS, c.MH, 4, 128).transpose(0, 1, 3, 2), dtype=f)
    m["st_mm"] = np.ascontiguousarray(inp["state_mlstm_m"][0, s0:s0 + NS].reshape(1, NS * c.MH), dtype=f)
    return m


def make_shared(cfg, inp):
    c = cfg
    f = np.float32
    m = {}
    nv = [inp["norm_mix"][0], inp["norm_mix"][1], inp["norm_mlp"][0], inp["norm_mlp"][1], inp["norm_final"]]
    m["nrm"] = np.ascontiguousarray(np.stack([_lay_vec(v, c.KD) for v in nv], axis=1), dtype=f)
    m["w_in_e"] = np.ascontiguousarray(inp["w_in_even"][0], dtype=f)
    m["w_out_e"] = np.ascontiguousarray(inp["w_out_even"][0], dtype=f)
    m["w_in_o"] = np.ascontiguousarray(inp["w_in_odd"][0], dtype=f)
    m["w_out_o"] = np.ascontiguousarray(inp["w_out_odd"][0], dtype=f)
    m["w_gate"] = np.ascontiguousarray(np.repeat(inp["w_in_odd"][0][:, 4 * c.D:], 128, axis=1), dtype=f)
    m["w_up"] = np.ascontiguousarray(inp["w_mlp_up"], dtype=f)
    m["w_dn"] = np.ascontiguousarray(inp["w_mlp_down"], dtype=f)

    def gl(a):
        return a.reshape(c.NCT, 2, 64).transpose(1, 2, 0).reshape(128, c.NCT)
    ls = np.repeat(inp["s5_log_step"][0][:, None], 64, axis=1)
    m["s5p"] = np.ascontiguousarray(np.stack([gl(inp["s5_a_re"][0]), gl(inp["s5_a_im"][0]), gl(ls)], axis=1), dtype=f)
    bem = np.zeros((2, 128, c.SC, 2, 128), f)
    cem = np.zeros((2, 128, c.NCT, 128), f)
    for ri, (bsrc, csrc) in enumerate(((inp["s5_b_re"][0], inp["s5_c_re"][0]), (inp["s5_b_im"][0], inp["s5_c_im"][0]))):
        bg = bsrc.reshape(c.SC, 4, 2, 64, 16)
        cg = csrc.reshape(c.SC, 4, 2, 16, 64)
        for g2 in range(2):
            blk_ = bg[:, :, g2].transpose(1, 3, 0, 2)
            for r in range(4):
                bem[ri].reshape(4, 2, 16, c.SC, 2, 2, 64)[r, g2, :, :, r % 2, g2, :] = blk_[r]
            cb_ = cg[:, :, g2]
            for r in range(4):
                cem[ri].reshape(2, 64, c.SC, 4, 128)[g2, :, :, r, r * 32 + g2 * 16:r * 32 + g2 * 16 + 16] = cb_[:, r].transpose(2, 0, 1)
    m["bemb"] = np.ascontiguousarray(bem.reshape(2, 128, c.SC * 2, 128))
    m["cemb"] = cem
    m["s5d"] = np.ascontiguousarray(np.stack([_lay_vec(inp["s5_d"][0], c.SC), _lay_vec(inp["s5_glu_b"][0], c.SC)], axis=1), dtype=f)
    m["glu_w"] = np.ascontiguousarray(inp["s5_glu_w"][0], dtype=f)
    m["gk_up"] = np.ascontiguousarray(inp["gla_gk_up"][0], dtype=f)
    m["gkb"] = _lay_vec(inp["gla_gk_b"][0], c.GH).astype(f)
    m["gnorm"] = _lay_vec(inp["gla_norm"][0], 2).astype(f)
    m["mb"] = np.ascontiguousarray(np.concatenate([inp["mlstm_b_i"][0], inp["mlstm_b_f"][0]]).reshape(1, 2 * c.MH), dtype=f)
    m["mnorm"] = _lay_vec(inp["mlstm_norm"][0], 4).astype(f)
    m["ident"] = np.eye(128, dtype=f)
    m["tri"] = np.triu(np.ones((128, 128), f))
    i = np.arange(128)
    m["blk"] = ((i[:, None] // 4 == i[None, :] // 4) & (i[:, None] <= i[None, :])).astype(f)
    return m


def assemble(cfg, results, n_prompt, n_cores):
    c = cfg
    NS = c.NS
    yp = np.stack([results[b]["yp"] for b in range(n_prompt)])
    ys = np.concatenate([results[k]["ys"].reshape(NS, 4, c.D) for k in range(n_cores)], axis=0)

    def unl(a):
        return a.reshape(2, 64, c.NCT).transpose(2, 0, 1).reshape(c.NG, 64)
    ps5 = [np.stack([unl(results[b]["o_ps5"][ri]) for b in range(n_prompt)])[None] for ri in range(2)]
    pgla = np.stack([results[b]["o_pgla"] for b in range(n_prompt)])[None]
    pmc = np.stack([results[b]["o_pmc"] for b in range(n_prompt)])[None]
    pmn = np.stack([results[b]["o_pmn"].transpose(0, 2, 1).reshape(c.MH, 512) for b in range(n_prompt)])[None]
    pmm = np.stack([results[b]["o_pmm"].reshape(c.MH) for b in range(n_prompt)])[None]

    def unls(a):
        return a.reshape(2, 64, c.NCT, NS).transpose(3, 2, 0, 1).reshape(NS, c.NG, 64)
    ss5 = [np.concatenate([unls(results[k]["o_ss5"][ri]) for k in range(n_cores)])[None] for ri in range(2)]
    sgla = np.concatenate([results[k]["o_sgla"] for k in range(n_cores)])[None]
    smc = np.concatenate([results[k]["o_smc"] for k in range(n_cores)])[None]
    smn = np.concatenate([results[k]["o_smn"].transpose(0, 1, 3, 2).reshape(NS, c.MH, 512) for k in range(n_cores)])[None]
    smm = np.concatenate([results[k]["o_smm"].reshape(c.MH, NS).T for k in range(n_cores)])[None]
    outs = (yp, ys, ps5[0], ps5[1], pgla, pmc, pmn, pmm, ss5[0], ss5[1], sgla, smc, smn, smm)
    return tuple(np.ascontiguousarray(o, dtype=np.float32) for o in outs)


def kernel(**inputs):
    n_cores = 8
    cfg = Cfg()
    inp = {k: np.asarray(v) for k, v in inputs.items()}
    nc = build(cfg)
    shared = make_shared(cfg, inp)
    in_maps = []
    for k in range(n_cores):
        m = dict(shared)
        m.update(make_inputs(cfg, k, inp))
        in_maps.append(m)
    res = run_bass_kernel_spmd(nc, in_maps, core_ids=list(range(n_cores)))
    return assemble(cfg, res.results, inp["x_prompt"].shape[0], n_cores)
```

```python
import contextlib
import math
import numpy as np
import concourse.bass as bass
import concourse.mybir as mybir
from concourse.bass_utils import run_bass_kernel_spmd

F32 = mybir.dt.float32
BF16 = mybir.dt.bfloat16
AF = mybir.ActivationFunctionType
ALU = mybir.AluOpType
EPS = 1e-6
NEG = -1.0e30

CH = 16000
COMPUTE = ("tensor", "vector", "scalar", "gpsimd")
ENGS = ("tensor", "vector", "scalar", "gpsimd", "sync")


class Prog:
    def __init__(self, nc):
        self.nc = nc
        self.ops = {e: [] for e in ENGS}
        self.last_w = {}
        self.readers = {}
        self.seen = {e: {} for e in ENGS}
        self.dma_cnt = {}
        self.stack = contextlib.ExitStack()

    def sb(self, name, shape, dt):
        return self.stack.enter_context(self.nc.sbuf_tensor(name, list(shape), dt))

    def ps(self, name, shape, dt):
        return self.stack.enter_context(self.nc.psum_tensor(name, list(shape), dt))

    def _deps(self, eng, reads, writes):
        deps = []
        for r in reads:
            ev = self.last_w.get(r)
            if ev is not None:
                deps.append(ev)
            if r.startswith("pb"):
                deps.extend(o for o in self.readers.get(r, ()) if o[1] != eng)
        for w in writes:
            ev = self.last_w.get(w)
            if ev is not None:
                deps.append(ev)
            deps.extend(self.readers.get(w, ()))
        best = {}
        for (kind, who, idx) in deps:
            if kind == "eng" and who == "tensor" and eng == "tensor":
                continue
            key = (kind, who)
            if best.get(key, -1) < idx:
                best[key] = idx
        out = []
        seen = self.seen[eng]
        for key, idx in best.items():
            if seen.get(key, -1) >= idx:
                continue
            seen[key] = idx
            out.append((key[0], key[1], idx))
        return out

    def _commit(self, ev, reads, writes):
        for w in writes:
            self.last_w[w] = ev
            self.readers[w] = []
        for r in reads:
            if r not in writes:
                lst = self.readers.setdefault(r, [])
                for i, o in enumerate(lst):
                    if o[0] == ev[0] and o[1] == ev[1]:
                        lst[i] = ev
                        break
                else:
                    lst.append(ev)

    def op(self, eng, fn, reads=(), writes=()):
        reads = list(reads)
        writes = list(writes)
        deps = self._deps(eng, reads, writes)
        ev = ("eng", eng, len(self.ops[eng]))
        self.ops[eng].append(dict(fn=fn, deps=deps, ev=ev, dma=None))
        self._commit(ev, reads, writes)
        return ev

    def dma(self, fn, slot, reads=(), writes=(), queue="sync"):
        reads = list(reads)
        writes = list(writes)
        deps = self._deps(queue, reads, writes)
        k = self.dma_cnt.get(slot, 0) + 1
        self.dma_cnt[slot] = k
        ev = ("dma", slot, k)
        self.ops[queue].append(dict(fn=fn, deps=deps, ev=None, dma=(slot, k)))
        self._commit(ev, reads, writes)
        return ev

    def seal(self, slot):
        k = self.dma_cnt.get(slot, 0)
        for key, ev in list(self.last_w.items()):
            if ev[0] == "dma" and ev[1] == slot:
                self.last_w[key] = ("dma", slot, k)

    def emit(self, final_waits=()):
        nc = self.nc
        waited = {e: set() for e in COMPUTE}
        for e in ENGS:
            for o in self.ops[e]:
                for (kind, who, idx) in o["deps"]:
                    if kind == "eng":
                        waited[who].add(idx)
        rank = {}
        for e in COMPUTE:
            for r, idx in enumerate(sorted(waited[e])):
                rank[(e, idx)] = r
        sems = {}
        for e in COMPUTE:
            for j in range((len(waited[e]) + CH - 1) // CH):
                sems[(e, j)] = self.stack.enter_context(nc.semaphore(f"s_{e}_{j}"))
        DCH = 1000
        dsems = {}
        for slot, tot in self.dma_cnt.items():
            for j in range((tot + DCH - 1) // DCH):
                dsems[(slot, j)] = self.stack.enter_context(nc.semaphore(f"d_{slot}_{j}"))
        self.stats = dict(nsem=len(sems) + len(dsems), nops={e: len(self.ops[e]) for e in ENGS},
                          waited={e: len(waited[e]) for e in COMPUTE})

        def wait(engobj, ev):
            kind, who, idx = ev
            if kind == "eng":
                r = rank[(who, idx)]
                engobj.wait_ge(sems[(who, r // CH)], r % CH + 1)
            else:
                engobj.wait_ge(dsems[(who, (idx - 1) // DCH)], 16 * ((idx - 1) % DCH + 1))

        block = self.stack.enter_context(nc.Block())

        def make(e):
            def body(engobj):
                for o in self.ops[e]:
                    for ev in o["deps"]:
                        wait(engobj, ev)
                    name, a, kw = o["fn"]
                    inst = getattr(engobj, name)(*a, **kw)
                    if o["dma"] is not None:
                        inst.then_inc(dsems[(o["dma"][0], (o["dma"][1] - 1) // DCH)], 16)
                    elif (e, o["ev"][2]) in rank:
                        r = rank[(e, o["ev"][2])]
                        inst.then_inc(sems[(e, r // CH)], 1)
                if e == "sync":
                    for ev in final_waits:
                        wait(engobj, ev)
            return body

        block.tensor(make("tensor"))
        block.vector(make("vector"))
        block.scalar(make("scalar"))
        block.gpsimd(make("gpsimd"))
        block.sync(make("sync"))
        self.stack.close()


def I(name, *a, **kw):
    return (name, a, kw)


class Cfg:
    def __init__(self, D=2048, GH=4, MH=4, SEQ=2048, NS=16, T=256):
        self.D, self.GH, self.MH, self.SEQ, self.NS, self.T = D, GH, MH, SEQ, NS, T
        self.KD = D // 128
        self.S5W = D // 2
        self.NG = self.S5W // 16
        self.NCT = self.NG // 2
        self.SC = self.S5W // 128
        self.GDV = D - self.S5W
        self.GDK = self.GDV // 2
        self.HK = self.GDK // GH
        self.HV = self.GDV // GH
        assert self.HK == 128 and self.HV == 256
        self.EIN = self.S5W + 2 * self.GDK + 2 * self.GDV + 16
        self.DH = D // MH
        assert self.DH == 512
        self.OIN = 4 * D + 2 * MH
        self.DFF = 4 * D
        self.DS = 4
        self.TS = NS * self.DS


WCOLS = 256


def build(cfg):
    c = cfg
    D, KD, T, NS = c.D, c.KD, c.T, c.NS
    nc = bass.Bass("TRN2", target_bir_lowering=False)
    P = Prog(nc)

    def din(name, shape):
        return nc.dram_tensor(name, list(shape), F32, kind="ExternalInput").ap()

    def dout(name, shape):
        return nc.dram_tensor(name, list(shape), F32, kind="ExternalOutput").ap()

    xp = din("xp", [c.SEQ, D]); xs = din("xs", [c.TS, D])
    st_s5 = din("st_s5", [2, 128, c.NCT, NS])
    st_gla = din("st_gla", [NS, c.GH, 128, 256])
    st_mc = din("st_mc", [NS, c.MH, 512, 512])
    st_mn = din("st_mn", [NS, c.MH, 128, 4])
    st_mm = din("st_mm", [1, NS * c.MH])
    nrm = din("nrm", [128, 5, KD])
    w_in_e = din("w_in_e", [D, c.EIN]); w_out_e = din("w_out_e", [D, D])
    w_in_o = din("w_in_o", [D, c.OIN]); w_out_o = din("w_out_o", [D, D])
    w_gate = din("w_gate", [D, 2 * c.MH * 128])
    w_up = din("w_up", [2, D, c.DFF]); w_dn = din("w_dn", [2, c.DFF, D])
    s5p = din("s5p", [128, 3, c.NCT])
    bemb = din("bemb", [2, 128, c.SC * 2, 128]); cemb = din("cemb", [2, 128, c.NCT, 128])
    s5d = din("s5d", [128, 2, c.SC])
    glu_w = din("glu_w", [c.S5W, c.S5W])
    gk_up = din("gk_up", [16, c.GDK]); gkb = din("gkb", [128, c.GH]); gnorm = din("gnorm", [128, 2])
    mb = din("mb", [1, 2 * c.MH]); mnorm = din("mnorm", [128, 4])
    ident_d = din("ident", [128, 128]); tri_d = din("tri", [128, 128]); blk_d = din("blk", [128, 128])

    yp = dout("yp", [c.SEQ, D]); ys = dout("ys", [c.TS, D])
    o_ps5 = dout("o_ps5", [2, 128, c.NCT]); o_ss5 = dout("o_ss5", [2, 128, c.NCT, NS])
    o_pgla = dout("o_pgla", [c.GH, 128, 256]); o_sgla = dout("o_sgla", [NS, c.GH, 128, 256])
    o_pmc = dout("o_pmc", [c.MH, 512, 512]); o_smc = dout("o_smc", [NS, c.MH, 512, 512])
    o_pmn = dout("o_pmn", [c.MH, 128, 4]); o_smn = dout("o_smn", [NS, c.MH, 128, 4])
    o_pmm = dout("o_pmm", [1, c.MH]); o_smm = dout("o_smm", [1, c.MH * NS])

    hT = P.sb("hT", [128, KD, T], F32)
    xnT = P.sb("xnT", [128, KD, T], BF16)
    yT = P.sb("yT", [128, KD, T], BF16)
    hid = yT
    xtok = P.sb("xtok", [128, D], F32)
    W4all = P.sb("w4all", [128, 4, 16, 256], BF16)
    W4 = [W4all[:, i] for i in range(4)]
    ws = [W4[i].bitcast(F32) for i in range(2)]
    W4K = ["w4_0", "w4_1", "w4_2", "w4_3"]
    rstdB = P.sb("rstdB", [128, T], F32)
    nrm_s = P.sb("nrm_s", [128, 5, KD], F32)
    ident = P.sb("ident_s", [128, 128], F32)
    tri = P.sb("tri_s", [128, 128], F32); blk = P.sb("blk_s", [128, 128], F32)
    ones_bf = P.sb("ones_bf", [128, 128], BF16)
    ones_f = P.sb("ones_f", [128, T], F32)
    s5p_s = P.sb("s5p_s", [128, 3, c.NCT], F32)
    s5d_s = P.sb("s5d_s", [128, 2, c.SC], F32)
    lam = P.sb("lam", [128, 10, 3, c.NCT], F32)
    cco = P.sb("cco", [128, 2, c.NCT], F32)
    cinv = P.sb("cinv", [128, 2, c.NCT], F32)
    ncci = P.sb("ncci", [128, c.NCT], F32)
    tA = P.sb("tA", [128, 6, c.NCT], F32)
    bemb_s = P.sb("bemb_s", [128, 2, c.SC * 2, 128], BF16)
    cemb_s = P.sb("cemb_s", [128, 2, c.NCT, 128], BF16)
    zst = P.sb("zst", [128, 2, c.NCT, NS], F32)
    scA = P.sb("scA", [128, 2, T], F32); scB = P.sb("scB", [128, 2, T], F32)
    zbf = P.sb("zbf", [128, 2, T], BF16)
    scA2 = P.sb("scA2", [128, 2, T], F32); scB2 = P.sb("scB2", [128, 2, T], F32); zbf2 = P.sb("zbf2", [128, 2, T], BF16)
    uT = P.sb("uT", [128, c.SC, T], BF16)
    ygl = P.sb("ygl", [128, c.SC, T], BF16)
    tmpA = P.sb("tmpA", [128, 4, T], F32)
    tmpB = P.sb("tmpB", [128, 2, T], F32)
    gkl = P.sb("gkl", [16, T], F32); gku = P.sb("gku", [16, c.GDK], F32)
    gkb_s = P.sb("gkb_s", [128, c.GH], F32); ngkb = P.sb("ngkb", [128, c.GH], F32)
    gnorm_s = P.sb("gnorm_s", [128, 2], F32)
    S_sb = P.sb("S_sb", [128, c.GH, 256], F32); S_bf = P.sb("S_bf", [128, 256], BF16)
    cmask = P.sb("cmask", [128, T], F32)
    qd = P.sb("qd", [128, T], BF16); kd = P.sb("kd", [128, T], BF16); kdc = P.sb("kdc", [128, T], BF16)
    Ep = P.sb("Ep", [128, T], F32); Em = P.sb("Em", [128, T], F32)
    vtok = P.sb("vtok", [128, 512], BF16); ktok = P.sb("ktok", [128, 512], BF16)
    kwt = P.sb("kwt", [128, 512], BF16)
    attT = P.sb("attT", [128, 128], BF16)
    gsil = P.sb("gsil", [128, 2, T], F32)
    oT = P.sb("oT", [128, 4, T], F32)
    mb_s = P.sb("mb_s", [128, 2 * c.MH], F32); nmb = P.sb("nmb", [128, 2 * c.MH], F32)
    mnorm_s = P.sb("mnorm_s", [128, 4], F32)
    mm0 = P.sb("mm0", [128, NS * c.MH], F32)
    C_sb = P.sb("C_sb", [128, c.MH, 4, 512], F32); C_bf = P.sb("C_bf", [128, 4, 512], BF16)
    n_sb = P.sb("n_sb", [128, c.MH, 4], F32); Nrep = P.sb("Nrep", [128, 4, 128], BF16)
    Fcar = P.sb("Fcar", [128, c.MH], F32); Mcar = P.sb("Mcar", [128, c.MH], F32)
    FgB = P.sb("FgB", [128, T], F32); MB = P.sb("MB", [128, T], F32); gB = P.sb("gB", [128, T], F32)
    wpB = P.sb("wpB", [128, T], F32); emB = P.sb("emB", [128, T], F32)
    gcol = P.sb("gcol", [128, 1], F32)
    wT = P.sb("wT", [128, 128], F32); wTm = P.sb("wTm", [128, 128], F32)
    qTb = P.sb("qTb", [128, 4, T], BF16); kTb = P.sb("kTb", [128, 4, T], BF16); qwp = P.sb("qwp", [128, 4, T], BF16)
    ogs = P.sb("ogs", [128, 4, T], BF16)
    rden = P.sb("rden", [128, 128], F32)
    mout = P.sb("mout", [1, c.MH * max(NS, 1)], F32)
    pb = [P.ps(f"pb{i}", [128, 512], F32) for i in range(8)]

    V, S, G, PE = "vector", "scalar", "gpsimd", "tensor"
    cnt = {"w": 0, "pd": 0}

    def ld(dst, src, key):
        P.dma(I("dma_start", out=dst, in_=src), "setup", writes=[key])

    ld(nrm_s[:], nrm, "nrm_s"); ld(ident[:], ident_d, "ident"); ld(tri[:], tri_d, "tri"); ld(blk[:], blk_d, "blk")
    ld(s5p_s[:], s5p, "s5p_s"); ld(s5d_s[:], s5d, "s5d_s")
    ld(gku[:], gk_up, "gku"); ld(gkb_s[:], gkb, "gkb_s"); ld(gnorm_s[:], gnorm, "gnorm_s")
    ld(mb_s[:], mb.broadcast_to([128, 2 * c.MH]), "mb_s"); ld(mnorm_s[:], mnorm, "mnorm_s")
    ld(mm0[:], st_mm.broadcast_to([128, NS * c.MH]), "mm0")
    P.seal("setup")
    P.op(V, I("memset", ones_bf[:], 1.0), writes=["ones_bf"])
    P.op(V, I("memset", ones_f[:], 1.0), writes=["ones_f"])
    P.op(V, I("tensor_scalar", out=ngkb[:], in0=gkb_s[:], scalar1=-1.0, scalar2=None, op0=ALU.mult),
         reads=["gkb_s"], writes=["ngkb"])
    P.op(V, I("tensor_scalar", out=nmb[:], in0=mb_s[:], scalar1=-1.0, scalar2=None, op0=ALU.mult),
         reads=["mb_s"], writes=["nmb"])

    def tt(out, a, b, op, keys=("tiny",)):
        P.op(V, I("tensor_tensor", out=out, in0=a, in1=b, op=op), reads=keys, writes=keys)

    def tsc(out, a, s1, op0, s2=None, op1=None, keys=("tiny",)):
        if op1 is None:
            P.op(V, I("tensor_scalar", out=out, in0=a, scalar1=s1, scalar2=None, op0=op0), reads=keys, writes=keys)
        else:
            P.op(V, I("tensor_scalar", out=out, in0=a, scalar1=s1, scalar2=s2, op0=op0, op1=op1), reads=keys, writes=keys)

    def act(out, a, func, keys=("tiny",), **kw):
        P.op(S, I("activation", out=out, in_=a, func=func, **kw), reads=keys, writes=keys)

    def wtile(scrt, cb0, ncb, k0, kc):
        i = cnt["w"] % 4
        cnt["w"] += 1
        view = W4[i].rearrange("p a b -> p (a b)")[:, 0:ncb * kc * 128].rearrange("p (c k n) -> p c k n", c=ncb, k=kc)
        P.dma(I("dma_start", out=view, in_=scrt[cb0:cb0 + ncb, :, k0:k0 + kc, :].rearrange("c p k n -> p c k n")),
              f"w4_{i}", reads=["dw0", "dw1"], writes=[f"w4_{i}"])
        return view, f"w4_{i}"

    def pbank(lo=0, hi=4):
        i = lo + cnt["pd"] % (hi - lo)
        cnt["pd"] += 1
        return pb[i], f"pb{i}"

    def dense_fm(w, kin, ncols_total, inT, inkey, Tn, epi, rows0=0, col0=0):
        assert col0 % 128 == 0 and rows0 % 128 == 0 and ncols_total % 128 == 0
        nb_tot = ncols_total // 128
        for b0 in range(0, nb_tot, 2):
            ncb = min(2, nb_tot - b0)
            wt, wkey = wtile(w, col0 // 128 + b0, ncb, rows0 // 128, kin)
            for sub in range(ncb):
                ps, pkey = pbank()
                for k in range(kin):
                    P.op(PE, I("matmul", out=ps[:, 0:Tn], lhsT=wt[:, sub, k, :], rhs=inT[:, k, 0:Tn],
                               start=(k == 0), stop=(k == kin - 1)), reads=[wkey, inkey], writes=[pkey])
                epi(b0 + sub, ps, pkey, 128)

    def rmsnorm(idx, Tn, dst=None):
        for k in range(KD):
            P.op(S, I("activation", out=xnT[:, k, 0:Tn], in_=hT[:, k, 0:Tn], func=AF.Square),
                 reads=["hT"], writes=["xnT"])
        ps, pkey = pbank()
        for k in range(KD):
            P.op(PE, I("matmul", out=ps[:, 0:Tn], lhsT=ones_bf[:, :], rhs=xnT[:, k, 0:Tn],
                                            start=(k == 0), stop=(k == KD - 1)),
                 reads=["xnT", "ones_bf"], writes=[pkey])
        P.op(S, I("activation", out=rstdB[:, 0:Tn], in_=ps[:, 0:Tn], func=AF.Sqrt, scale=1.0 / D, bias=EPS),
             reads=[pkey], writes=["rstdB"])
        P.op(V, I("reciprocal", out=rstdB[:, 0:Tn], in_=rstdB[:, 0:Tn]), reads=["rstdB"], writes=["rstdB"])
        for k in range(KD):
            o = xnT[:, k, 0:Tn] if dst is None else dst[:, k, 0:Tn]
            P.op(V, I("scalar_tensor_tensor",
                out=o, in0=hT[:, k, 0:Tn], scalar=nrm_s[:, idx, k:k + 1], in1=rstdB[:, 0:Tn],
                op0=ALU.mult, op1=ALU.mult), reads=["hT", "rstdB", "nrm_s"], writes=["xnT" if dst is None else "hT"])

    def scr(name, rows, cols):
        return nc.dram_tensor(name, [(cols + 127) // 128, 128, rows // 128, 128], BF16, kind="Internal").ap()

    bw_in_e = scr("bw_in_e", D, c.EIN); bw_out_e = scr("bw_out_e", D, D)
    bw_in_o = scr("bw_in_o", D, 4 * D); bw_out_o = scr("bw_out_o", D, D)
    bw_gate = scr("bw_gate", D, 2 * c.MH * 128)
    bw_up = [scr(f"bw_up{l_}", D, c.DFF) for l_ in range(2)]; bw_dn = [scr(f"bw_dn{l_}", c.DFF, D) for l_ in range(2)]
    bglu = scr("bglu", c.S5W, c.S5W)
    cnt["pc"] = 0
    stg_f = [C_sb[:].rearrange("p a b c -> p (a b c)"), W4all[:].bitcast(F32).rearrange("p a b c -> p (a b c)")]
    stg_k = [[f"C_sb{i}" for i in range(c.MH)], W4K]
    ob_b = [hT[:].bitcast(BF16).rearrange("p a b -> p (a b)"), cemb_s[:].rearrange("p a b c -> p (a b c)")]
    ob_k = ["hT", "cemb_s"]
    PCW = 2048
    kb_max = min(4, c.MH * 4 * 512 // PCW, 8192 // PCW, KD * T * 2 // PCW, 2 * c.NCT * 128 // PCW)
    assert kb_max >= 1

    def precast(src, dst, rows, cols):
        KC = rows // 128
        for c0 in range(0, cols, PCW):
            ncol = min(PCW, cols - c0)
            ncb = (ncol + 127) // 128
            n_ = min(128, ncol)
            assert ncol % 128 == 0 or ncb == 1
            for k0 in range(0, KC, kb_max):
                kb = min(kb_max, KC - k0)
                i = cnt["pc"] % 2
                cnt["pc"] += 1
                stg = stg_f[i][:, 0:kb * PCW].rearrange("p (k n) -> p k n", k=kb)
                ob = ob_b[i][:, 0:ncb * kb * 128].rearrange("p (c k n) -> p c k n", c=ncb, k=kb)
                P.dma(I("dma_start", out=stg[:, :, 0:ncol], in_=src[k0 * 128:(k0 + kb) * 128, c0:c0 + ncol].rearrange("(k p) n -> p k n", p=128)),
                      f"pcl{i}", writes=stg_k[i])
                cin = stg[:, :, 0:ncb * n_].rearrange("p k (c n) -> p c k n", c=ncb)
                if n_ < 128:
                    P.op(V, I("memset", ob[:, :, :, :], 0.0), reads=[ob_k[i]], writes=[ob_k[i]])
                if i == 0:
                    P.op(V, I("tensor_copy", out=ob[:, :, :, 0:n_], in_=cin), reads=stg_k[i], writes=[ob_k[i]])
                else:
                    P.op(S, I("copy", out=ob[:, :, :, 0:n_], in_=cin), reads=stg_k[i], writes=[ob_k[i]])
                P.dma(I("dma_start", out=dst[c0 // 128:c0 // 128 + ncb, :, k0:k0 + kb, :].rearrange("c p k n -> p c k n"), in_=ob[:, :, :, :]),
                      f"pcs{i}", reads=[ob_k[i]], writes=[f"dw{i}"])

    precast(w_in_e, bw_in_e, D, c.EIN); precast(w_out_e, bw_out_e, D, D)
    precast(w_in_o[:, 0:4 * D], bw_in_o, D, 4 * D); precast(w_out_o, bw_out_o, D, D)
    precast(w_gate, bw_gate, D, 2 * c.MH * 128); precast(glu_w, bglu, c.S5W, c.S5W)
    for l_ in range(2):
        precast(w_up[l_], bw_up[l_], D, c.DFF); precast(w_dn[l_], bw_dn[l_], c.DFF, D)

    NCT = c.NCT
    t0, t1, t2, t3, t4, t5 = (tA[:, i, 0:NCT] for i in range(6))
    sk = ("tiny", "s5p_s")
    act(t0, s5p_s[:, 2, :], AF.Exp, keys=sk)
    tt(t1, s5p_s[:, 0, :], t0, ALU.mult, keys=sk)
    tt(t2, s5p_s[:, 1, :], t0, ALU.mult, keys=sk)
    act(t3, t1, AF.Exp, scale=1.0 / 32.0)
    act(t4, t2, AF.Sin, scale=1.0 / 32.0)
    act(t5, t2, AF.Sin, scale=1.0 / 32.0, bias=math.pi / 2)
    lk = ("tiny", "lam")
    tt(lam[:, 0, 0, :], t3, t5, ALU.mult, keys=lk)
    tt(lam[:, 0, 1, :], t3, t4, ALU.mult, keys=lk)

    def csquare(dst_re, dst_im, a, b):
        tt(t0, a, a, ALU.mult, keys=lk); tt(t1, b, b, ALU.mult, keys=lk); tt(t2, a, b, ALU.mult, keys=lk)
        tt(dst_re, t0, t1, ALU.subtract, keys=lk)
        tsc(dst_im, t2, 2.0, ALU.mult, keys=lk)

    for _ in range(5):
        csquare(t3, t4, lam[:, 0, 0, :], lam[:, 0, 1, :])
        tsc(lam[:, 0, 0, :], t3, 1.0, ALU.mult, keys=lk); tsc(lam[:, 0, 1, :], t4, 1.0, ALU.mult, keys=lk)
    for k in range(1, 10):
        csquare(lam[:, k, 0, :], lam[:, k, 1, :], lam[:, k - 1, 0, :], lam[:, k - 1, 1, :])
    for k in range(10):
        tsc(lam[:, k, 2, :], lam[:, k, 1, :], -1.0, ALU.mult, keys=lk)
    ck = ("tiny", "lam", "cco", "s5p_s")
    ar_, ai_ = s5p_s[:, 0, :], s5p_s[:, 1, :]
    tsc(t0, lam[:, 0, 0, :], -1.0, ALU.add, keys=ck)
    tt(t1, ar_, ar_, ALU.mult, keys=ck); tt(t2, ai_, ai_, ALU.mult, keys=ck); tt(t1, t1, t2, ALU.add, keys=ck)
    P.op(V, I("reciprocal", out=t1, in_=t1), reads=ck, writes=ck)
    tt(t2, t0, ar_, ALU.mult, keys=ck); tt(t3, lam[:, 0, 1, :], ai_, ALU.mult, keys=ck); tt(t2, t2, t3, ALU.add, keys=ck)
    tt(cco[:, 0, :], t2, t1, ALU.mult, keys=ck)
    tt(t2, lam[:, 0, 1, :], ar_, ALU.mult, keys=ck); tt(t3, t0, ai_, ALU.mult, keys=ck); tt(t2, t2, t3, ALU.subtract, keys=ck)
    tt(cco[:, 1, :], t2, t1, ALU.mult, keys=ck)
    tsc(ncci[:, :], cco[:, 1, :], -1.0, ALU.mult, keys=("tiny", "cco", "ncci"))
    tt(t0, cco[:, 0, :], cco[:, 0, :], ALU.mult, keys=ck); tt(t1, cco[:, 1, :], cco[:, 1, :], ALU.mult, keys=ck)
    tt(t0, t0, t1, ALU.add, keys=ck)
    P.op(V, I("reciprocal", out=t0, in_=t0), reads=ck, writes=ck)
    ck2 = ("tiny", "cco", "cinv")
    tt(cinv[:, 0, :], cco[:, 0, :], t0, ALU.mult, keys=ck2)
    tt(t1, cco[:, 1, :], t0, ALU.mult, keys=ck2); tsc(cinv[:, 1, :], t1, -1.0, ALU.mult, keys=ck2)
    for ri in range(2):
        P.dma(I("dma_start", out=ws[ri][:, 0:c.SC * 2, 0:128], in_=bemb[ri]),
              f"w4_{ri}", writes=[f"w4_{ri}"])
        P.op(V, I("tensor_copy", out=bemb_s[:, ri, :, :], in_=ws[ri][:, 0:c.SC * 2, 0:128]),
             reads=[f"w4_{ri}"], writes=["bemb_s"])
    for ct0 in range(0, NCT, 16):
        nb = min(16, NCT - ct0)
        for ri in range(2):
            P.dma(I("dma_start", out=ws[ri][:, 0:nb, 0:128], in_=cemb[ri, :, ct0:ct0 + nb, :]),
                  f"w4_{ri}", writes=[f"w4_{ri}"])
        for j in range(nb):
            ct = ct0 + j
            kk = ["w4_0", "w4_1", "cco", "cemb_s", "tmpA"]
            P.op(V, I("tensor_scalar", out=tmpA[:, 0, 0:128], in0=ws[1][:, j, 0:128], scalar1=cco[:, 1, ct:ct + 1],
                                                           scalar2=None, op0=ALU.mult), reads=kk, writes=["tmpA"])
            P.op(V, I("scalar_tensor_tensor", out=cemb_s[:, 0, ct, :], in0=ws[0][:, j, 0:128], scalar=cco[:, 0, ct:ct + 1],
                                                                  in1=tmpA[:, 0, 0:128], op0=ALU.mult, op1=ALU.subtract), reads=kk, writes=["cemb_s"])
            P.op(V, I("tensor_scalar", out=tmpA[:, 1, 0:128], in0=ws[1][:, j, 0:128], scalar1=cco[:, 0, ct:ct + 1],
                                                           scalar2=None, op0=ALU.mult), reads=kk, writes=["tmpA"])
            P.op(V, I("scalar_tensor_tensor", out=cemb_s[:, 1, ct, :], in0=ws[0][:, j, 0:128], scalar=ncci[:, ct:ct + 1],
                                                                  in1=tmpA[:, 1, 0:128], op0=ALU.mult, op1=ALU.subtract), reads=kk + ["ncci"], writes=["cemb_s"])

    final_evs = []

    def out_dma(dst, src, skey, slot):
        if slot.startswith("fin"):
            slot = f"{slot}_{len(final_evs)}"
        ev = P.dma(I("dma_start", out=dst, in_=src), slot, reads=[skey], queue="gpsimd")
        final_evs.append(ev)
        return ev

    def load_x(src, Tn):
        for g0 in range(0, Tn, 128):
            gs = min(128, Tn - g0)
            P.dma(I("dma_start", out=xtok[0:gs, :], in_=src[g0:g0 + gs, :]), "xtok", writes=["xtok"])
            for k4 in range(0, KD, 4):
                ps, pkey = pbank()
                for j in range(4):
                    k = k4 + j
                    P.op(PE, I("transpose",
                        out=ps[:, j * 128:j * 128 + gs], in_=xtok[0:gs, k * 128:(k + 1) * 128], identity=ident[0:gs, 0:gs]),
                        reads=["xtok", "ident"], writes=[pkey])
                for j in range(4):
                    k = k4 + j
                    eng = V if j % 2 == 0 else S
                    if eng == V:
                        P.op(V, I("tensor_copy", out=hT[:, k, g0:g0 + gs], in_=ps[:, j * 128:j * 128 + gs]),
                             reads=[pkey], writes=["hT"])
                    else:
                        P.op(S, I("copy", out=hT[:, k, g0:g0 + gs], in_=ps[:, j * 128:j * 128 + gs]),
                             reads=[pkey], writes=["hT"])

    def store_y(dst, Tn, norm=True):
        if norm:
            rmsnorm(4, Tn, dst=hT)
        for g0 in range(0, Tn, 128):
            gs = min(128, Tn - g0)
            for k4 in range(0, KD, 4):
                ps, pkey = pbank()
                for j in range(4):
                    k = k4 + j
                    P.op(PE, I("transpose",
                        out=ps[0:gs, j * 128:(j + 1) * 128], in_=hT[:, k, g0:g0 + gs], identity=ident[:, :]),
                        reads=["hT", "ident"], writes=[pkey])
                P.op(V, I("tensor_copy", out=xtok[0:gs, k4 * 128:(k4 + 4) * 128], in_=ps[0:gs, :]),
                     reads=[pkey], writes=["xtok"])
            out_dma(dst[g0:g0 + gs, :], xtok[0:gs, :], "xtok", "ytok")

    def resid_epi(Tn):
        def epi(j, ps, pkey, m):
            P.op(V, I("tensor_tensor", out=hT[:, j, 0:Tn], in0=hT[:, j, 0:Tn], in1=ps[:, 0:Tn], op=ALU.add),
                 reads=[pkey, "hT"], writes=["hT"])
        return epi

    def mlp(layer, Tn):
        rmsnorm(2 + layer, Tn)
        for hb in range(0, c.DFF, D):
            def epi_up(j, ps, pkey, m):
                P.op(S, I("activation", out=tmpA[:, 0, 0:Tn], in_=ps[:, 0:Tn], func=AF.Relu), reads=[pkey], writes=["tmpA"])
                P.op(V, I("tensor_tensor", out=hid[:, j, 0:Tn], in0=tmpA[:, 0, 0:Tn], in1=tmpA[:, 0, 0:Tn], op=ALU.mult),
                     reads=["tmpA"], writes=["yT"])
            dense_fm(bw_up[layer], KD, D, xnT, "xnT", Tn, epi_up, col0=hb)
            dense_fm(bw_dn[layer], KD, D, hid, "yT", Tn, resid_epi(Tn), rows0=hb)

    def s5(Tn, runs, is_sample, hook=None):
        nr = len(runs)
        L = runs[0][1]
        nsteps = int(math.log2(L))
        WAYS = 4

        def f32v(t):
            return t.bitcast(F32).rearrange("p a b -> p (a b)")[:, 0:2 * T].rearrange("p (c t) -> p c t", c=2)

        def bf16v(t):
            return t.bitcast(BF16).rearrange("p (c t) -> p c t", c=2)
        sets = [dict(A=scA, B=scB, kA="scA", kB="scB", tr=tmpA[:, 0, 0:Tn], ti=tmpA[:, 1, 0:Tn], kt="tmpA", zb=zbf, kz="zbf",
                     tn=tA[:, 0, 0:nr], ktn="tiny"),
                dict(A=scA2, B=scB2, kA="scA2", kB="scB2", tr=tmpA[:, 2, 0:Tn], ti=tmpA[:, 3, 0:Tn], kt="tmpA2", zb=zbf2, kz="zbf2",
                     tn=tA[:, 1, 0:nr], ktn="tiny"),
                dict(A=f32v(qTb[:]), B=f32v(kTb[:]), kA="qTb", kB="kTb", tr=emB[:, 0:Tn], ti=wpB[:, 0:Tn], kt="emB", zb=bf16v(gB[:]), kz="gB",
                     tn=tA[:, 2, 0:nr], ktn="tiny"),
                dict(A=f32v(qwp[:]), B=f32v(C_bf[:, 0:2, :].rearrange("p a b -> p (a b)").rearrange("p (a b) -> p a b", a=4)), kA="qwp", kB="C_bf",
                     tr=FgB[:, 0:Tn], ti=MB[:, 0:Tn], kt="FgB", zb=ogs[:, 0:2, :], kz="ogs",
                     tn=tA[:, 3, 0:nr], ktn="tiny")]
        def v3(ap):
            return ap.rearrange("p (r l) -> p r l", l=L)

        def body(ct, bs):
            cch, r = ct // 4, ct % 4
            A, B_, kA, kB = bs["A"], bs["B"], bs["kA"], bs["kB"]
            psr, kr = pbank(4, 8)
            psi, ki = pbank(4, 8)
            for ri, (ps_, k_) in enumerate(((psr, kr), (psi, ki))):
                P.op(PE, I("matmul", out=ps_[:, 0:Tn], lhsT=bemb_s[64 * (r // 2):64 * (r // 2) + 64, ri, cch * 2 + r % 2, :],
                           rhs=uT[64 * (r // 2):64 * (r // 2) + 64, cch, 0:Tn], start=True, stop=True),
                     reads=["bemb_s", "uT"], writes=[k_])
            P.op(S, I("copy", out=A[:, 0, 0:Tn], in_=psr[:, 0:Tn]), reads=[kr], writes=[kA])
            P.op(S, I("copy", out=A[:, 1, 0:Tn], in_=psi[:, 0:Tn]), reads=[ki], writes=[kA])
            yield
            lr, li = lam[:, 0, 0, ct:ct + 1], lam[:, 0, 1, ct:ct + 1]
            z0r, z0i = zst[:, 0, ct, 0:nr], zst[:, 1, ct, 0:nr]
            a0r = v3(A[:, 0, 0:Tn])[:, :, 0]
            a0i = v3(A[:, 1, 0:Tn])[:, :, 0]
            kk = [kA, "zst", "lam"]
            tn, ktn = bs["tn"], bs["ktn"]
            P.op(V, I("scalar_tensor_tensor", out=a0r, in0=z0r, scalar=lr, in1=a0r, op0=ALU.mult, op1=ALU.add), reads=kk, writes=[kA])
            P.op(V, I("scalar_tensor_tensor", out=a0i, in0=z0i, scalar=lr, in1=a0i, op0=ALU.mult, op1=ALU.add), reads=kk, writes=[kA])
            P.op(V, I("tensor_scalar", out=tn, in0=z0i, scalar1=li, scalar2=-1.0, op0=ALU.mult, op1=ALU.mult), reads=kk + [ktn], writes=[ktn])
            P.op(V, I("tensor_tensor", out=a0r, in0=a0r, in1=tn, op=ALU.add), reads=kk + [ktn], writes=[kA])
            P.op(V, I("scalar_tensor_tensor", out=a0i, in0=z0r, scalar=li, in1=a0i, op0=ALU.mult, op1=ALU.add), reads=kk, writes=[kA])
            yield
            src, dst, ks, kd_ = A, B_, kA, kB
            tr, ti, kt = v3(bs["tr"]), v3(bs["ti"]), bs["kt"]
            kts = {"emB": ["emB", "wpB"], "FgB": ["FgB", "MB"]}.get(kt, [kt])
            for st in range(nsteps):
                dlt = 1 << st
                pr, pi, npi = lam[:, st, 0, ct:ct + 1], lam[:, st, 1, ct:ct + 1], lam[:, st, 2, ct:ct + 1]
                sr = v3(src[:, 0, 0:Tn]); si = v3(src[:, 1, 0:Tn])
                dr = v3(dst[:, 0, 0:Tn]); di = v3(dst[:, 1, 0:Tn])
                kk2 = [ks, kd_, "lam"] + kts
                P.op(S, I("copy", out=dr[:, :, 0:dlt], in_=sr[:, :, 0:dlt]), reads=[ks], writes=[kd_])
                P.op(S, I("copy", out=di[:, :, 0:dlt], in_=si[:, :, 0:dlt]), reads=[ks], writes=[kd_])
                P.op(V, I("scalar_tensor_tensor", out=tr[:, :, dlt:L], in0=sr[:, :, 0:L - dlt], scalar=pr, in1=sr[:, :, dlt:L],
                           op0=ALU.mult, op1=ALU.add), reads=kk2, writes=kts)
                P.op(V, I("scalar_tensor_tensor", out=ti[:, :, dlt:L], in0=si[:, :, 0:L - dlt], scalar=pr, in1=si[:, :, dlt:L],
                           op0=ALU.mult, op1=ALU.add), reads=kk2, writes=kts)
                yield
                P.op(V, I("scalar_tensor_tensor", out=dr[:, :, dlt:L], in0=si[:, :, 0:L - dlt], scalar=npi, in1=tr[:, :, dlt:L],
                           op0=ALU.mult, op1=ALU.add), reads=kk2, writes=[kd_])
                P.op(V, I("scalar_tensor_tensor", out=di[:, :, dlt:L], in0=sr[:, :, 0:L - dlt], scalar=pi, in1=ti[:, :, dlt:L],
                           op0=ALU.mult, op1=ALU.add), reads=kk2, writes=[kd_])
                yield
                src, dst, ks, kd_ = dst, src, kd_, ks
            zl_r = v3(src[:, 0, 0:Tn])[:, :, L - 1]
            zl_i = v3(src[:, 1, 0:Tn])[:, :, L - 1]
            P.op(V, I("tensor_copy", out=zst[:, 0, ct, 0:nr], in_=zl_r), reads=[ks, "zst"], writes=["zst"])
            P.op(V, I("tensor_copy", out=zst[:, 1, ct, 0:nr], in_=zl_i), reads=[ks, "zst"], writes=["zst"])
            zb, kz = bs["zb"], bs["kz"]
            P.op(S, I("copy", out=zb[:, :, 0:Tn], in_=src[:, :, 0:Tn]), reads=[ks], writes=[kz])
            if r == 0:
                s5.ps, s5.pk = pbank(0, 4)
            psy, ky = s5.ps, s5.pk
            P.op(PE, I("matmul", out=psy[:, 0:Tn], lhsT=cemb_s[:, 0, ct, :], rhs=zb[:, 0, 0:Tn], start=(r == 0), stop=False),
                 reads=["cemb_s", kz], writes=[ky])
            P.op(PE, I("matmul", out=psy[:, 0:Tn], lhsT=cemb_s[:, 1, ct, :], rhs=zb[:, 1, 0:Tn], start=False, stop=(r == 3)),
                 reads=["cemb_s", kz], writes=[ky])
            if r == 3:
                a, b = tmpB[:, 0, 0:Tn], tmpB[:, 1, 0:Tn]
                P.op(V, I("scalar_tensor_tensor", out=a, in0=uT[:, cch, 0:Tn], scalar=s5d_s[:, 0, cch:cch + 1], in1=psy[:, 0:Tn],
                           op0=ALU.mult, op1=ALU.add), reads=[ky, "uT", "s5d_s"], writes=["tmpB"])
                P.op(V, I("tensor_tensor", out=b, in0=a, in1=a, op=ALU.mult), reads=["tmpB"], writes=["tmpB"])
                P.op(V, I("tensor_scalar", out=b, in0=b, scalar1=0.044715, scalar2=1.0, op0=ALU.mult, op1=ALU.add), reads=["tmpB"], writes=["tmpB"])
                P.op(V, I("tensor_tensor", out=b, in0=b, in1=a, op=ALU.mult), reads=["tmpB"], writes=["tmpB"])
                P.op(S, I("activation", out=b, in_=b, func=AF.Sigmoid, scale=1.5957691216057308), reads=["tmpB"], writes=["tmpB"])
                P.op(V, I("tensor_tensor", out=ygl[:, cch, 0:Tn], in0=a, in1=b, op=ALU.mult), reads=["tmpB"], writes=["ygl"])
            yield

        for ct0 in range(0, NCT, WAYS):
            gens = [body(ct0 + w, sets[w]) for w in range(min(WAYS, NCT - ct0))]
            alive = list(gens)
            while alive:
                for g in list(alive):
                    try:
                        next(g)
                    except StopIteration:
                        alive.remove(g)
            if hook is not None and (ct0 + WAYS) % 4 == 0:
                hook((ct0 + WAYS) // 4 - 1)
        def epi_glu(j, ps, pkey, m):
            P.op(S, I("activation", out=tmpB[:, 0, 0:Tn], in_=ps[:, 0:Tn], func=AF.Sigmoid, bias=s5d_s[:, 1, j:j + 1]),
                 reads=[pkey, "s5d_s"], writes=["tmpB"])
            P.op(V, I("tensor_tensor", out=yT[:, j, 0:Tn], in0=tmpB[:, 0, 0:Tn], in1=ygl[:, j, 0:Tn], op=ALU.mult),
                 reads=["tmpB", "ygl"], writes=["yT"])
        dense_fm(bglu, c.SC, c.S5W, ygl, "ygl", Tn, epi_glu)
    s5.ps = None

    def tok_proj(w, col0, ncols, Tn, g0, gs, dst, dkey, dcol0, scale=None):
        assert col0 % 128 == 0 and ncols % 256 == 0
        for cb in range(0, ncols, 256):
            wt, wkey = wtile(w, (col0 + cb) // 128, 2, 0, KD)
            ps, pkey = pbank()
            for k in range(KD):
                P.op(PE, I("matmul", out=ps[0:gs, 0:256].rearrange("p (c n) -> p c n", c=2), lhsT=xnT[:, k, g0:g0 + gs], rhs=wt[:, :, k, :],
                           start=(k == 0), stop=(k == KD - 1)), reads=[wkey, "xnT"], writes=[pkey])
            if scale is None:
                P.op(S, I("copy", out=dst[0:gs, dcol0 + cb:dcol0 + cb + 256], in_=ps[0:gs, 0:256]), reads=[pkey], writes=[dkey])
            else:
                P.op(S, I("activation", out=dst[0:gs, dcol0 + cb:dcol0 + cb + 256], in_=ps[0:gs, 0:256], func=AF.Copy, scale=scale),
                     reads=[pkey], writes=[dkey])

    def gla(Tn, groups, is_sample):
        o1 = c.S5W; o2 = o1 + c.GDK; o3 = o2 + c.GDK; o4 = o3 + c.GDV; o5 = o4 + c.GDV
        wt, wkey = wtile(bw_in_e, o5 // 128, 1, 0, KD)
        ps, pkey = pbank()
        for k in range(KD):
            P.op(PE, I("matmul", out=ps[0:16, 0:Tn], lhsT=wt[:, 0, k, 0:16], rhs=xnT[:, k, 0:Tn], start=(k == 0), stop=(k == KD - 1)),
                 reads=[wkey, "xnT"], writes=[pkey])
        P.op(V, I("tensor_copy", out=gkl[:, 0:Tn], in_=ps[0:16, 0:Tn]), reads=[pkey], writes=["gkl"])
        yield
        for h in range(c.GH):
            ps, pkey = pbank(4, 8)
            P.op(PE, I("matmul", out=ps[:, 0:Tn], lhsT=gku[:, h * 128:(h + 1) * 128], rhs=gkl[:, 0:Tn], start=True, stop=True),
                 reads=["gku", "gkl"], writes=[pkey])
            P.op(S, I("activation", out=Ep[:, 0:Tn], in_=ps[:, 0:Tn], func=AF.Exp, scale=-1.0, bias=ngkb[:, h:h + 1]), reads=[pkey, "ngkb"], writes=["Ep"])
            P.op(S, I("activation", out=Ep[:, 0:Tn], in_=Ep[:, 0:Tn], func=AF.Ln, bias=1.0), reads=["Ep"], writes=["Ep"])
            P.op(V, I("tensor_scalar", out=Ep[:, 0:Tn], in0=Ep[:, 0:Tn], scalar1=-1.0 / 16.0, scalar2=None, op0=ALU.mult), reads=["Ep"], writes=["Ep"])
            for (g0, gs, chunks) in groups:
                for (c0, cl, sq) in chunks:
                    P.op(V, I("tensor_tensor_scan", out=Em[:, c0:c0 + cl], data0=ones_f[:, 0:cl], data1=Ep[:, c0:c0 + cl],
                                                                          initial=0.0, op0=ALU.mult, op1=ALU.add), reads=["Ep", "ones_f"], writes=["Em"])
            P.op(S, I("activation", out=Ep[:, 0:Tn], in_=Em[:, 0:Tn], func=AF.Exp), reads=["Em"], writes=["Ep"])
            P.op(S, I("activation", out=Em[:, 0:Tn], in_=Em[:, 0:Tn], func=AF.Exp, scale=-1.0), reads=["Em", "Ep"], writes=["Em"])
            def epi_q(j, ps, pkey, m):
                P.op(V, I("scalar_tensor_tensor", out=qd[:, 0:Tn], in0=ps[:, 0:Tn], scalar=float(c.HK) ** -0.5, in1=Ep[:, 0:Tn], op0=ALU.mult, op1=ALU.mult),
                     reads=[pkey, "Ep"], writes=["qd"])
            dense_fm(bw_in_e, KD, 128, xnT, "xnT", Tn, epi_q, col0=o1 + h * 128)
            def epi_k(j, ps, pkey, m):
                P.op(V, I("tensor_tensor", out=kd[:, 0:Tn], in0=ps[:, 0:Tn], in1=Em[:, 0:Tn], op=ALU.mult), reads=[pkey, "Em"], writes=["kd"])
            dense_fm(bw_in_e, KD, 128, xnT, "xnT", Tn, epi_k, col0=o2 + h * 128)
            def epi_g(j, ps, pkey, m):
                P.op(S, I("activation", out=gsil[:, j, 0:Tn], in_=ps[:, 0:Tn], func=AF.Silu), reads=[pkey], writes=["gsil"])
            dense_fm(bw_in_e, KD, 256, xnT, "xnT", Tn, epi_g, col0=o4 + h * 256)
            for (g0, gs, chunks) in groups:
                for (c0, cl, sq) in chunks:
                    P.op(V, I("tensor_scalar", out=kdc[:, c0:c0 + cl], in0=kd[:, c0:c0 + cl], scalar1=Ep[:, c0 + cl - 1:c0 + cl],
                                                                    scalar2=None, op0=ALU.mult), reads=["kd", "Ep"], writes=["kdc"])
            for (g0, gs, chunks) in groups:
                msk = tri if not is_sample else blk
                tok_proj(bw_in_e, o3 + h * 256, 256, Tn, g0, gs, vtok, "vtok", 0)
                ps, pkey = pbank(4, 8)
                P.op(PE, I("matmul", out=ps[0:gs, 0:gs], lhsT=kd[:, g0:g0 + gs], rhs=qd[:, g0:g0 + gs], start=True, stop=True),
                     reads=["kd", "qd"], writes=[pkey])
                P.op(V, I("tensor_tensor", out=attT[0:gs, 0:gs], in0=ps[0:gs, 0:gs], in1=msk[0:gs, 0:gs], op=ALU.mult),
                     reads=[pkey, "tri", "blk"], writes=["attT"])
                ps2, pkey2 = pbank(4, 8)
                P.op(PE, I("matmul", out=ps2[0:gs, 0:128], lhsT=kdc[:, g0:g0 + gs], rhs=ones_id[:, :], start=True, stop=True),
                     reads=["kdc", "ones_id"], writes=[pkey2])
                P.op(S, I("copy", out=ktok[0:gs, 0:128], in_=ps2[0:gs, 0:128]), reads=[pkey2], writes=["ktok"])
                pso = [pbank(4, 8) for _ in range(2)]
                for ec in range(2):
                    P.op(PE, I("matmul", out=pso[ec][0][:, 0:gs], lhsT=vtok[0:gs, ec * 128:(ec + 1) * 128], rhs=attT[0:gs, 0:gs], start=True, stop=False),
                         reads=["vtok", "attT"], writes=[pso[ec][1]])
                for ci, (c0, cl, sq) in enumerate(chunks):
                    if is_sample:
                        P.dma(I("dma_start", out=S_sb[:, h, :], in_=st_gla[sq, h]), "S_ld", writes=["S_sb"])
                    P.op(S, I("copy", out=S_bf[:, :], in_=S_sb[:, h, :]), reads=["S_sb"], writes=["S_bf"])
                    last = ci == len(chunks) - 1
                    for ec in range(2):
                        P.op(PE, I("matmul", out=pso[ec][0][:, c0 - g0:c0 - g0 + cl], lhsT=S_bf[:, ec * 128:(ec + 1) * 128],
                                                                        rhs=qd[:, c0:c0 + cl], start=False, stop=last),
                             reads=["S_bf", "qd"], writes=[pso[ec][1]])
                    if len(chunks) > 1:
                        P.op(V, I("tensor_scalar", out=kwt[0:gs, 0:128], in0=ktok[0:gs, 0:128], scalar1=blk[0:gs, c0 - g0 + cl - 1:c0 - g0 + cl],
                                                                        scalar2=None, op0=ALU.mult), reads=["ktok", "blk"], writes=["kwt"])
                        lk_, lkey = kwt, "kwt"
                    else:
                        lk_, lkey = ktok, "ktok"
                    psS, kS = pbank(0, 4)
                    P.op(PE, I("matmul", out=psS[:, 0:256], lhsT=lk_[0:gs, 0:128], rhs=vtok[0:gs, 0:256], start=True, stop=True),
                         reads=[lkey, "vtok"], writes=[kS])
                    P.op(V, I("scalar_tensor_tensor", out=S_sb[:, h, :], in0=S_sb[:, h, :], scalar=Ep[:, c0 + cl - 1:c0 + cl],
                                                                                    in1=psS[:, 0:256], op0=ALU.mult, op1=ALU.add), reads=[kS, "S_sb", "S_bf", "Ep"], writes=["S_sb"])
                    if is_sample:
                        out_dma(o_sgla[sq, h], S_sb[:, h, :], "S_sb", "S_st")
                for ec in range(2):
                    P.op(V, I("tensor_copy", out=oT[:, ec, g0:g0 + gs], in_=pso[ec][0][:, 0:gs]), reads=[pso[ec][1]], writes=["oT"])
            for ec in range(2):
                P.op(S, I("activation", out=qwp[:, ec, 0:Tn], in_=oT[:, ec, 0:Tn], func=AF.Square), reads=["oT"], writes=["qwp"])
            ps, pkey = pbank()
            for ec in range(2):
                P.op(PE, I("matmul", out=ps[:, 0:Tn], lhsT=ones_bf[:, :], rhs=qwp[:, ec, 0:Tn], start=(ec == 0), stop=(ec == 1)),
                     reads=["qwp", "ones_bf"], writes=[pkey])
            P.op(S, I("activation", out=rstdB[:, 0:Tn], in_=ps[:, 0:Tn], func=AF.Sqrt, scale=1.0 / c.HV, bias=EPS), reads=[pkey], writes=["rstdB"])
            P.op(V, I("reciprocal", out=rstdB[:, 0:Tn], in_=rstdB[:, 0:Tn]), reads=["rstdB"], writes=["rstdB"])
            for ec in range(2):
                P.op(V, I("scalar_tensor_tensor", out=oT[:, ec, 0:Tn], in0=oT[:, ec, 0:Tn], scalar=gnorm_s[:, ec:ec + 1], in1=rstdB[:, 0:Tn],
                                                                op0=ALU.mult, op1=ALU.mult), reads=["oT", "rstdB", "gnorm_s"], writes=["oT"])
                P.op(V, I("tensor_tensor", out=yT[:, c.SC + h * 2 + ec, 0:Tn], in0=oT[:, ec, 0:Tn], in1=gsil[:, ec, 0:Tn], op=ALU.mult),
                     reads=["oT", "gsil"], writes=["yT"])
            yield

    ones_id = P.sb("ones_id", [128, 128], BF16)
    P.op(V, I("tensor_copy", out=ones_id[:, :], in_=ident[:, :]), reads=["ident"], writes=["ones_id"])

    def mlstm(Tn, groups, runs, is_sample):
        for h in range(c.MH):
            bi, nbf = mb_s[:, h:h + 1], nmb[:, c.MH + h:c.MH + h + 1]
            def epi_i(j, ps, pkey, m):
                P.op(V, I("tensor_scalar", out=gB[:, 0:Tn], in0=ps[:, 0:Tn], scalar1=bi, scalar2=None, op0=ALU.add), reads=[pkey, "mb_s"], writes=["gB"])
            dense_fm(bw_gate, KD, 128, xnT, "xnT", Tn, epi_i, col0=h * 128)
            def epi_f(j, ps, pkey, m):
                P.op(S, I("activation", out=FgB[:, 0:Tn], in_=ps[:, 0:Tn], func=AF.Exp, scale=-1.0, bias=nbf), reads=[pkey, "nmb"], writes=["FgB"])
                P.op(S, I("activation", out=FgB[:, 0:Tn], in_=FgB[:, 0:Tn], func=AF.Ln, bias=1.0), reads=["FgB"], writes=["FgB"])
                P.op(V, I("tensor_scalar", out=wpB[:, 0:Tn], in0=FgB[:, 0:Tn], scalar1=-1.0, scalar2=None, op0=ALU.mult), reads=["FgB"], writes=["wpB"])
            dense_fm(bw_gate, KD, 128, xnT, "xnT", Tn, epi_f, col0=(c.MH + h) * 128)
            for ri, (r0, rl) in enumerate(runs):
                if is_sample:
                    fin = 0.0
                    min_ = mm0[:, ri * c.MH + h:ri * c.MH + h + 1]
                else:
                    fin = Fcar[:, h:h + 1]
                    min_ = Mcar[:, h:h + 1]
                P.op(V, I("tensor_tensor_scan", out=FgB[:, r0:r0 + rl], data0=ones_f[:, 0:rl], data1=wpB[:, r0:r0 + rl],
                                                                              initial=fin, op0=ALU.mult, op1=ALU.add), reads=["wpB", "ones_f", "Fcar", "FgB"], writes=["FgB"])
                P.op(V, I("tensor_tensor", out=gB[:, r0:r0 + rl], in0=gB[:, r0:r0 + rl], in1=FgB[:, r0:r0 + rl], op=ALU.subtract),
                     reads=["gB", "FgB"], writes=["gB"])
                P.op(V, I("tensor_tensor_scan", out=MB[:, r0:r0 + rl], data0=ones_f[:, 0:rl], data1=gB[:, r0:r0 + rl],
                                                                                initial=min_, op0=ALU.mult, op1=ALU.max), reads=["gB", "ones_f", "Mcar", "mm0", "MB"], writes=["MB"])
            P.op(V, I("tensor_tensor", out=emB[:, 0:Tn], in0=FgB[:, 0:Tn], in1=MB[:, 0:Tn], op=ALU.add), reads=["FgB", "MB"], writes=["emB"])
            for ri, (r0, rl) in enumerate(runs):
                if is_sample:
                    P.op(V, I("tensor_copy", out=mout[0:1, h * NS + ri:h * NS + ri + 1], in_=emB[0:1, r0 + rl - 1:r0 + rl]),
                         reads=["emB"], writes=["mout"])
                else:
                    P.op(V, I("tensor_copy", out=mout[0:1, h:h + 1], in_=emB[0:1, r0 + rl - 1:r0 + rl]), reads=["emB"], writes=["mout"])
            P.op(S, I("activation", out=emB[:, 0:Tn], in_=emB[:, 0:Tn], func=AF.Exp, scale=-1.0), reads=["emB", "mout"], writes=["emB"])
            for (g0, gs, chunks) in groups:
                for (c0, cl, sq) in chunks:
                    first = any(c0 == r0 for (r0, rl) in runs)
                    if first:
                        ri = [i for i, (r0, rl) in enumerate(runs) if r0 == c0][0]
                        mp = mm0[:, ri * c.MH + h:ri * c.MH + h + 1] if is_sample else Mcar[:, h:h + 1]
                    else:
                        mp = MB[:, c0 - 1:c0]
                    P.op(S, I("activation", out=wpB[:, c0:c0 + cl], in_=MB[:, c0:c0 + cl], func=AF.Exp, scale=-1.0, bias=mp),
                         reads=["MB", "Mcar", "mm0", "wpB", "FgB"], writes=["wpB"])
            def epi_q(j, ps, pkey, m):
                P.op(S, I("copy", out=qTb[:, j, 0:Tn], in_=ps[:, 0:Tn]), reads=[pkey], writes=["qTb"])
                P.op(V, I("tensor_tensor", out=qwp[:, j, 0:Tn], in0=ps[:, 0:Tn], in1=wpB[:, 0:Tn], op=ALU.mult), reads=[pkey, "wpB"], writes=["qwp"])
            dense_fm(bw_in_o, KD, 512, xnT, "xnT", Tn, epi_q, col0=h * 512)
            def epi_k(j, ps, pkey, m):
                P.op(S, I("activation", out=kTb[:, j, 0:Tn], in_=ps[:, 0:Tn], func=AF.Copy, scale=float(c.DH) ** -0.5), reads=[pkey], writes=["kTb"])
            dense_fm(bw_in_o, KD, 512, xnT, "xnT", Tn, epi_k, col0=D + h * 512)
            def epi_og(j, ps, pkey, m):
                P.op(S, I("activation", out=ogs[:, j, 0:Tn], in_=ps[:, 0:Tn], func=AF.Sigmoid), reads=[pkey], writes=["ogs"])
            dense_fm(bw_in_o, KD, 512, xnT, "xnT", Tn, epi_og, col0=3 * D + h * 512)
            if not is_sample:
                P.op(S, I("copy", out=C_bf[:, :, :], in_=C_sb[:, h, :, :]), reads=[f"C_sb{h}"], writes=["C_bf"])
            for (g0, gs, chunks) in groups:
                msk = tri if not is_sample else blk
                tok_proj(bw_in_o, D + h * 512, 512, Tn, g0, gs, ktok, "ktok", 0, scale=float(c.DH) ** -0.5)
                tok_proj(bw_in_o, 2 * D + h * 512, 512, Tn, g0, gs, vtok, "vtok", 0)
                ps, pkey = pbank(5, 8)
                P.op(PE, I("transpose", out=ps[0:gs, 0:128], in_=gB[:, g0:g0 + gs], identity=ident[:, :]), reads=["gB", "ident"], writes=[pkey])
                P.op(V, I("tensor_copy", out=gcol[0:gs, 0:1], in_=ps[0:gs, 0:1]), reads=[pkey], writes=["gcol"])
                P.op(S, I("activation", out=wT[0:gs, 0:gs], in_=MB[0:gs, g0:g0 + gs], func=AF.Exp, scale=-1.0, bias=gcol[0:gs, 0:1]),
                     reads=["MB", "gcol"], writes=["wT"])
                P.op(V, I("tensor_tensor", out=wTm[0:gs, 0:gs], in0=wT[0:gs, 0:gs], in1=msk[0:gs, 0:gs], op=ALU.mult), reads=["wT", "tri", "blk"], writes=["wTm"])
                ps, pkey = pbank(5, 8)
                for dc in range(4):
                    P.op(PE, I("matmul", out=ps[0:gs, 0:gs], lhsT=kTb[:, dc, g0:g0 + gs], rhs=qTb[:, dc, g0:g0 + gs], start=(dc == 0), stop=(dc == 3)),
                         reads=["kTb", "qTb"], writes=[pkey])
                P.op(V, I("tensor_tensor", out=attT[0:gs, 0:gs], in0=ps[0:gs, 0:gs], in1=wTm[0:gs, 0:gs], op=ALU.mult), reads=[pkey, "wTm"], writes=["attT"])
                psn = [(pb[i], f"pb{i}") for i in range(4)]
                psd, kdn = pb[4], "pb4"
                for ec in range(4):
                    P.op(PE, I("matmul", out=psn[ec][0][:, 0:gs], lhsT=vtok[0:gs, ec * 128:(ec + 1) * 128], rhs=attT[0:gs, 0:gs], start=True, stop=False),
                         reads=["vtok", "attT"], writes=[psn[ec][1]])
                P.op(PE, I("matmul", out=psd[:, 0:gs], lhsT=ones_bf[0:gs, :], rhs=attT[0:gs, 0:gs], start=True, stop=False), reads=["ones_bf", "attT"], writes=[kdn])
                NSL = c.MH

                def update_state(ci, c0, cl, sq, sl):
                    lc = c0 - g0 + cl - 1
                    P.op(V, I("tensor_scalar", out=kwt[0:gs, 0:512], in0=ktok[0:gs, 0:512], scalar1=wTm[0:gs, lc:lc + 1], scalar2=None, op0=ALU.mult),
                         reads=["ktok", "wTm"], writes=["kwt"])
                    dcol = wpB[:, c0 + cl - 1:c0 + cl]
                    for dc in range(4):
                        psC, kC = pbank(5, 8)
                        P.op(PE, I("matmul", out=psC[:, 0:512], lhsT=kwt[0:gs, dc * 128:(dc + 1) * 128], rhs=vtok[0:gs, 0:512], start=True, stop=True),
                             reads=["kwt", "vtok"], writes=[kC])
                        P.op(V, I("scalar_tensor_tensor", out=C_sb[:, sl, dc, :], in0=C_sb[:, sl, dc, :], scalar=dcol, in1=psC[:, 0:512],
                                   op0=ALU.mult, op1=ALU.add), reads=[kC, f"C_sb{sl}", "C_bf", "wpB"], writes=[f"C_sb{sl}"])
                        psN, kN = pbank(5, 8)
                        P.op(PE, I("matmul", out=psN[:, 0:2], lhsT=kwt[0:gs, dc * 128:(dc + 1) * 128], rhs=ones_bf[0:gs, 0:2], start=True, stop=True),
                             reads=["kwt", "ones_bf"], writes=[kN])
                        P.op(V, I("scalar_tensor_tensor", out=n_sb[:, sl, dc:dc + 1], in0=n_sb[:, sl, dc:dc + 1], scalar=dcol, in1=psN[:, 0:1],
                                   op0=ALU.mult, op1=ALU.add), reads=[kN, f"n_sb{sl}", "Nrep", "wpB"], writes=[f"n_sb{sl}"])
                    if is_sample:
                        out_dma(o_smc[sq, h].rearrange("(dc p) e -> p dc e", p=128), C_sb[:, sl, :, :], f"C_sb{sl}", f"C_st{sl}")
                        out_dma(o_smn[sq, h], n_sb[:, sl, :], f"n_sb{sl}", f"n_st{sl}")
                    else:
                        P.op(S, I("copy", out=C_bf[:, :, :], in_=C_sb[:, sl, :, :]), reads=[f"C_sb{sl}"], writes=["C_bf"])

                def load_state(ci):
                    sq_ = chunks[ci][2]
                    sl_ = ci % NSL
                    P.dma(I("dma_start", out=C_sb[:, sl_, :, :], in_=st_mc[sq_, h].rearrange("(dc p) e -> p dc e", p=128)), f"C_ld{sl_}", writes=[f"C_sb{sl_}"])
                    P.dma(I("dma_start", out=n_sb[:, sl_, :], in_=st_mn[sq_, h]), f"n_ld{sl_}", writes=[f"n_sb{sl_}"])

                PF = max(1, min(2, NSL - 1))
                if is_sample:
                    for ci_ in range(min(PF, len(chunks))):
                        load_state(ci_)
                for ci, (c0, cl, sq) in enumerate(chunks):
                    last = ci == len(chunks) - 1
                    sl = ci % NSL if is_sample else h
                    if is_sample:
                        if ci + PF < len(chunks):
                            load_state(ci + PF)
                        P.op(S, I("copy", out=C_bf[:, :, :], in_=C_sb[:, sl, :, :]), reads=[f"C_sb{sl}"], writes=["C_bf"])
                    for dc in range(4):
                        P.op(V, I("tensor_scalar", out=Nrep[:, dc, :], in0=ones_bf[:, :], scalar1=n_sb[:, sl, dc:dc + 1], scalar2=None, op0=ALU.mult),
                             reads=["ones_bf", f"n_sb{sl}"], writes=["Nrep"])
                    for dc in range(4):
                        lst = last and dc == 3
                        for ec in range(4):
                            P.op(PE, I("matmul", out=psn[ec][0][:, c0 - g0:c0 - g0 + cl], lhsT=C_bf[:, dc, ec * 128:(ec + 1) * 128],
                                                                                         rhs=qwp[:, dc, c0:c0 + cl], start=False, stop=lst), reads=["C_bf", "qwp"], writes=[psn[ec][1]])
                        P.op(PE, I("matmul", out=psd[:, c0 - g0:c0 - g0 + cl], lhsT=Nrep[:, dc, :], rhs=qwp[:, dc, c0:c0 + cl], start=False, stop=lst),
                             reads=["Nrep", "qwp"], writes=[kdn])
                    if is_sample:
                        update_state(ci, c0, cl, sq, sl)
                P.op(S, I("activation", out=rden[:, 0:gs], in_=psd[:, 0:gs], func=AF.Abs), reads=[kdn], writes=["rden"])
                P.op(V, I("tensor_tensor", out=rden[:, 0:gs], in0=rden[:, 0:gs], in1=emB[:, g0:g0 + gs], op=ALU.max), reads=["rden", "emB"], writes=["rden"])
                P.op(V, I("reciprocal", out=rden[:, 0:gs], in_=rden[:, 0:gs]), reads=["rden"], writes=["rden"])
                for ec in range(4):
                    P.op(V, I("tensor_tensor", out=oT[:, ec, g0:g0 + gs], in0=psn[ec][0][:, 0:gs], in1=rden[:, 0:gs], op=ALU.mult), reads=[psn[ec][1], "rden"], writes=["oT"])
                    P.op(V, I("tensor_tensor", out=oT[:, ec, g0:g0 + gs], in0=oT[:, ec, g0:g0 + gs], in1=ogs[:, ec, g0:g0 + gs], op=ALU.mult), reads=["oT", "ogs"], writes=["oT"])
                if not is_sample:
                    for ci, (c0, cl, sq) in enumerate(chunks):
                        update_state(ci, c0, cl, sq, h)
            if not is_sample:
                r0, rl = runs[-1]
                P.op(V, I("tensor_copy", out=Fcar[:, h:h + 1], in_=FgB[:, r0 + rl - 1:r0 + rl]), reads=["FgB"], writes=["Fcar"])
                P.op(V, I("tensor_copy", out=Mcar[:, h:h + 1], in_=MB[:, r0 + rl - 1:r0 + rl]), reads=["MB"], writes=["Mcar"])
            for ec in range(4):
                P.op(S, I("activation", out=uT[:, ec, 0:Tn], in_=oT[:, ec, 0:Tn], func=AF.Square), reads=["oT"], writes=["uT"])
            ps, pkey = pbank()
            for ec in range(4):
                P.op(PE, I("matmul", out=ps[:, 0:Tn], lhsT=ones_bf[:, :], rhs=uT[:, ec, 0:Tn], start=(ec == 0), stop=(ec == 3)), reads=["uT", "ones_bf"], writes=[pkey])
            P.op(S, I("activation", out=rstdB[:, 0:Tn], in_=ps[:, 0:Tn], func=AF.Sqrt, scale=1.0 / c.DH, bias=EPS), reads=[pkey], writes=["rstdB"])
            P.op(V, I("reciprocal", out=rstdB[:, 0:Tn], in_=rstdB[:, 0:Tn]), reads=["rstdB"], writes=["rstdB"])
            for ec in range(4):
                P.op(V, I("scalar_tensor_tensor", out=yT[:, h * 4 + ec, 0:Tn], in0=oT[:, ec, 0:Tn], scalar=mnorm_s[:, ec:ec + 1], in1=rstdB[:, 0:Tn],
                                                                op0=ALU.mult, op1=ALU.mult), reads=["oT", "rstdB", "mnorm_s"], writes=["yT"])

    def segment(src, dst, Tn, groups, runs, is_sample):
        load_x(src, Tn)
        rmsnorm(0, Tn)
        def epi_u(j, ps, pkey, m):
            P.op(S, I("copy", out=uT[:, j, 0:Tn], in_=ps[:, 0:Tn]), reads=[pkey], writes=["uT"])
        dense_fm(bw_in_e, KD, c.S5W, xnT, "xnT", Tn, epi_u)
        gg = gla(Tn, groups, is_sample)
        next(gg)

        def hook(chunk):
            if chunk % 2 == 0:
                next(gg, None)
        s5(Tn, runs, is_sample, hook=hook)
        for _ in gg:
            pass
        dense_fm(bw_out_e, KD, D, yT, "yT", Tn, resid_epi(Tn))
        if getattr(c, "dbg", 0) == 1:
            store_y(dst, Tn, norm=False)
            return
        mlp(0, Tn)
        rmsnorm(1, Tn)
        mlstm(Tn, groups, runs, is_sample)
        dense_fm(bw_out_o, KD, D, yT, "yT", Tn, resid_epi(Tn))
        mlp(1, Tn)
        store_y(dst, Tn)

    P.op(V, I("memset", zst[:], 0.0), writes=["zst"])
    P.op(V, I("memset", S_sb[:], 0.0), writes=["S_sb"])
    P.op(V, I("memset", C_sb[:], 0.0), writes=[f"C_sb{i}" for i in range(c.MH)])
    P.op(V, I("memset", n_sb[:], 0.0), writes=[f"n_sb{i}" for i in range(c.MH)])
    P.op(V, I("memset", Fcar[:], 0.0), writes=["Fcar"])
    P.op(V, I("memset", mout[:], 0.0), writes=["mout"])
    P.op(V, I("memset", Mcar[:], NEG), writes=["Mcar"])
    for s0 in range(0, c.SEQ, T):
        groups = [(g0, 128, [(g0, 128, 0)]) for g0 in range(0, T, 128)]
        segment(xp[s0:s0 + T, :], yp[s0:s0 + T, :], T, groups, [(0, T)], False)
    zk = ["zst", "cco", "tiny"]
    zr, zi = zst[:, 0, :, 0], zst[:, 1, :, 0]
    tt(t0, zr, cco[:, 0, :], ALU.mult, keys=zk); tt(t1, zi, cco[:, 1, :], ALU.mult, keys=zk); tt(t0, t0, t1, ALU.subtract, keys=zk)
    tt(t2, zr, cco[:, 1, :], ALU.mult, keys=zk); tt(t3, zi, cco[:, 0, :], ALU.mult, keys=zk); tt(t2, t2, t3, ALU.add, keys=zk)
    out_dma(o_ps5[0], t0, "tiny", "fin"); out_dma(o_ps5[1], t2, "tiny", "fin")
    for h in range(c.GH):
        out_dma(o_pgla[h], S_sb[:, h, :], "S_sb", "fin")
    for h in range(c.MH):
        out_dma(o_pmc[h].rearrange("(dc p) e -> p dc e", p=128), C_sb[:, h, :, :], f"C_sb{h}", "fin")
        out_dma(o_pmn[h], n_sb[:, h, :], f"n_sb{h}", "fin")
    out_dma(o_pmm, mout[0:1, 0:c.MH], "mout", "fin")

    if NS > 0:
        TS = c.TS
        P.dma(I("dma_start", out=zst[:, 0, :, :], in_=st_s5[0]), "zld", reads=["tiny"], writes=["zst"])
        P.dma(I("dma_start", out=zst[:, 1, :, :], in_=st_s5[1]), "zld", reads=["tiny"], writes=["zst"])
        P.seal("zld")
        W_ = NCT * NS
        nsl = (W_ + T - 1) // T
        assert 6 * nsl <= KD
        big = [hT[:, nsl * i:nsl * i + nsl, :].rearrange("p a b -> p (a b)")[:, 0:W_].rearrange("p (a b) -> p a b", b=NS) for i in range(6)]
        cr_b = cinv[:, 0, :].unsqueeze(2).broadcast_to([128, NCT, NS]); ci_b = cinv[:, 1, :].unsqueeze(2).broadcast_to([128, NCT, NS])
        zk2 = ["zst", "cinv", "tiny", "hT"]
        tt(big[0], zst[:, 0, :, :], cr_b, ALU.mult, keys=zk2); tt(big[1], zst[:, 1, :, :], ci_b, ALU.mult, keys=zk2)
        tt(big[2], zst[:, 0, :, :], ci_b, ALU.mult, keys=zk2); tt(big[3], zst[:, 1, :, :], cr_b, ALU.mult, keys=zk2)
        tt(zst[:, 0, :, :], big[0], big[1], ALU.subtract, keys=zk2); tt(zst[:, 1, :, :], big[2], big[3], ALU.add, keys=zk2)
        chunks = [(4 * s, 4, s) for s in range(NS)]
        segment(xs, ys, TS, [(0, TS, chunks)], [(4 * s, 4) for s in range(NS)], True)
        cr_b = cco[:, 0, :].unsqueeze(2).broadcast_to([128, NCT, NS]); ci_b = cco[:, 1, :].unsqueeze(2).broadcast_to([128, NCT, NS])
        zk3 = ["zst", "cco", "tiny", "hT"]
        tt(big[0], zst[:, 0, :, :], cr_b, ALU.mult, keys=zk3); tt(big[1], zst[:, 1, :, :], ci_b, ALU.mult, keys=zk3)
        tt(big[2], zst[:, 0, :, :], ci_b, ALU.mult, keys=zk3); tt(big[3], zst[:, 1, :, :], cr_b, ALU.mult, keys=zk3)
        tt(big[4], big[0], big[1], ALU.subtract, keys=zk3); tt(big[5], big[2], big[3], ALU.add, keys=zk3)
        out_dma(o_ss5[0], big[4], "hT", "fin2"); out_dma(o_ss5[1], big[5], "hT", "fin2")
        out_dma(o_smm, mout[0:1, 0:c.MH * NS], "mout", "fin2")

    fw = {}
    for ev in final_evs:
        fw[ev[1]] = max(fw.get(ev[1], 0), ev[2])
    P.emit(final_waits=[("dma", s, k) for s, k in fw.items()])
    nc._stats = P.stats
    return nc


def _lay_vec(v, n):
    return np.ascontiguousarray(v.reshape(n, 128).T)


def make_inputs(cfg, core, inp):
    c = cfg
    f = np.float32
    b = core % inp["x_prompt"].shape[0]
    NS = c.NS
    s0 = core * NS
    m = {}
    m["xp"] = np.ascontiguousarray(inp["x_prompt"][b], dtype=f)
    m["xs"] = np.ascontiguousarray(inp["x_sample"][s0:s0 + NS].reshape(NS * 4, c.D), dtype=f)

    def s5lay(a):
        return a.reshape(NS, c.NCT, 2, 64).transpose(2, 3, 1, 0).reshape(128, c.NCT, NS)
    m["st_s5"] = np.ascontiguousarray(np.stack([s5lay(inp["state_s5_re"][0, s0:s0 + NS]), s5lay(inp["state_s5_im"][0, s0:s0 + NS])]), dtype=f)
    m["st_gla"] = np.ascontiguousarray(inp["state_gla"][0, s0:s0 + NS], dtype=f)
    m["st_mc"] = np.ascontiguousarray(inp["state_mlstm_c"][0, s0:s0 + NS], dtype=f)
    m["st_mn"] = np.ascontiguousarray(inp["state_mlstm_n"][0, s0:s0 + NS].reshape(NS, c.MH, 4, 128).transpose(0, 1, 3, 2), dtype=f)
    m["st_mm"] = np.ascontiguousarray(inp["state_mlstm_m"][0, s0:s0 + NS].reshape(1, NS * c.MH), dtype=f)
    return m


def make_shared(cfg, inp):
    c = cfg
    f = np.float32
    m = {}
    nv = [inp["norm_mix"][0], inp["norm_mix"][1], inp["norm_mlp"][0], inp["norm_mlp"][1], inp["norm_final"]]
    m["nrm"] = np.ascontiguousarray(np.stack([_lay_vec(v, c.KD) for v in nv], axis=1), dtype=f)
    m["w_in_e"] = np.ascontiguousarray(inp["w_in_even"][0], dtype=f)
    m["w_out_e"] = np.ascontiguousarray(inp["w_out_even"][0], dtype=f)
    m["w_in_o"] = np.ascontiguousarray(inp["w_in_odd"][0], dtype=f)
    m["w_out_o"] = np.ascontiguousarray(inp["w_out_odd"][0], dtype=f)
    m["w_gate"] = np.ascontiguousarray(np.repeat(inp["w_in_odd"][0][:, 4 * c.D:], 128, axis=1), dtype=f)
    m["w_up"] = np.ascontiguousarray(inp["w_mlp_up"], dtype=f)
    m["w_dn"] = np.ascontiguousarray(inp["w_mlp_down"], dtype=f)

    def gl(a):
        return a.reshape(c.NCT, 2, 64).transpose(1, 2, 0).reshape(128, c.NCT)
    ls = np.repeat(inp["s5_log_step"][0][:, None], 64, axis=1)
    m["s5p"] = np.ascontiguousarray(np.stack([gl(inp["s5_a_re"][0]), gl(inp["s5_a_im"][0]), gl(ls)], axis=1), dtype=f)
    bem = np.zeros((2, 128, c.SC, 2, 128), f)
    cem = np.zeros((2, 128, c.NCT, 128), f)
    for ri, (bsrc, csrc) in enumerate(((inp["s5_b_re"][0], inp["s5_c_re"][0]), (inp["s5_b_im"][0], inp["s5_c_im"][0]))):
        bg = bsrc.reshape(c.SC, 4, 2, 64, 16)
        cg = csrc.reshape(c.SC, 4, 2, 16, 64)
        for g2 in range(2):
            blk_ = bg[:, :, g2].transpose(1, 3, 0, 2)
            for r in range(4):
                bem[ri].reshape(4, 2, 16, c.SC, 2, 2, 64)[r, g2, :, :, r % 2, g2, :] = blk_[r]
            cb_ = cg[:, :, g2]
            for r in range(4):
                cem[ri].reshape(2, 64, c.SC, 4, 128)[g2, :, :, r, r * 32 + g2 * 16:r * 32 + g2 * 16 + 16] = cb_[:, r].transpose(2, 0, 1)
    m["bemb"] = np.ascontiguousarray(bem.reshape(2, 128, c.SC * 2, 128))
    m["cemb"] = cem
    m["s5d"] = np.ascontiguousarray(np.stack([_lay_vec(inp["s5_d"][0], c.SC), _lay_vec(inp["s5_glu_b"][0], c.SC)], axis=1), dtype=f)
    m["glu_w"] = np.ascontiguousarray(inp["s5_glu_w"][0], dtype=f)
    m["gk_up"] = np.ascontiguousarray(inp["gla_gk_up"][0], dtype=f)
    m["gkb"] = _lay_vec(inp["gla_gk_b"][0], c.GH).astype(f)
    m["gnorm"] = _lay_vec(inp["gla_norm"][0], 2).astype(f)
    m["mb"] = np.ascontiguousarray(np.concatenate([inp["mlstm_b_i"][0], inp["mlstm_b_f"][0]]).reshape(1, 2 * c.MH), dtype=f)
    m["mnorm"] = _lay_vec(inp["mlstm_norm"][0], 4).astype(f)
    m["ident"] = np.eye(128, dtype=f)
    m["tri"] = np.triu(np.ones((128, 128), f))
    i = np.arange(128)
    m["blk"] = ((i[:, None] // 4 == i[None, :] // 4) & (i[:, None] <= i[None, :])).astype(f)
    return m


def assemble(cfg, results, n_prompt, n_cores):
    c = cfg
    NS = c.NS
    yp = np.stack([results[b]["yp"] for b in range(n_prompt)])
    ys = np.concatenate([results[k]["ys"].reshape(NS, 4, c.D) for k in range(n_cores)], axis=0)

    def unl(a):
        return a.reshape(2, 64, c.NCT).transpose(2, 0, 1).reshape(c.NG, 64)
    ps5 = [np.stack([unl(results[b]["o_ps5"][ri]) for b in range(n_prompt)])[None] for ri in range(2)]
    pgla = np.stack([results[b]["o_pgla"] for b in range(n_prompt)])[None]
    pmc = np.stack([results[b]["o_pmc"] for b in range(n_prompt)])[None]
    pmn = np.stack([results[b]["o_pmn"].transpose(0, 2, 1).reshape(c.MH, 512) for b in range(n_prompt)])[None]
    pmm = np.stack([results[b]["o_pmm"].reshape(c.MH) for b in range(n_prompt)])[None]

    def unls(a):
        return a.reshape(2, 64, c.NCT, NS).transpose(3, 2, 0, 1).reshape(NS, c.NG, 64)
    ss5 = [np.concatenate([unls(results[k]["o_ss5"][ri]) for k in range(n_cores)])[None] for ri in range(2)]
    sgla = np.concatenate([results[k]["o_sgla"] for k in range(n_cores)])[None]
    smc = np.concatenate([results[k]["o_smc"] for k in range(n_cores)])[None]
    smn = np.concatenate([results[k]["o_smn"].transpose(0, 1, 3, 2).reshape(NS, c.MH, 512) for k in range(n_cores)])[None]
    smm = np.concatenate([results[k]["o_smm"].reshape(c.MH, NS).T for k in range(n_cores)])[None]
    outs = (yp, ys, ps5[0], ps5[1], pgla, pmc, pmn, pmm, ss5[0], ss5[1], sgla, smc, smn, smm)
    return tuple(np.ascontiguousarray(o, dtype=np.float32) for o in outs)


def kernel(**inputs):
    n_cores = 8
    cfg = Cfg()
    inp = {k: np.asarray(v) for k, v in inputs.items()}
    nc = build(cfg)
    shared = make_shared(cfg, inp)
    in_maps = []
    for k in range(n_cores):
        m = dict(shared)
        m.update(make_inputs(cfg, k, inp))
        in_maps.append(m)
    res = run_bass_kernel_spmd(nc, in_maps, core_ids=list(range(n_cores)))
    return assemble(cfg, res.results, inp["x_prompt"].shape[0], n_cores)
```
